# Optimizing a Trainium2 kernel written in Bass

```python
import math
import jax, jax.numpy as jnp
from jax import lax
import numpy as np

D_MODEL = 1024
BATCH = 16
SEQ = 2048
DEPTH = 1

N_META = 16
D_MIX = D_MODEL
D_HYENA = D_MIX // 2
D_HGRN = D_MIX - D_HYENA
HYENA_ORDER = 2
SHORT_CONV = 3
FILTER_EMB = 33
FILTER_BANDS = (FILTER_EMB - 1) // 2
FILTER_HIDDEN = 64
DECAY_TARGET = 1e-2
FAST_DECAY_PCT = 0.3
SLOW_DECAY_PCT = 1.5
HGRN_HEAD_DIM = 128
HGRN_HEADS = D_HGRN // HGRN_HEAD_DIM
CHUNK = 64
N_GROUPS = 8
EXPERTS_PER_GROUP = 8
N_EXPERTS = N_GROUPS * EXPERTS_PER_GROUP
TOP_K = 2
D_EXPERT = D_MODEL // 2
MOE_BLOCK = 128
D_HYENA_PROJ = (HYENA_ORDER + 1) * D_HYENA
D_HGRN_PROJ = 5 * D_HGRN
D_IN_PROJ = D_HYENA_PROJ + D_HGRN_PROJ
EPS = 1e-6

kernel_name = 'hymba_hyena_hgrn2_hmoe_encoder'


def rms_norm(x, gain):
    xf = x.astype(jnp.float32)
    y = xf * lax.rsqrt(jnp.mean(xf * xf, axis=-1, keepdims=True) + EPS)
    return (y * gain.astype(jnp.float32)).astype(x.dtype)


def centred_short_conv(u, w, b):
    L = u.shape[1]
    half = SHORT_CONV // 2
    up = jnp.pad(u, ((0, 0), (half, SHORT_CONV - 1 - half), (0, 0)))
    y = b
    for j in range(SHORT_CONV):
        y = y + up[:, j:j + L] * w[j]
    return y


def hyena_filter_spectra(L, w1, b1, w2, b2, w3, freq):
    f32 = jnp.float32
    pos = jnp.arange(L, dtype=f32)
    t = pos / max(L - 1, 1)
    bands = jnp.linspace(1e-4, FILTER_BANDS - 1, FILTER_BANDS, dtype=f32)
    ang = (2.0 * math.pi / L) * pos[:, None] * bands[None, :]
    z = jnp.concatenate([t[:, None], jnp.cos(ang), -jnp.sin(ang)], axis=-1)
    fr = freq.astype(f32)
    hid = jnp.sin(fr * (z @ w1.astype(f32) + b1.astype(f32)))
    hid = jnp.sin(fr * (hid @ w2.astype(f32) + b2.astype(f32)))
    filt = (hid @ w3.astype(f32)).reshape(L, 2, HYENA_ORDER, D_HYENA)
    deltas = jnp.abs(jnp.linspace(math.log(DECAY_TARGET) / SLOW_DECAY_PCT,
                                  math.log(DECAY_TARGET) / FAST_DECAY_PCT, D_HYENA, dtype=f32))
    window = jnp.exp(-t[:, None] * deltas[None, :])
    filt = filt * window[:, None, None, :]
    h_fwd, h_bwd = filt[:, 0], filt[:, 1]
    two_sided = jnp.concatenate([h_fwd, jnp.zeros_like(h_fwd[:1]), h_bwd[:0:-1]], axis=0)
    return jnp.fft.rfft(two_sided, axis=0)


def fft_long_conv(u, spec, skip):
    L = u.shape[1]
    uf = u.astype(jnp.float32)
    y = jnp.fft.irfft(jnp.fft.rfft(uf, n=2 * L, axis=1) * spec[None], n=2 * L, axis=1)[:, :L]
    return (y + uf * skip.astype(jnp.float32)).astype(u.dtype)


def hyena_mixer(p, conv_w, conv_b, w1, b1, w2, b2, w3, freq, skip):
    L = p.shape[1]
    u = centred_short_conv(p, conv_w, conv_b)
    z = u[..., :D_HYENA]
    spectra = hyena_filter_spectra(L, w1, b1, w2, b2, w3, freq)
    for n in range(HYENA_ORDER):
        gate = u[..., (n + 1) * D_HYENA:(n + 2) * D_HYENA]
        z = gate * fft_long_conv(z, spectra[:, n], skip[n])
    return z


def chunked_gated_linear_scan(q, k, v, log_f):
    B, T, H, DK = q.shape
    DV = v.shape[-1]
    N = T // CHUNK
    q, k, v, log_f = [a.reshape(B, N, CHUNK, H, a.shape[-1]) for a in (q, k, v, log_f)]
    b = jnp.cumsum(log_f, axis=2)
    b_last = b[:, :, -1]
    b_mid = b[:, :, CHUNK // 2][:, :, None]
    scores = jnp.einsum('bnthd,bnshd->bnhts', q * jnp.exp(b - b_mid), k * jnp.exp(b_mid - b))
    lower_tri = jnp.tril(jnp.ones((CHUNK, CHUNK), dtype=bool))
    scores = jnp.where(lower_tri, scores, 0.0)
    o_intra = jnp.einsum('bnhts,bnshv->bnthv', scores, v)
    chunk_state = jnp.einsum('bnshd,bnshv->bnhdv', k * jnp.exp(b_last[:, :, None] - b), v)
    chunk_decay = jnp.exp(b_last)

    def step(S, inp):
        dec, U = inp
        return dec[..., None] * S + U, S

    S0 = jnp.zeros((B, H, DK, DV), q.dtype)
    _, S_in = lax.scan(step, S0, (jnp.moveaxis(chunk_decay, 1, 0), jnp.moveaxis(chunk_state, 1, 0)))
    S_in = jnp.moveaxis(S_in, 0, 1)
    o_inter = jnp.einsum('bnthd,bnhdv->bnthv', q * jnp.exp(b), S_in)
    return (o_intra + o_inter).reshape(B, T, H, DV)


def hgrn2_mixer(p, lb_f, lb_b, norm_w):
    f32 = jnp.float32
    B, L, _ = p.shape
    q, f_fwd, f_bwd, i_in, g = [p[..., j * D_HGRN:(j + 1) * D_HGRN] for j in range(5)]

    def heads(a):
        return a.reshape(B, L, HGRN_HEADS, HGRN_HEAD_DIM)

    qh = heads(jax.nn.silu(q.astype(f32)))
    vh = heads(i_in.astype(f32))

    def forget(logit, lb):
        lb = lb.astype(f32)
        f = lb + (1.0 - lb) * jax.nn.sigmoid(logit.astype(f32))
        return heads(1.0 - f), heads(jnp.log(f))

    k_f, lf_f = forget(f_fwd, lb_f)
    k_b, lf_b = forget(f_bwd, lb_b)
    n_pad = (-N_META) % CHUNK

    def pad(a):
        return jnp.pad(a, ((0, 0), (n_pad, 0), (0, 0), (0, 0)))

    def flip(a):
        return a[:, ::-1]

    qp, vp = pad(qh), pad(vh)
    o_f = chunked_gated_linear_scan(qp, pad(k_f), vp, pad(lf_f))
    o_b = flip(chunked_gated_linear_scan(flip(qp), flip(pad(k_b)), flip(vp), flip(pad(lf_b))))
    o = (o_f + o_b)[:, n_pad:]
    o = o * lax.rsqrt(jnp.mean(o * o, axis=-1, keepdims=True) + EPS)
    o = o.reshape(B, L, D_HGRN) * norm_w.astype(f32) * jax.nn.silu(g.astype(f32))
    return o.astype(p.dtype)


def hier_moe(h, w_rg, w_re, w_gate, w_up, w_down):
    f32 = jnp.float32
    B, L, D = h.shape
    n_tok = B * L
    hf = h.reshape(n_tok, D)
    g_logits = (hf @ w_rg).astype(f32)
    g_sel = jnp.argmax(g_logits, axis=-1)
    p_group = jnp.take_along_axis(jax.nn.softmax(g_logits, axis=-1), g_sel[:, None], axis=-1)
    e_logits = jnp.einsum('nd,dge->nge', hf, w_re).astype(f32)
    e_logits = e_logits[jnp.arange(n_tok), g_sel]
    p_top, e_top = lax.top_k(jax.nn.softmax(e_logits, axis=-1), TOP_K)
    gate = p_group * p_top / jnp.sum(p_top, axis=-1, keepdims=True)
    expert_id = (g_sel[:, None] * EXPERTS_PER_GROUP + e_top).reshape(-1).astype(jnp.int32)

    M = n_tok * TOP_K
    n_blocks = -(-M // MOE_BLOCK) + N_EXPERTS
    n_slots = n_blocks * MOE_BLOCK
    order = jnp.argsort(expert_id)
    eid_s = expert_id[order]
    tok_s = (jnp.arange(M, dtype=jnp.int32) // TOP_K)[order]
    gate_s = gate.reshape(-1)[order]
    counts = jnp.bincount(expert_id, length=N_EXPERTS)
    padded = (counts + MOE_BLOCK - 1) // MOE_BLOCK * MOE_BLOCK
    start = jnp.cumsum(counts) - counts
    pend = jnp.cumsum(padded)
    dest = (pend - padded)[eid_s] + jnp.arange(M, dtype=jnp.int32) - start[eid_s]
    tok_buf = jnp.zeros((n_slots,), jnp.int32).at[dest].set(tok_s)
    gate_buf = jnp.zeros((n_slots,), f32).at[dest].set(gate_s)
    block_eid = jnp.minimum(
        jnp.searchsorted(pend, jnp.arange(n_blocks, dtype=pend.dtype) * MOE_BLOCK, side='right'),
        N_EXPERTS - 1)
    xb = hf[tok_buf].reshape(n_blocks, MOE_BLOCK, D)

    def expert_block(args):
        xe, e = args
        return (jax.nn.silu(xe @ w_gate[e]) * (xe @ w_up[e])) @ w_down[e]

    yb = lax.map(expert_block, (xb, block_eid)).reshape(n_slots, D)
    y = jnp.zeros_like(hf).at[tok_buf].add(yb * gate_buf[:, None].astype(yb.dtype))
    return y.reshape(B, L, D)


def setup_inputs(seed: int = 0) -> dict:
    key = jax.random.key(seed)
    ks = jax.random.split(key, 25)

    def nrm(k, shape, scale):
        return scale * jax.random.normal(k, shape, jnp.float32)

    return {
        'x': nrm(ks[0], (BATCH, SEQ, D_MODEL), 1.0),
        'meta_tokens': nrm(ks[1], (N_META, D_MODEL), 1.0),
        'w_in': nrm(ks[2], (DEPTH, D_MODEL, D_IN_PROJ), D_MODEL ** -0.5),
        'conv_w': nrm(ks[3], (DEPTH, SHORT_CONV, D_HYENA_PROJ), SHORT_CONV ** -0.5),
        'conv_b': nrm(ks[4], (DEPTH, D_HYENA_PROJ), 0.01),
        'filt_w1': nrm(ks[5], (DEPTH, FILTER_EMB, FILTER_HIDDEN), FILTER_EMB ** -0.5),
        'filt_b1': nrm(ks[6], (DEPTH, FILTER_HIDDEN), 0.1),
        'filt_w2': nrm(ks[7], (DEPTH, FILTER_HIDDEN, FILTER_HIDDEN), FILTER_HIDDEN ** -0.5),
        'filt_b2': nrm(ks[8], (DEPTH, FILTER_HIDDEN), 0.1),
        'filt_w3': nrm(ks[9], (DEPTH, FILTER_HIDDEN, 2 * HYENA_ORDER * D_HYENA), 0.02),
        'filt_freq': 1.0 + nrm(ks[10], (DEPTH, FILTER_HIDDEN), 0.01),
        'filt_skip': nrm(ks[11], (DEPTH, HYENA_ORDER, D_HYENA), 1.0),
        'hyena_norm': 1.0 + nrm(ks[12], (DEPTH, D_HYENA), 0.01),
        'lb_fwd': nrm(ks[13], (DEPTH + 1, D_HGRN), 0.1),
        'lb_bwd': nrm(ks[14], (DEPTH + 1, D_HGRN), 0.1),
        'hgrn_norm': 1.0 + nrm(ks[15], (DEPTH, D_HGRN), 0.01),
        'w_out': nrm(ks[16], (DEPTH, D_MIX, D_MODEL), D_MIX ** -0.5),
        'norm_mix': 1.0 + nrm(ks[17], (DEPTH, D_MODEL), 0.01),
        'norm_ffn': 1.0 + nrm(ks[18], (DEPTH, D_MODEL), 0.01),
        'w_router_group': nrm(ks[19], (DEPTH, D_MODEL, N_GROUPS), D_MODEL ** -0.5),
        'w_router_expert': nrm(ks[20], (DEPTH, D_MODEL, N_GROUPS, EXPERTS_PER_GROUP), D_MODEL ** -0.5),
        'w_gate': nrm(ks[21], (DEPTH, N_EXPERTS, D_MODEL, D_EXPERT), D_MODEL ** -0.5),
        'w_up': nrm(ks[22], (DEPTH, N_EXPERTS, D_MODEL, D_EXPERT), D_MODEL ** -0.5),
        'w_down': nrm(ks[23], (DEPTH, N_EXPERTS, D_EXPERT, D_MODEL), D_EXPERT ** -0.5),
        'norm_final': 1.0 + nrm(ks[24], (D_MODEL,), 0.01),
    }


def reference(x, meta_tokens, w_in, conv_w, conv_b, filt_w1, filt_b1, filt_w2, filt_b2, filt_w3,
              filt_freq, filt_skip, hyena_norm, lb_fwd, lb_bwd, hgrn_norm, w_out, norm_mix, norm_ffn,
              w_router_group, w_router_expert, w_gate, w_up, w_down, norm_final):
    B = x.shape[0]
    meta = jnp.broadcast_to(meta_tokens[None].astype(x.dtype), (B, N_META, x.shape[-1]))
    h = jnp.concatenate([meta, x], axis=1)
    lbf_all = jnp.cumsum(jax.nn.softmax(lb_fwd.astype(jnp.float32), axis=0), axis=0)
    lbb_all = jnp.cumsum(jax.nn.softmax(lb_bwd.astype(jnp.float32), axis=0), axis=0)
    for l in range(DEPTH):
        a = rms_norm(h, norm_mix[l])
        p = a @ w_in[l]
        y_hy = hyena_mixer(p[..., :D_HYENA_PROJ], conv_w[l], conv_b[l], filt_w1[l], filt_b1[l],
                           filt_w2[l], filt_b2[l], filt_w3[l], filt_freq[l], filt_skip[l])
        y_hy = rms_norm(y_hy, hyena_norm[l])
        y_hg = hgrn2_mixer(p[..., D_HYENA_PROJ:], lbf_all[l], lbb_all[l], hgrn_norm[l])
        h = h + jnp.concatenate([y_hy, y_hg], axis=-1) @ w_out[l]
        h = h + hier_moe(rms_norm(h, norm_ffn[l]), w_router_group[l], w_router_expert[l],
                         w_gate[l], w_up[l], w_down[l])
    h = rms_norm(h, norm_final)
    return h[:, N_META:]
```

```python
import math
import numpy as np
import ml_dtypes
import concourse.bass as bass
import concourse.mybir as mybir
from concourse.bass_utils import run_bass_kernel_spmd

F32 = mybir.dt.float32
BF16 = mybir.dt.bfloat16
I32 = mybir.dt.int32
AF = mybir.ActivationFunctionType
OP = mybir.AluOpType
AX = mybir.AxisListType

ENGS = ("pe", "act", "dve", "pool", "sp")


import types


def _freeze(fn):
    if fn.__closure__ is None:
        return fn
    cells = []
    for c in fn.__closure__:
        try:
            cells.append(types.CellType(c.cell_contents))
        except ValueError:
            cells.append(c)
    return types.FunctionType(fn.__code__, fn.__globals__, fn.__name__, fn.__defaults__, tuple(cells))


class Tk:
    __slots__ = ("w", "r", "name")

    def __init__(self, name=""):
        self.w = None
        self.r = []
        self.name = name


class Sched:
    def __init__(self, nc, n_dma_sems=24):
        self.nc = nc
        self.ops = {e: [] for e in ENGS}
        self.count = {e: 0 for e in ENGS}
        self.known = {e: {} for e in ENGS}
        self.n_hw = n_dma_sems
        self.n_grp1 = 12
        self.n_dma = n_dma_sems + self.n_grp1
        self.dma_cnt = [0] * self.n_dma
        self.dma_tok = [Tk("dmasem%d" % i) for i in range(self.n_dma)]
        self.dma_rr = 0
        self.rr1 = 0
        self.out_events = []

    def _need(self, eng, ev, waits):
        if ev is None:
            return
        key, val, clock = ev
        kn = self.known[eng]
        if key == eng and eng == "pe":
            return
        if kn.get(key, 0) >= val:
            return
        waits[key] = max(waits.get(key, 0), val)
        for k, v in clock.items():
            if kn.get(k, 0) < v:
                kn[k] = v
        if kn.get(key, 0) < val:
            kn[key] = val

    def op(self, eng, fn, r=(), w=(), dma=False, silent=False, grp=0):
        fn = _freeze(fn)
        silent = bool(silent) and eng == "pe" and not dma
        waits = {}
        slot = None
        toks_w = list(w)
        if dma:
            if grp == 1:
                slot = self.n_hw + self.rr1
                self.rr1 = (self.rr1 + 1) % self.n_grp1
            else:
                slot = self.dma_rr
                self.dma_rr = (self.dma_rr + 1) % self.n_hw
            toks_w.append(self.dma_tok[slot])
        for t in r:
            self._need(eng, t.w, waits)
        for t in toks_w:
            self._need(eng, t.w, waits)
            for ev in t.r:
                self._need(eng, ev, waits)
        if dma:
            self.dma_cnt[slot] += 16
            key, val = ("d", slot), self.dma_cnt[slot]
        elif silent:
            key, val = eng, self.count[eng] + 1
        else:
            self.count[eng] += 1
            key, val = eng, self.count[eng]
        clock = dict(self.known[eng])
        ev = (key, val, clock)
        if not dma and eng == "pe" and not silent:
            self.known[eng][eng] = val
        for t in r:
            t.r.append(ev)
        for t in toks_w:
            t.w = ev
            t.r = []
        self.ops[eng].append((sorted(waits.items(), key=str), fn, key, 16 if dma else (0 if silent else 1)))
        return ev

    def barrier(self):
        evs = []
        for e in ENGS:
            if self.count[e]:
                evs.append((e, self.count[e], {}))
        for s in range(self.n_dma):
            if self.dma_cnt[s]:
                evs.append((("d", s), self.dma_cnt[s], {}))
        for e in ENGS:
            waits = {}
            for ev in evs:
                if ev[0] != e:
                    self._need(e, ev, waits)
            if waits:
                self.ops[e].append((sorted(waits.items(), key=str), None, None, 0))

    def emit(self):
        nc = self.nc
        import contextlib
        with contextlib.ExitStack() as st:
            sems = {}
            for e in ENGS:
                sems[e] = st.enter_context(nc.semaphore("sem_" + e))
            for s in range(self.n_dma):
                sems[("d", s)] = st.enter_context(nc.semaphore("sem_d%d" % s))
            fin = {}
            for e in ENGS:
                if e != "sp" and self.count[e]:
                    fin[e] = self.count[e]
            for s in range(self.n_dma):
                if self.dma_cnt[s]:
                    fin[("d", s)] = self.dma_cnt[s]
            self.ops["sp"].append((sorted(fin.items(), key=str), None, None, 0))
            block = st.enter_context(nc.Block())

            def run(engname):
                def body(eng):
                    for waits, fn, key, inc in self.ops[engname]:
                        for k, v in waits:
                            eng.wait_ge(sems[k], v)
                        if fn is not None:
                            if inc:
                                fn(eng).then_inc(sems[key], inc)
                            else:
                                fn(eng)
                return body

            block.tensor(run("pe"))
            block.scalar(run("act"))
            block.vector(run("dve"))
            block.gpsimd(run("pool"))
            block.sync(run("sp"))


class Arena:
    def __init__(self, ap, nwords):
        self.ap = ap
        self.n = nwords
        self.top = 0

    def mark(self):
        return self.top

    def release(self, m):
        self.top = m

    def alloc(self, shape, dtype=F32):
        n = int(np.prod(shape))
        words = n if dtype in (F32, I32) else (n + 1) // 2
        words = (words + 7) // 8 * 8
        assert self.top + words <= self.n, "SBUF arena overflow %d + %d > %d" % (self.top, words, self.n)
        v = self.ap[:, self.top:self.top + words]
        self.top += words
        if dtype != F32:
            v = v.bitcast(dtype)
        v = v[:, 0:n]
        if len(shape) == 2:
            v = v.rearrange("p (a b) -> p a b", b=shape[1])
        elif len(shape) == 3:
            v = v.rearrange("p (a b c) -> p a b c", b=shape[1], c=shape[2])
        return v


N_META = 16
D = 1024
DH = 512
EPS = 1e-6
NWORDS = 52992


def host_consts(L, NT):
    T = NT * 128
    N = 2 * T
    t = np.arange(T, dtype=np.float64)
    f = np.arange(T, dtype=np.float64) + 0.5
    ang = 2.0 * np.pi / N * np.outer(t, f)
    valid = (t < L)[:, None]
    Cc = np.where(valid, np.cos(ang), 0.0)
    Cs = np.where(valid, np.sin(ang), 0.0)

    def lay(M):
        return np.ascontiguousarray(M.reshape(NT, 128, NT, 128).transpose(2, 1, 0, 3)).astype(ml_dtypes.bfloat16)

    c = {}
    c["dft_cc"] = lay(Cc)
    c["dft_cs"] = lay(Cs)
    c["dft_cct"] = lay(Cc.T.copy())
    c["dft_cst"] = lay(Cs.T.copy())
    pos = np.arange(L, dtype=np.float32)
    tt = pos / max(L - 1, 1)
    bands = np.linspace(1e-4, 15, 16, dtype=np.float32)
    a2 = (2.0 * math.pi / L) * pos[:, None] * bands[None, :]
    z = np.concatenate([tt[:, None], np.cos(a2), -np.sin(a2)], axis=-1).astype(np.float32)
    zT = np.zeros((33, T), np.float32)
    zT[:, :L] = z.T
    c["zfeat"] = zT
    deltas = np.abs(np.linspace(math.log(1e-2) / 1.5, math.log(1e-2) / 0.3, DH, dtype=np.float32))
    win = np.zeros((T, DH), np.float32)
    win[:L] = np.exp(-tt[:, None] * deltas[None, :])
    winb = win.copy()
    winb[0] = 0.0
    c["win_f"] = np.ascontiguousarray(win.reshape(NT, 128, DH).transpose(1, 0, 2))
    c["win_b"] = np.ascontiguousarray(winb.reshape(NT, 128, DH).transpose(1, 0, 2))
    i = np.arange(128)
    misc = np.zeros((128, 1024), np.float32)
    misc[:, 0:128] = np.eye(128)
    misc[:, 128:256] = (i[None, :] >= i[:, None])
    misc[:, 256:384] = (i[None, :] <= i[:, None])
    misc[:, 384:512] = (i[:, None] < i[None, :])
    misc[:, 512:640] = 1.0
    misc[:, 640:704] = np.arange(64)[None, :]
    misc[:, 704:712] = np.arange(8)[None, :]
    misc[:, 712] = i
    misc[:, 720:720 + NT] = 1.0
    misc[L - (NT - 1) * 128:, 720 + NT - 1] = 0.0
    c["misc"] = misc
    return c


DEBUG = False


def build_program(SEQ, NG, CAP):
    L = SEQ + N_META
    NT = (L + 127) // 128
    T = NT * 128
    LAST = L - (NT - 1) * 128
    NE = NG * 8
    NSLOT = NE * CAP
    NTT = 2 * NT
    NDFT = 2 * T
    nc = bass.Bass("TRN2", target_bir_lowering=False)

    def din(name, shape, dt=F32):
        return nc.dram_tensor(name, list(shape), dt, kind="ExternalInput").ap()

    x_d = din("x", [2, SEQ, D])
    meta_d = din("meta", [N_META, D])
    w_in_d = din("w_in", [D, 4096])
    w_out_d = din("w_out", [D, D])
    wg_d = din("w_gate", [NE, D, 512])
    wu_d = din("w_up", [NE, D, 512])
    wd_d = din("w_down", [NE, 512, D])
    wr_d = din("w_r", [128, 8, 8 + NE])
    rows_d = din("rows", [128, 6144])
    convw_d = din("convw", [128, 12, 4])
    lb_d = din("lb", [128, 2, 2, 4])
    f1_d = din("f_w1", [33, 64])
    f2_d = din("f_w2", [64, 64])
    f3_d = din("f_w3", [64, 2048])
    fb_d = din("f_b", [64, 4])
    misc_d = din("misc", [128, 1024])
    zfeat_d = din("zfeat", [33, T])
    winf_d = din("win_f", [128, NT, DH])
    winb_d = din("win_b", [128, NT, DH])
    cc_d = din("dft_cc", [NT, 128, NT, 128], BF16)
    cs_d = din("dft_cs", [NT, 128, NT, 128], BF16)
    cct_d = din("dft_cct", [NT, 128, NT, 128], BF16)
    cst_d = din("dft_cst", [NT, 128, NT, 128], BF16)
    out_d = nc.dram_tensor("out", [2, SEQ, D], F32, kind="ExternalOutput").ap()
    dk = dict(kind="ExternalOutput") if DEBUG else {}
    kspec_d = nc.dram_tensor("kspec", [2, 2, T, DH], F32, **dk).ap()
    h1_d = nc.dram_tensor("h1s", [2 * T, D], F32, **dk).ap()
    xs_d = nc.dram_tensor("xsort", [NSLOT, D], BF16, **dk).ap()
    y_d = nc.dram_tensor("ysort", [NSLOT, D], F32, **dk).ap()
    wgb_d = nc.dram_tensor("wg_bf", [NE, 128, 8, 512], BF16).ap()
    wub_d = nc.dram_tensor("wu_bf", [NE, 128, 8, 512], BF16).ap()
    wdb_d = nc.dram_tensor("wd_bf", [NE, 128, 4, D], BF16).ap()
    if DEBUG:
        dbg_mix = nc.dram_tensor("dbg_mix", [2, 128, 8, T], BF16, kind="ExternalOutput").ap()
        dbg_zx = nc.dram_tensor("dbg_zx", [2, 3, 128, NT, DH], BF16, kind="ExternalOutput").ap()
        dbg_aT = nc.dram_tensor("dbg_aT", [2, 128, 8, T], BF16, kind="ExternalOutput").ap()
        dbg_sg = nc.dram_tensor("dbg_sg", [128, NTT, 4], F32, kind="ExternalOutput").ap()

    import contextlib
    st = contextlib.ExitStack()
    with st:
        big = st.enter_context(nc.sbuf_tensor("arena", [128, NWORDS], F32))
        psb = [st.enter_context(nc.psum_tensor("ps%d" % i, [128, 512], F32)) for i in range(8)]
        S = Sched(nc)
        o0 = 0
        AC = Arena(big[:, o0:o0 + 8320], 8320); o0 += 8320
        A1 = Arena(big[:, o0:o0 + 8704], 8704); o0 += 8704
        A2 = Arena(big[:, o0:o0 + 8960], 8960); o0 += 8960
        AR = Arena(big[:, o0:NWORDS], NWORDS - o0)

        ps_tok = [Tk("ps%d" % i) for i in range(8)]
        ps_rr = [0]

        def PS(dtype=F32):
            i = ps_rr[0]
            ps_rr[0] = (i + 1) % 8
            ap = psb[i][:]
            if dtype != F32:
                ap = ap.bitcast(dtype)
            return ap, ps_tok[i]

        def new_phase():
            S.barrier()
            for a in (A1, A2, AR):
                a.release(0)

        class B:
            def __init__(self, arena, shape, dtype=F32, name=""):
                self.ap = arena.alloc(shape, dtype)
                self.t = Tk(name)

        regc = {}

        def bcreg(e):
            if "r" not in regc:
                regc["r"] = e.to_reg(NSLOT - 1)
            return regc["r"]

        def dma(q, out, in_, r=(), w=(), grp=0):
            S.op(q, lambda e: e.dma_start(out=out, in_=in_), r=r, w=w, dma=True, grp=grp)

        conv_tok = [[Tk(), Tk(), Tk()] for _ in range(NE)]
        conv_jobs = []
        for ex_ in range(NE):
            conv_jobs.append((wgb_d[ex_], wg_d[ex_].rearrange("(dc p) c -> p dc c", p=128), conv_tok[ex_][0]))
            conv_jobs.append((wub_d[ex_], wu_d[ex_].rearrange("(dc p) c -> p dc c", p=128), conv_tok[ex_][1]))
            conv_jobs.append((wdb_d[ex_], wd_d[ex_].rearrange("(fc p) c -> p fc c", p=128), conv_tok[ex_][2]))
        conv_pos = [0]

        def conv_some(n):
            return
            while n > 0 and conv_pos[0] < len(conv_jobs):
                o_, i_, tk_c = conv_jobs[conv_pos[0]]
                conv_pos[0] += 1
                n -= 1
                dma("pool", o_, i_, w=[tk_c], grp=1)

        def rstd_from_ss(ss, rs, n, npart, tk):
            S.op("act", lambda e: e.activation(out=rs[:npart], in_=ss[:npart], func=AF.Sqrt, scale=1.0 / n, bias=epsb.ap[:npart, 0:1]), r=[tk, epsb.t], w=[tk])
            S.op("dve", lambda e: e.reciprocal(out=rs[:npart], in_=rs[:npart]), r=[tk], w=[tk])

        def sumsq(src_ap, junk_ap, ss_ap, r, jt, st):
            S.op("act", lambda e: e.activation(out=junk_ap, in_=src_ap, func=AF.Square), r=r, w=[jt])
            S.op("dve", lambda e: e.tensor_reduce(out=ss_ap, in_=junk_ap, axis=AX.X, op=OP.add), r=[jt], w=[st])

        misc = B(AC, [1024]); dma("sp", misc.ap, misc_d, w=[misc.t])
        ident = misc.ap[:, 0:128]
        rows = B(AC, [5120]); dma("sp", rows.ap, rows_d[:, 0:5120], w=[rows.t])
        g_mix, g_ffn, g_fin = rows.ap[:, 0:1024], rows.ap[:, 1024:2048], rows.ap[:, 2048:3072]
        g_hy, g_hg = rows.ap[:, 3072:3584], rows.ap[:, 3584:4096]
        skipB = [rows.ap[:, 4096:4608], rows.ap[:, 4608:5120]]
        convw = B(AC, [12, 4]); dma("sp", convw.ap, convw_d, w=[convw.t])
        lbr = B(AC, [2, 2, 4]); dma("sp", lbr.ap, lb_d, w=[lbr.t])
        wr = B(AC, [8, 8 + NE]); dma("sp", wr.ap, wr_d, w=[wr.t])
        identb = B(AC, [128], BF16)
        S.op("dve", lambda e: e.tensor_copy(out=identb.ap, in_=ident), r=[misc.t], w=[identb.t])
        maskb = B(AC, [2, 128], BF16)
        S.op("dve", lambda e: e.tensor_copy(out=maskb.ap, in_=misc.ap[:, 128:384].rearrange("p (a b) -> p a b", b=128)), r=[misc.t], w=[maskb.t])
        ustrict = misc.ap[:, 384:512]
        ones = misc.ap[:, 512:640]
        iota64 = misc.ap[:, 640:704]
        iota8 = misc.ap[:, 704:712]
        iotap = misc.ap[:, 712:713]
        epsb = B(AC, [1])
        onesT = B(AC, [512])
        S.op("pool", lambda e: e.memset(onesT.ap, 1.0), w=[onesT.t])
        S.op("pool", lambda e: e.memset(epsb.ap, EPS), w=[epsb.t])
        lb = B(AC, [2, 4]); oml = B(AC, [2, 4])
        S.op("dve", lambda e: e.tensor_tensor(out=lb.ap, in0=lbr.ap[:, :, 0, :], in1=lbr.ap[:, :, 1, :], op=OP.subtract), r=[lbr.t], w=[lb.t])
        S.op("act", lambda e: e.activation(out=lb.ap, in_=lb.ap, func=AF.Sigmoid), r=[lb.t], w=[lb.t])
        S.op("dve", lambda e: e.tensor_scalar(out=oml.ap, in0=lb.ap, scalar1=-1.0, scalar2=1.0, op0=OP.mult, op1=OP.add), r=[lb.t], w=[oml.t])
        slots = B(AC, [NTT, 2], I32)
        gates = B(AC, [NTT, 2])
        S.op("pool", lambda e: e.memset(slots.ap, 0), w=[slots.t])
        S.op("pool", lambda e: e.memset(gates.ap, 0.0), w=[gates.t])
        cnt = B(AC, [NE])
        S.op("pool", lambda e: e.memset(cnt.ap, 0.0), w=[cnt.t])
        slotbase = B(AC, [NE])
        S.op("dve", lambda e: e.tensor_scalar(out=slotbase.ap, in0=iota64[:, 0:NE], scalar1=float(CAP), scalar2=None, op0=OP.mult), r=[misc.t], w=[slotbase.t])
        zero_bf = B(AC, [1024], BF16)
        S.op("pool", lambda e: e.memset(zero_bf.ap, 0.0), w=[zero_bf.t])
        t_xs = Tk("xsort"); t_y = Tk("ysort"); t_h1 = Tk("h1s"); t_ks = Tk("kspec")
        for r0 in range(0, NSLOT, 128):
            dma("sp", xs_d[r0:r0 + 128, :], zero_bf.ap, r=[zero_bf.t], w=[t_xs])

        def wrap(buf, tmp, npart):
            for _ in range(2):
                S.op("dve", lambda e: e.tensor_scalar(out=tmp.ap[:npart], in0=buf.ap[:npart], scalar1=math.pi, scalar2=-2 * math.pi, op0=OP.is_gt, op1=OP.mult), r=[buf.t], w=[tmp.t])
                S.op("dve", lambda e: e.tensor_tensor(out=buf.ap[:npart], in0=buf.ap[:npart], in1=tmp.ap[:npart], op=OP.add), r=[tmp.t, buf.t], w=[buf.t])
                S.op("dve", lambda e: e.tensor_scalar(out=tmp.ap[:npart], in0=buf.ap[:npart], scalar1=-math.pi, scalar2=2 * math.pi, op0=OP.is_lt, op1=OP.mult), r=[buf.t], w=[tmp.t])
                S.op("dve", lambda e: e.tensor_tensor(out=buf.ap[:npart], in0=buf.ap[:npart], in1=tmp.ap[:npart], op=OP.add), r=[tmp.t, buf.t], w=[buf.t])

        zT = B(A2, [T]); dma("sp", zT.ap[:33], zfeat_d, w=[zT.t])
        fw1 = B(A1, [64]); dma("sp", fw1.ap[:33], f1_d, w=[fw1.t])
        fw2 = B(A1, [64]); dma("sp", fw2.ap[:64], f2_d, w=[fw2.t])
        fw3 = B(A2, [2048]); dma("sp", fw3.ap[:64], f3_d, w=[fw3.t])
        fbb = B(A1, [4]); dma("sp", fbb.ap[:64], fb_d, w=[fbb.t])
        hT = [B(A1, [T]), B(A1, [T])]
        tmpw = B(A1, [T])
        slabs = [(s0, min(512, T - s0)) for s0 in range(0, T, 512)]
        for layer in range(2):
            src = zT if layer == 0 else hT[0]
            kk = 33 if layer == 0 else 64
            wl = fw1 if layer == 0 else fw2
            dst = hT[layer]
            for (s0, sn) in slabs:
                pa, pt = PS()
                S.op("pe", lambda e, pa=pa, s0=s0, sn=sn: e.matmul(pa[:64, 0:sn], lhsT=wl.ap[:kk, 0:64], rhs=src.ap[:kk, s0:s0 + sn], start=True, stop=True), r=[wl.t, src.t], w=[pt])
                S.op("dve", lambda e, pa=pa, s0=s0, sn=sn: e.tensor_scalar(out=dst.ap[:64, s0:s0 + sn], in0=pa[:64, 0:sn], scalar1=fbb.ap[:64, layer:layer + 1], scalar2=fbb.ap[:64, 2:3], op0=OP.add, op1=OP.mult), r=[pt, fbb.t], w=[dst.t])
            wrap(dst, tmpw, 64)
            S.op("act", lambda e: e.activation(out=dst.ap[:64], in_=dst.ap[:64], func=AF.Sin), r=[dst.t], w=[dst.t])
        h2T = hT[1]
        hs = B(AR, [NT, 1024], BF16); hdd = B(AR, [NT, 1024], BF16)
        mk0 = AR.mark()
        wtile = [[B(AR, [DH]), B(AR, [DH])] for _ in range(2)]
        ftmp = [B(AR, [DH]) for _ in range(4)]
        for i in range(NT):
            wf, wb_ = wtile[i % 2]
            dma("sp", wf.ap, winf_d[:, i, :], w=[wf.t])
            dma("sp", wb_.ap, winb_d[:, i, :], w=[wb_.t])
            for o in range(2):
                pf, ptf = PS(); pb, ptb = PS()
                S.op("pe", lambda e, pf=pf: e.matmul(pf, lhsT=h2T.ap[:64, i * 128:(i + 1) * 128], rhs=fw3.ap[:64, o * 512:(o + 1) * 512], start=True, stop=True), r=[h2T.t, fw3.t], w=[ptf])
                S.op("pe", lambda e, pb=pb: e.matmul(pb, lhsT=h2T.ap[:64, i * 128:(i + 1) * 128], rhs=fw3.ap[:64, 1024 + o * 512:1024 + (o + 1) * 512], start=True, stop=True), r=[h2T.t, fw3.t], w=[ptb])
                ta, tb = ftmp[2 * o], ftmp[2 * o + 1]
                S.op("dve", lambda e, pf=pf, ta=ta: e.tensor_tensor(out=ta.ap, in0=pf, in1=wf.ap, op=OP.mult), r=[ptf, wf.t], w=[ta.t])
                S.op("dve", lambda e, pb=pb, tb=tb: e.tensor_tensor(out=tb.ap, in0=pb, in1=wb_.ap, op=OP.mult), r=[ptb, wb_.t], w=[tb.t])
                S.op("pool", lambda e, ta=ta, tb=tb: e.tensor_tensor(out=hs.ap[:, i, o * 512:(o + 1) * 512], in0=ta.ap, in1=tb.ap, op=OP.add), r=[ta.t, tb.t], w=[hs.t])
                S.op("pool", lambda e, ta=ta, tb=tb: e.tensor_tensor(out=hdd.ap[:, i, o * 512:(o + 1) * 512], in0=ta.ap, in1=tb.ap, op=OP.subtract), r=[ta.t, tb.t], w=[hdd.t])
        S.barrier()
        AR.release(mk0)
        dftb = [[B(AR, [NT, 128], BF16), B(AR, [NT, 128], BF16)] for _ in range(2)]
        kout = [B(AR, [DH]) for _ in range(4)]
        dma("sp", dftb[0][0].ap, cc_d[0], w=[dftb[0][0].t])
        dma("sp", dftb[0][1].ap, cs_d[0], w=[dftb[0][1].t])
        for j in range(NT):
            cb, sb_ = dftb[j % 2]
            if j + 1 < NT:
                dma("sp", dftb[(j + 1) % 2][0].ap, cc_d[j + 1], w=[dftb[(j + 1) % 2][0].t])
                dma("sp", dftb[(j + 1) % 2][1].ap, cs_d[j + 1], w=[dftb[(j + 1) % 2][1].t])
            for o in range(2):
                for ri, (mat, src) in enumerate(((cb, hs), (sb_, hdd))):
                    pa, pt = PS()
                    for tc_ in range(NT):
                        S.op("pe", lambda e, pa=pa, mat=mat, src=src, tc_=tc_: e.matmul(pa, lhsT=mat.ap[:, tc_, :], rhs=src.ap[:, tc_, o * 512:(o + 1) * 512], start=(tc_ == 0), stop=(tc_ == NT - 1)), r=[mat.t, src.t], w=[pt], silent=not (tc_ == NT - 1))
                    ko = kout[2 * o + ri]
                    S.op("act", lambda e, pa=pa, ko=ko: e.activation(out=ko.ap, in_=pa, func=AF.Copy, scale=2.0 / NDFT), r=[pt], w=[ko.t])
                    dma("sp", kspec_d[o, ri, j * 128:(j + 1) * 128, :], ko.ap, r=[ko.t], w=[t_ks])

        def load_h_tile(q, dst, s, i, npart):
            if i == 0:
                dma(q, dst.ap[0:N_META], meta_d, w=[dst.t])
                dma(q, dst.ap[N_META:128], x_d[s, 0:128 - N_META, :], w=[dst.t])
            else:
                r0 = i * 128 - N_META
                dma(q, dst.ap[:npart], x_d[s, r0:r0 + npart, :], w=[dst.t])

        def wload(arena_buf, c0, ncols):
            dma("pool", arena_buf.ap[:, :, 0:ncols], w_in_d.rearrange("(dc p) c -> p dc c", p=128)[:, :, c0:c0 + ncols], w=[arena_buf.t])

        for s in range(2):
            new_phase()
            mixT = B(A1, [8, T], BF16)
            aT = B(A2, [8, T], BF16)
            if LAST < 128:
                S.op("pool", lambda e: e.memset(aT.ap[:, :, (NT - 1) * 128 + LAST:T], 0.0), w=[aT.t])
            m0 = AR.mark()
            hin = [B(AR, [D]) for _ in range(2)]
            abf = [B(AR, [D], BF16) for _ in range(2)]
            junk = B(AR, [D])
            ssb = [B(AR, [2]) for _ in range(2)]
            for i in range(NT):
                npart = 128 if i < NT - 1 else LAST
                hi, ab, ss = hin[i % 2], abf[i % 2], ssb[i % 2]
                load_h_tile("sp", hi, s, i, npart)
                sumsq(hi.ap[:npart], junk.ap[:npart], ss.ap[:npart, 0:1], [hi.t], junk.t, ss.t)
                rstd_from_ss(ss.ap[:, 0:1], ss.ap[:, 1:2], D, npart, ss.t)
                S.op("dve", lambda e, hi=hi, ss=ss, ab=ab: e.scalar_tensor_tensor(out=ab.ap[:npart], in0=hi.ap[:npart], scalar=ss.ap[:npart, 1:2], in1=g_mix[:npart], op0=OP.mult, op1=OP.mult), r=[hi.t, ss.t, rows.t], w=[ab.t])
                pa, pt = PS(BF16)
                for dc in range(8):
                    S.op("pe", lambda e, pa=pa, ab=ab, dc=dc: e.transpose(out=pa[:, dc * 128:dc * 128 + npart], in_=ab.ap[:npart, dc * 128:(dc + 1) * 128], identity=identb.ap[:npart, :npart]), r=[ab.t, identb.t], w=[pt])
                S.op("act", lambda e, pa=pa: e.activation(out=aT.ap[:, :, i * 128:i * 128 + npart], in_=pa.rearrange("p (a b) -> p a b", b=128)[:, :, 0:npart], func=AF.Copy), r=[pt], w=[aT.t])
            AR.release(m0)

            S.barrier()
            m0 = AR.mark()
            wch = [B(AR, [8, 128], BF16) for _ in range(3)]
            qT = B(AR, [T], BF16); kTd = B(AR, [T], BF16)
            PTe = B(AR, [T + 128])
            lf = B(AR, [T]); sgt = B(AR, [T])
            V = B(AR, [NT, 128], BF16); sgate = B(AR, [NT, 128], BF16); oacc = B(AR, [NT, 128])
            cs3 = [[B(AR, [NT]) for _ in range(4)] for _ in range(2)]
            Sst = [B(AR, [128]), B(AR, [128])]
            KtT1 = B(AR, [T], BF16)
            QtA = [B(AR, [T], BF16) for _ in range(2)]
            sTA = [B(AR, [NT, 128], BF16) for _ in range(2)]
            KtA = [B(AR, [NT, 128], BF16) for _ in range(2)]
            Spb = [B(AR, [128], BF16) for _ in range(2)]
            tmpb = [B(AR, [128]) for _ in range(2)]
            fins = [dict(junk=B(AR, [128]), ss=B(AR, [2]), yb=B(AR, [128], BF16)) for _ in range(2)]
            QhA = [B(AR, [T], BF16) for _ in range(2)]
            onesT2 = B(AR, [T], BF16)
            S.op("pool", lambda e: e.memset(onesT2.ap, 1.0), w=[onesT2.t])
            lf3 = lf.ap.rearrange("p (c k) -> p c k", k=128)
            sg3 = sgt.ap.rearrange("p (c k) -> p c k", k=128)
            for hd in range(4):
                cq = 1536 + hd * 128
                for j, wb in enumerate(wch):
                    wload(wb, cq + j * 512, 128)
                for j in range(3):
                    d = j - 1
                    for (s0, sn) in slabs:
                        pa, pt = PS()
                        for dc in range(8):
                            S.op("pe", lambda e, pa=pa, dc=dc: e.matmul(pa[:, 0:sn], lhsT=wch[j].ap[:, dc, :], rhs=aT.ap[:, dc, s0:s0 + sn], start=(dc == 0), stop=(dc == 7)), r=[wch[j].t, aT.t], w=[pt], silent=not (dc == 7))
                        if j == 0:
                            S.op("act", lambda e, pa=pa: e.activation(out=qT.ap[:, s0:s0 + sn], in_=pa[:, 0:sn], func=AF.Silu), r=[pt], w=[qT.t])
                        else:
                            S.op("act", lambda e, pa=pa: e.activation(out=sgt.ap[:, s0:s0 + sn], in_=pa[:, 0:sn], func=AF.Sigmoid), r=[pt], w=[sgt.t])
                    if j > 0:
                        S.op("dve", lambda e: e.tensor_scalar(out=sgt.ap, in0=sgt.ap, scalar1=oml.ap[:, d, hd:hd + 1], scalar2=lb.ap[:, d, hd:hd + 1], op0=OP.mult, op1=OP.add), r=[sgt.t, oml.t, lb.t], w=[sgt.t])
                        S.op("pool", lambda e: e.tensor_scalar(out=kTd.ap, in0=sgt.ap, scalar1=-1.0, scalar2=1.0, op0=OP.mult, op1=OP.add), r=[sgt.t], w=[kTd.t])
                        S.op("act", lambda e: e.activation(out=lf.ap, in_=sgt.ap, func=AF.Ln), r=[sgt.t], w=[lf.t])
                        S.op("pool", lambda e: e.memset(PTe.ap[:, 0:1], 0.0), w=[PTe.t])
                        S.op("dve", lambda e: e.tensor_tensor_scan(out=PTe.ap[:, 1:1 + T], data0=onesT2.ap, data1=lf.ap, initial=0.0, op0=OP.mult, op1=OP.add), r=[lf.t, onesT2.t, PTe.t], w=[PTe.t])
                    if j == 0:
                        continue
                    P3 = PTe.ap[:, 0:T].rearrange("p (c k) -> p c k", k=128)
                    Z3 = PTe.ap[:, 128:T + 128].rearrange("p (c k) -> p c k", k=128)
                    Acol, Zcol = P3[:, :, 0], Z3[:, :, 0]
                    Mcol = P3[:, :, 65] if d == 0 else P3[:, :, 64]
                    M, c1, c2, dec = cs3[d]
                    S.op("dve", lambda e: e.tensor_copy(out=M.ap, in_=Mcol), r=[PTe.t], w=[M.t])
                    e1, e2 = (c1, c2) if d == 0 else (c2, c1)
                    S.op("dve", lambda e: e.tensor_tensor(out=e1.ap, in0=M.ap, in1=Acol, op=OP.subtract), r=[M.t, PTe.t], w=[e1.t])
                    S.op("dve", lambda e: e.tensor_tensor(out=e2.ap, in0=Zcol, in1=M.ap, op=OP.subtract), r=[M.t, PTe.t], w=[e2.t])
                    S.op("dve", lambda e: e.tensor_tensor(out=dec.ap, in0=Zcol, in1=Acol, op=OP.subtract), r=[PTe.t], w=[dec.t])
                    for bb in (c1, c2, dec):
                        S.op("act", lambda e, bb=bb: e.activation(out=bb.ap, in_=bb.ap, func=AF.Exp), r=[bb.t], w=[bb.t])
                    off = 1 if d == 0 else 0
                    Pv = PTe.ap[:, off:off + T].rearrange("p (c k) -> p c k", k=128)
                    S.op("dve", lambda e: e.tensor_tensor(out=lf3, in0=Pv, in1=M.ap.unsqueeze(2).to_broadcast([128, NT, 128]), op=OP.subtract), r=[PTe.t, M.t, lf.t], w=[lf.t])
                    sq = 1.0 if d == 0 else -1.0
                    S.op("act", lambda e: e.activation(out=sgt.ap, in_=lf.ap, func=AF.Exp, scale=sq), r=[lf.t, sgt.t], w=[sgt.t])
                    S.op("dve", lambda e: e.tensor_tensor(out=QtA[d].ap, in0=qT.ap, in1=sgt.ap, op=OP.mult), r=[qT.t, sgt.t], w=[QtA[d].t])
                    S.op("act", lambda e: e.activation(out=lf.ap, in_=lf.ap, func=AF.Exp, scale=-sq), r=[lf.t], w=[lf.t])
                    S.op("pool", lambda e: e.tensor_tensor(out=KtT1.ap, in0=kTd.ap, in1=lf.ap, op=OP.mult), r=[kTd.t, lf.t], w=[KtT1.t])
                    for c0 in range(0, NT, 4):
                        n4 = min(4, NT - c0)
                        p1, t1 = PS()
                        for k in range(n4):
                            cs_ = slice((c0 + k) * 128, (c0 + k + 1) * 128)
                            S.op("pe", lambda e, p1=p1, k=k, cs_=cs_: e.matmul(p1[:, k * 128:(k + 1) * 128], lhsT=KtT1.ap[:, cs_], rhs=QtA[d].ap[:, cs_], start=True, stop=True), r=[KtT1.t, QtA[d].t], w=[t1])
                        S.op("dve", lambda e, p1=p1: e.tensor_tensor(out=sTA[d].ap[:, c0:c0 + n4, :], in0=p1[:, 0:n4 * 128].rearrange("p (a b) -> p a b", b=128), in1=maskb.ap[:, d:d + 1, :].to_broadcast([128, n4, 128]), op=OP.mult), r=[t1, maskb.t], w=[sTA[d].t])
                    S.op("pool", lambda e: e.tensor_tensor(out=QhA[d].ap.rearrange("p (c k) -> p c k", k=128), in0=QtA[d].ap.rearrange("p (c k) -> p c k", k=128), in1=c1.ap.unsqueeze(2).to_broadcast([128, NT, 128]), op=OP.mult), r=[QtA[d].t, c1.t], w=[QhA[d].t])
                    S.op("pool", lambda e: e.tensor_tensor(out=KtT1.ap.rearrange("p (c k) -> p c k", k=128), in0=KtT1.ap.rearrange("p (c k) -> p c k", k=128), in1=c2.ap.unsqueeze(2).to_broadcast([128, NT, 128]), op=OP.mult), r=[KtT1.t, c2.t], w=[KtT1.t])
                    for c0 in range(0, NT, 8):
                        n8 = min(8, NT - c0)
                        p2, t2 = PS(BF16)
                        for k in range(n8):
                            cs_ = slice((c0 + k) * 128, (c0 + k + 1) * 128)
                            S.op("pe", lambda e, p2=p2, k=k, cs_=cs_: e.transpose(out=p2[:, k * 128:(k + 1) * 128], in_=KtT1.ap[:, cs_], identity=identb.ap), r=[KtT1.t, identb.t], w=[t2])
                        S.op("act", lambda e, p2=p2: e.activation(out=KtA[d].ap[:, c0:c0 + n8, :], in_=p2[:, 0:n8 * 128].rearrange("p (a b) -> p a b", b=128), func=AF.Copy), r=[t2], w=[KtA[d].t])
                wload(wch[0], cq + 3 * 512, 128)
                wload(wch[1], cq + 4 * 512, 128)
                for i0 in range(0, NT, 4):
                    n4 = min(4, NT - i0)
                    for j in range(2):
                        pa, pt = PS()
                        for k in range(n4):
                            i = i0 + k
                            for dc in range(8):
                                S.op("pe", lambda e, pa=pa, k=k, i=i, dc=dc: e.matmul(pa[:, k * 128:(k + 1) * 128], lhsT=aT.ap[:, dc, i * 128:(i + 1) * 128], rhs=wch[j].ap[:, dc, :], start=(dc == 0), stop=(dc == 7)), r=[wch[j].t, aT.t], w=[pt], silent=not (dc == 7))
                        pv = pa[:, 0:n4 * 128].rearrange("p (a b) -> p a b", b=128)
                        if j == 0:
                            S.op("act", lambda e, pv=pv: e.activation(out=V.ap[:, i0:i0 + n4, :], in_=pv, func=AF.Copy), r=[pt], w=[V.t])
                        else:
                            S.op("act", lambda e, pv=pv: e.activation(out=sgate.ap[:, i0:i0 + n4, :], in_=pv, func=AF.Silu), r=[pt], w=[sgate.t])
                S.op("pool", lambda e: e.tensor_tensor(out=sgate.ap, in0=sgate.ap, in1=g_hg[:, hd * 128:(hd + 1) * 128].unsqueeze(1).to_broadcast([128, NT, 128]), op=OP.mult), r=[sgate.t, rows.t], w=[sgate.t])
                S.op("pool", lambda e: e.memset(oacc.ap, 0.0), w=[oacc.t])
                for d in range(2):
                    S.op("pool", lambda e, d=d: e.memset(Sst[d].ap, 0.0), w=[Sst[d].t])
                for step in range(NT):
                    for d in range(2):
                        c = step if d == 0 else NT - 1 - step
                        M, c1, c2, dec = cs3[d]
                        cs_ = slice(128 * c, 128 * c + 128)
                        S.op("act", lambda e: e.activation(out=Spb[d].ap, in_=Sst[d].ap, func=AF.Copy), r=[Sst[d].t], w=[Spb[d].t])
                        p4, t4 = PS()
                        S.op("pe", lambda e, p4=p4: e.matmul(p4[:, 0:128], lhsT=KtA[d].ap[:, c, :], rhs=V.ap[:, c, :], start=True, stop=True), r=[KtA[d].t, V.t], w=[t4])
                        p3, t3 = PS()
                        S.op("pe", lambda e, p3=p3: e.matmul(p3[:, 0:128], lhsT=sTA[d].ap[:, c, :], rhs=V.ap[:, c, :], start=True, stop=False), r=[sTA[d].t, V.t], w=[t3], silent=not False)
                        S.op("pe", lambda e, p3=p3: e.matmul(p3[:, 0:128], lhsT=QhA[d].ap[:, cs_], rhs=Spb[d].ap, start=False, stop=True), r=[QhA[d].t, Spb[d].t], w=[t3])
                        S.op("dve", lambda e, p4=p4: e.scalar_tensor_tensor(out=Sst[d].ap, in0=Sst[d].ap, scalar=dec.ap[:, c:c + 1], in1=p4[:, 0:128], op0=OP.mult, op1=OP.add), r=[t4, dec.t, Sst[d].t], w=[Sst[d].t])
                        S.op("dve", lambda e, p3=p3: e.tensor_tensor(out=oacc.ap[:, c, :], in0=p3[:, 0:128], in1=oacc.ap[:, c, :], op=OP.add), r=[t3, oacc.t], w=[oacc.t])
                for i in range(NT):
                    fin = fins[i % 2]
                    sumsq(oacc.ap[:, i, :], fin["junk"].ap, fin["ss"].ap[:, 0:1], [oacc.t], fin["junk"].t, fin["ss"].t)
                    rstd_from_ss(fin["ss"].ap[:, 0:1], fin["ss"].ap[:, 1:2], 128, 128, fin["ss"].t)
                    S.op("dve", lambda e: e.scalar_tensor_tensor(out=fin["yb"].ap, in0=oacc.ap[:, i, :], scalar=fin["ss"].ap[:, 1:2], in1=sgate.ap[:, i, :], op0=OP.mult, op1=OP.mult), r=[oacc.t, fin["ss"].t, sgate.t], w=[fin["yb"].t])
                    pa, pt = PS(BF16)
                    S.op("pe", lambda e, pa=pa: e.transpose(out=pa[:, 0:128], in_=fin["yb"].ap, identity=identb.ap), r=[fin["yb"].t, identb.t], w=[pt])
                    S.op("act", lambda e, pa=pa: e.activation(out=mixT.ap[:, 4 + hd, i * 128:(i + 1) * 128], in_=pa[:, 0:128], func=AF.Copy), r=[pt], w=[mixT.t])
            AR.release(m0)

            S.barrier()
            m0 = AR.mark()
            zx = [B(AR, [NT, DH], BF16, "zx%d" % k) for k in range(3)]
            m1 = AR.mark()
            wch = [B(AR, [8, 128], BF16) for _ in range(2)]
            pbuf = [B(AR, [T + 8]) for _ in range(2)]
            ub = B(AR, [T]); uT = [B(AR, [T], BF16) for _ in range(2)]
            for pb_ in pbuf:
                S.op("pool", lambda e, pb_=pb_: e.memset(pb_.ap[:, 0:1], 0.0), w=[pb_.t])
                S.op("pool", lambda e, pb_=pb_: e.memset(pb_.ap[:, T + 1:T + 2], 0.0), w=[pb_.t])
            for cc in range(12):
                wb, pb_, ut = wch[cc % 2], pbuf[cc % 2], uT[cc % 2]
                wload(wb, cc * 128, 128)
                for (s0, sn) in slabs:
                    pa, pt = PS()
                    for dc in range(8):
                        S.op("pe", lambda e, pa=pa, dc=dc: e.matmul(pa[:, 0:sn], lhsT=wb.ap[:, dc, :], rhs=aT.ap[:, dc, s0:s0 + sn], start=(dc == 0), stop=(dc == 7)), r=[wb.t, aT.t], w=[pt], silent=not (dc == 7))
                    S.op("act", lambda e, pa=pa: e.activation(out=pb_.ap[:, 1 + s0:1 + s0 + sn], in_=pa[:, 0:sn], func=AF.Copy), r=[pt], w=[pb_.t])
                S.op("act", lambda e: e.activation(out=ub.ap, in_=pb_.ap[:, 1:T + 1], func=AF.Identity, scale=convw.ap[:, cc, 1:2], bias=convw.ap[:, cc, 3:4]), r=[pb_.t, convw.t], w=[ub.t])
                S.op("dve", lambda e: e.scalar_tensor_tensor(out=ub.ap, in0=pb_.ap[:, 0:T], scalar=convw.ap[:, cc, 0:1], in1=ub.ap, op0=OP.mult, op1=OP.add), r=[pb_.t, convw.t, ub.t], w=[ub.t])
                S.op("dve", lambda e: e.scalar_tensor_tensor(out=ut.ap, in0=pb_.ap[:, 2:T + 2], scalar=convw.ap[:, cc, 2:3], in1=ub.ap, op0=OP.mult, op1=OP.add), r=[pb_.t, convw.t, ub.t], w=[ut.t])
                dst = zx[cc // 4]
                cq = (cc % 4) * 128
                for i0 in range(0, NT, 8):
                    n8 = min(8, NT - i0)
                    pa, pt = PS(BF16)
                    for k in range(n8):
                        S.op("pe", lambda e, pa=pa, k=k: e.transpose(out=pa[:, k * 128:(k + 1) * 128], in_=ut.ap[:, (i0 + k) * 128:(i0 + k + 1) * 128], identity=identb.ap), r=[ut.t, identb.t], w=[pt])
                    S.op("act", lambda e, pa=pa: e.activation(out=dst.ap[:, i0:i0 + n8, cq:cq + 128], in_=pa[:, 0:n8 * 128].rearrange("p (a b) -> p a b", b=128), func=AF.Copy), r=[pt], w=[dst.t])
            AR.release(m1)

            if DEBUG:
                for k in range(3):
                    dma("sp", dbg_zx[s, k], zx[k].ap, r=[zx[k].t])
                dma("sp", dbg_aT[s], aT.ap, r=[aT.t])
            S.barrier()
            A2.release(0)
            YR = B(A2, [NT, DH], BF16); YS = B(A2, [NT, DH], BF16)
            dftb = [[B(AR, [NT, 128], BF16), B(AR, [NT, 128], BF16)] for _ in range(2)]
            kb = [[B(AR, [DH]), B(AR, [DH])] for _ in range(2)]
            tq = [B(AR, [DH]) for _ in range(4)]
            sk_t = B(AR, [DH]); tsum = B(AR, [DH]); yh = B(AR, [DH]); yn = B(AR, [DH], BF16)
            junk = B(AR, [DH]); ssh = B(AR, [2])
            zin = zx[0]
            for o in range(2):
                gate = zx[1 + o]
                for j in range(NT):
                    conv_some(1)
                    cb, sb_ = dftb[j % 2]
                    kr, ks = kb[j % 2]
                    dma("sp", cb.ap, cc_d[j], w=[cb.t])
                    dma("sp", sb_.ap, cs_d[j], w=[sb_.t])
                    dma("sp", kr.ap, kspec_d[o, 0, j * 128:(j + 1) * 128, :], r=[t_ks], w=[kr.t])
                    dma("sp", ks.ap, kspec_d[o, 1, j * 128:(j + 1) * 128, :], r=[t_ks], w=[ks.t])
                    pr, tr = PS(); pq, tq_ = PS()
                    for tc_ in range(NT):
                        S.op("pe", lambda e, pr=pr, tc_=tc_: e.matmul(pr, lhsT=cb.ap[:, tc_, :], rhs=zin.ap[:, tc_, :], start=(tc_ == 0), stop=(tc_ == NT - 1)), r=[cb.t, zin.t], w=[tr], silent=not (tc_ == NT - 1))
                    for tc_ in range(NT):
                        S.op("pe", lambda e, pq=pq, tc_=tc_: e.matmul(pq, lhsT=sb_.ap[:, tc_, :], rhs=zin.ap[:, tc_, :], start=(tc_ == 0), stop=(tc_ == NT - 1)), r=[sb_.t, zin.t], w=[tq_], silent=not (tc_ == NT - 1))
                    S.op("dve", lambda e, pr=pr: e.tensor_tensor(out=tq[0].ap, in0=pr, in1=kr.ap, op=OP.mult), r=[tr, kr.t], w=[tq[0].t])
                    S.op("dve", lambda e, pq=pq: e.tensor_tensor(out=tq[1].ap, in0=pq, in1=ks.ap, op=OP.mult), r=[tq_, ks.t], w=[tq[1].t])
                    S.op("pool", lambda e: e.tensor_tensor(out=YR.ap[:, j, :], in0=tq[0].ap, in1=tq[1].ap, op=OP.subtract), r=[tq[0].t, tq[1].t], w=[YR.t])
                    S.op("dve", lambda e, pr=pr: e.tensor_tensor(out=tq[2].ap, in0=pr, in1=ks.ap, op=OP.mult), r=[tr, ks.t], w=[tq[2].t])
                    S.op("dve", lambda e, pq=pq: e.tensor_tensor(out=tq[3].ap, in0=pq, in1=kr.ap, op=OP.mult), r=[tq_, kr.t], w=[tq[3].t])
                    S.op("pool", lambda e: e.tensor_tensor(out=YS.ap[:, j, :], in0=tq[2].ap, in1=tq[3].ap, op=OP.add), r=[tq[2].t, tq[3].t], w=[YS.t])
                for i in range(NT):
                    conv_some(2)
                    cb, sb_ = dftb[i % 2]
                    dma("sp", cb.ap, cct_d[i], w=[cb.t])
                    dma("sp", sb_.ap, cst_d[i], w=[sb_.t])
                    pa, pt = PS()
                    for fc in range(NT):
                        S.op("pe", lambda e, pa=pa, fc=fc: e.matmul(pa, lhsT=cb.ap[:, fc, :], rhs=YR.ap[:, fc, :], start=(fc == 0), stop=False), r=[cb.t, YR.t], w=[pt], silent=not False)
                    for fc in range(NT):
                        S.op("pe", lambda e, pa=pa, fc=fc: e.matmul(pa, lhsT=sb_.ap[:, fc, :], rhs=YS.ap[:, fc, :], start=False, stop=(fc == NT - 1)), r=[sb_.t, YS.t], w=[pt], silent=not (fc == NT - 1))
                    S.op("pool", lambda e: e.tensor_tensor(out=sk_t.ap, in0=zin.ap[:, i, :], in1=skipB[o], op=OP.mult), r=[zin.t, rows.t], w=[sk_t.t])
                    S.op("dve", lambda e, pa=pa: e.tensor_tensor(out=tsum.ap, in0=pa, in1=sk_t.ap, op=OP.add), r=[pt, sk_t.t], w=[tsum.t])
                    if o == 0:
                        S.op("pool", lambda e: e.tensor_tensor(out=gate.ap[:, i, :], in0=tsum.ap, in1=gate.ap[:, i, :], op=OP.mult), r=[tsum.t, gate.t], w=[gate.t])
                    else:
                        S.op("pool", lambda e: e.tensor_tensor(out=yh.ap, in0=tsum.ap, in1=gate.ap[:, i, :], op=OP.mult), r=[tsum.t, gate.t], w=[yh.t])
                        sumsq(yh.ap, junk.ap, ssh.ap[:, 0:1], [yh.t], junk.t, ssh.t)
                        rstd_from_ss(ssh.ap[:, 0:1], ssh.ap[:, 1:2], DH, 128, ssh.t)
                        S.op("dve", lambda e: e.scalar_tensor_tensor(out=yn.ap, in0=yh.ap, scalar=ssh.ap[:, 1:2], in1=g_hy, op0=OP.mult, op1=OP.mult), r=[yh.t, ssh.t, rows.t], w=[yn.t])
                        pb2, pt2 = PS(BF16)
                        for k in range(4):
                            S.op("pe", lambda e, pb2=pb2, k=k: e.transpose(out=pb2[:, k * 128:(k + 1) * 128], in_=yn.ap[:, k * 128:(k + 1) * 128], identity=identb.ap), r=[yn.t, identb.t], w=[pt2])
                        S.op("act", lambda e, pb2=pb2: e.activation(out=mixT.ap[:, 0:4, i * 128:(i + 1) * 128], in_=pb2[:, 0:512].rearrange("p (a b) -> p a b", b=128), func=AF.Copy), r=[pt2], w=[mixT.t])
                zin = zx[1]
            AR.release(m0)

            if DEBUG:
                dma("sp", dbg_mix[s], mixT.ap, r=[mixT.t])
            S.barrier()
            A2.release(0)
            wo = B(A2, [8, D], BF16)
            dma("pool", wo.ap, w_out_d.rearrange("(dc p) c -> p dc c", p=128), w=[wo.t])
            m0 = AR.mark()
            hres = [B(AR, [D]) for _ in range(2)]
            h1 = [B(AR, [D]) for _ in range(2)]
            hf = [B(AR, [D]) for _ in range(2)]
            hfT = B(AR, [8, 128])
            junk = B(AR, [D]); ssr = [B(AR, [2]) for _ in range(2)]
            hfball = B(AR, [NT, D], BF16)
            lgall = B(AR, [NT, 8 + NE])
            S.op("pool", lambda e: e.memset(lgall.ap, 0.0), w=[lgall.t])
            for i in range(NT):
                npart = 128 if i < NT - 1 else LAST
                tix = s * NT + i
                hr, h1_, hf_, ss = hres[i % 2], h1[i % 2], hf[i % 2], ssr[i % 2]
                load_h_tile("sp", hr, s, i, npart)
                for half in range(2):
                    pa, pt = PS()
                    for cc in range(8):
                        S.op("pe", lambda e, pa=pa, cc=cc: e.matmul(pa[:npart], lhsT=mixT.ap[:, cc, i * 128:i * 128 + npart], rhs=wo.ap[:, cc, half * 512:(half + 1) * 512], start=(cc == 0), stop=(cc == 7)), r=[mixT.t, wo.t], w=[pt], silent=not (cc == 7))
                    S.op("dve", lambda e, pa=pa: e.tensor_tensor(out=h1_.ap[:npart, half * 512:(half + 1) * 512], in0=pa[:npart], in1=hr.ap[:npart, half * 512:(half + 1) * 512], op=OP.add), r=[pt, hr.t], w=[h1_.t])
                dma("sp", h1_d[s * T + i * 128:s * T + i * 128 + npart, :], h1_.ap[:npart], r=[h1_.t], w=[t_h1])
                sumsq(h1_.ap[:npart], junk.ap[:npart], ss.ap[:npart, 0:1], [h1_.t], junk.t, ss.t)
                rstd_from_ss(ss.ap[:, 0:1], ss.ap[:, 1:2], D, npart, ss.t)
                S.op("dve", lambda e: e.scalar_tensor_tensor(out=hf_.ap[:npart], in0=h1_.ap[:npart], scalar=ss.ap[:npart, 1:2], in1=g_ffn[:npart], op0=OP.mult, op1=OP.mult), r=[h1_.t, ss.t, rows.t], w=[hf_.t])
                S.op("pool", lambda e: e.tensor_copy(out=hfball.ap[:npart, i, :], in_=hf_.ap[:npart]), r=[hf_.t], w=[hfball.t])
                for q4 in range(2):
                    pa, pt = PS()
                    for k in range(4):
                        dc = q4 * 4 + k
                        S.op("pe", lambda e, pa=pa, k=k, dc=dc: e.transpose(out=pa[:, k * 128:k * 128 + npart], in_=hf_.ap[:npart, dc * 128:(dc + 1) * 128], identity=ident[:npart, :npart]), r=[hf_.t, misc.t], w=[pt])
                    S.op("act", lambda e, pa=pa: e.activation(out=hfT.ap[:, q4 * 4:q4 * 4 + 4, 0:npart], in_=pa.rearrange("p (a b) -> p a b", b=128)[:, :, 0:npart], func=AF.Copy), r=[pt], w=[hfT.t])
                pr, tr = PS()
                for dc in range(8):
                    S.op("pe", lambda e, pr=pr, dc=dc: e.matmul(pr[:npart, 0:8 + NE], lhsT=hfT.ap[:, dc, 0:npart], rhs=wr.ap[:, dc, :], start=(dc == 0), stop=(dc == 7)), r=[hfT.t, wr.t], w=[tr], silent=not (dc == 7))
                S.op("act", lambda e, pr=pr: e.activation(out=lgall.ap[:npart, i, :], in_=pr[:npart, 0:8 + NE], func=AF.Copy), r=[tr], w=[lgall.t])
            G = NG
            b2 = lambda: B(AR, [NT]).ap
            gmax, sume, pg, m1, m2, dd, sig, gidx, e1, e2, ei1, ei2 = (b2() for _ in range(12))
            rr = [b2(), b2()]
            b8 = lambda: B(AR, [NT, 8]).ap
            t8, ohg, el, el2, oh1, oh2 = (b8() for _ in range(6))
            O2 = B(AR, [NT, NE]).ap; Oa = B(AR, [NT, NE]).ap; rk = B(AR, [NT, NE]).ap; sv = B(AR, [NT, NE]).ap
            prod4 = B(A2, [NT, NE]).ap; O1 = B(A2, [NT, NE]).ap; csum = B(A2, [NT, NE]).ap; cum = B(A2, [NT, NE]).ap
            slf = B(AR, [NT, 2]).ap
            RT = Tk("route")
            iotaTE = B(AR, [NT, NE])
            for t_ in range(NT):
                S.op("pool", lambda e: e.tensor_copy(out=iotaTE.ap[:, t_, :], in_=iota64[:, 0:NE]), r=[misc.t], w=[iotaTE.t])
            valid = misc.ap[:, 720:720 + NT]
            bc = lambda a2, n: a2.unsqueeze(2).to_broadcast([128, NT, n])

            def dvb(fn, er=(), ew=()):
                S.op("dve", fn, r=[RT] + list(er), w=[RT] + list(ew))
            lg3 = lgall.ap[:, :, 0:G]
            le4 = lgall.ap[:, :, 8:8 + NE].rearrange("p t (g k) -> p t g k", k=8)
            dvb(lambda e: e.tensor_reduce(out=gmax, in_=lg3, axis=AX.X, op=OP.max), er=[lgall.t])
            dvb(lambda e: e.tensor_tensor(out=t8[:, :, 0:G], in0=lg3, in1=bc(gmax, G), op=OP.subtract), er=[lgall.t])
            dvb(lambda e: e.tensor_scalar(out=ohg[:, :, 0:G], in0=t8[:, :, 0:G], scalar1=0.0, scalar2=None, op0=OP.is_equal))
            S.op("act", lambda e: e.activation(out=t8[:, :, 0:G], in_=t8[:, :, 0:G], func=AF.Exp), r=[RT], w=[RT])
            dvb(lambda e: e.tensor_reduce(out=sume, in_=t8[:, :, 0:G], axis=AX.X, op=OP.add))
            dvb(lambda e: e.reciprocal(out=pg, in_=sume))
            p4v = prod4.rearrange("p t (g k) -> p t g k", k=8)
            dvb(lambda e: e.tensor_tensor(out=p4v, in0=le4, in1=ohg[:, :, 0:G].unsqueeze(3).to_broadcast([128, NT, G, 8]), op=OP.mult), er=[lgall.t])
            dvb(lambda e: e.tensor_reduce(out=el, in_=p4v.rearrange("p t g k -> p t k g"), axis=AX.X, op=OP.add))
            dvb(lambda e: e.tensor_reduce(out=m1, in_=el, axis=AX.X, op=OP.max))
            dvb(lambda e: e.tensor_tensor(out=oh1, in0=el, in1=bc(m1, 8), op=OP.is_equal))
            dvb(lambda e: e.scalar_tensor_tensor(out=el2, in0=oh1, scalar=-1e30, in1=el, op0=OP.mult, op1=OP.add))
            dvb(lambda e: e.tensor_reduce(out=m2, in_=el2, axis=AX.X, op=OP.max))
            dvb(lambda e: e.tensor_tensor(out=oh2, in0=el2, in1=bc(m2, 8), op=OP.is_equal))
            dvb(lambda e: e.tensor_tensor(out=dd, in0=m1, in1=m2, op=OP.subtract))
            S.op("act", lambda e: e.activation(out=sig, in_=dd, func=AF.Sigmoid), r=[RT], w=[RT])
            for (oh, dst, n) in ((ohg, gidx, G), (oh1, e1, 8), (oh2, e2, 8)):
                dvb(lambda e, oh=oh, n=n: e.tensor_tensor(out=t8[:, :, 0:n], in0=oh[:, :, 0:n], in1=iota8[:, 0:n].unsqueeze(1).to_broadcast([128, NT, n]), op=OP.mult), er=[misc.t])
                dvb(lambda e, dst=dst, n=n: e.tensor_reduce(out=dst, in_=t8[:, :, 0:n], axis=AX.X, op=OP.add))
            for (ek, eik, Ok) in ((e1, ei1, O1), (e2, ei2, O2)):
                dvb(lambda e, ek=ek, eik=eik: e.scalar_tensor_tensor(out=eik, in0=gidx, scalar=8.0, in1=ek, op0=OP.mult, op1=OP.add))
                dvb(lambda e, eik=eik, Ok=Ok: e.tensor_tensor(out=Ok, in0=iotaTE.ap, in1=bc(eik, NE), op=OP.is_equal), er=[iotaTE.t])
                dvb(lambda e, Ok=Ok: e.tensor_tensor(out=Ok, in0=Ok, in1=bc(valid, NE), op=OP.mult), er=[misc.t])
            dvb(lambda e: e.tensor_tensor(out=Oa, in0=O1, in1=O2, op=OP.add))
            NCOL = NT * NE
            fl = lambda a: a.rearrange("p t e -> p (t e)")
            for c0 in range(0, NCOL, 512):
                cn = min(512, NCOL - c0)
                pk, tk_ = PS()
                S.op("pe", lambda e, pk=pk: e.matmul(pk[:, 0:cn], lhsT=ustrict, rhs=fl(Oa)[:, c0:c0 + cn], start=True, stop=True), r=[RT, misc.t], w=[tk_])
                dvb(lambda e, pk=pk: e.tensor_copy(out=fl(rk)[:, c0:c0 + cn], in_=pk[:, 0:cn]), er=[tk_])
                pc, tc2 = PS()
                S.op("pe", lambda e, pc=pc: e.matmul(pc[:, 0:cn], lhsT=ones, rhs=fl(Oa)[:, c0:c0 + cn], start=True, stop=True), r=[RT, misc.t], w=[tc2])
                dvb(lambda e, pc=pc: e.tensor_copy(out=fl(csum)[:, c0:c0 + cn], in_=pc[:, 0:cn]), er=[tc2])
            dvb(lambda e: e.tensor_copy(out=cum[:, 0, :], in_=cnt.ap), er=[cnt.t])
            for t_ in range(1, NT):
                dvb(lambda e: e.tensor_tensor(out=cum[:, t_, :], in0=cum[:, t_ - 1, :], in1=csum[:, t_ - 1, :], op=OP.add))
            dvb(lambda e: e.tensor_tensor(out=cnt.ap, in0=cum[:, NT - 1, :], in1=csum[:, NT - 1, :], op=OP.add), er=[cnt.t], ew=[cnt.t])
            dvb(lambda e: e.tensor_tensor(out=rk, in0=rk, in1=cum, op=OP.add))
            dvb(lambda e: e.tensor_tensor(out=sv, in0=rk, in1=slotbase.ap.unsqueeze(1).to_broadcast([128, NT, NE]), op=OP.add), er=[slotbase.t])
            for kq, Ok in enumerate((O1, O2)):
                dvb(lambda e, Ok=Ok: e.tensor_tensor(out=Oa, in0=Ok, in1=rk, op=OP.mult))
                dvb(lambda e, kq=kq: e.tensor_reduce(out=rr[kq], in_=Oa, axis=AX.X, op=OP.add))
                dvb(lambda e, Ok=Ok: e.tensor_tensor(out=Oa, in0=Ok, in1=sv, op=OP.mult))
                dvb(lambda e, kq=kq: e.tensor_reduce(out=slf[:, :, kq], in_=Oa, axis=AX.X, op=OP.add))
                dvb(lambda e, kq=kq: e.tensor_scalar(out=rr[kq], in0=rr[kq], scalar1=float(CAP) - 0.5, scalar2=None, op0=OP.is_gt))
                dvb(lambda e, kq=kq: e.scalar_tensor_tensor(out=slf[:, :, kq], in0=rr[kq], scalar=float(NSLOT), in1=slf[:, :, kq], op0=OP.mult, op1=OP.add))
                dvb(lambda e, kq=kq: e.tensor_scalar(out=rr[kq], in0=rr[kq], scalar1=-1.0, scalar2=1.0, op0=OP.mult, op1=OP.add))
            gs = gates.ap[:, s * NT:(s + 1) * NT, :]
            dvb(lambda e: e.tensor_tensor(out=gs[:, :, 0], in0=pg, in1=sig, op=OP.mult), er=[gates.t], ew=[gates.t])
            dvb(lambda e: e.tensor_tensor(out=gs[:, :, 1], in0=pg, in1=gs[:, :, 0], op=OP.subtract), er=[gates.t], ew=[gates.t])
            for kq in range(2):
                dvb(lambda e, kq=kq: e.tensor_tensor(out=gs[:, :, kq], in0=gs[:, :, kq], in1=rr[kq], op=OP.mult), er=[gates.t], ew=[gates.t])
            dvb(lambda e: e.tensor_copy(out=slots.ap[:, s * NT:(s + 1) * NT, :], in_=slf), er=[slots.t], ew=[slots.t])
            for i in range(NT):
                P = 128 if i < NT - 1 else LAST
                tix = s * NT + i
                for kq in range(2):
                    S.op("pool", lambda e, kq=kq: e.indirect_dma_start(out=xs_d, out_offset=bass.IndirectOffsetOnAxis(ap=slots.ap[:P, tix, kq:kq + 1], axis=0), in_=hfball.ap[:P, i, :], in_offset=None, bounds_check=bcreg(e), oob_is_err=False), r=[slots.t, hfball.t], w=[t_xs], dma=True)
            AR.release(m0)

        conv_some(10 ** 9)
        if DEBUG:
            dma("sp", dbg_sg[:, :, 0:2], gates.ap, r=[gates.t])
            dma("sp", dbg_sg[:, :, 2:4].bitcast(I32), slots.ap, r=[slots.t])
        new_phase()
        NWB = 4
        wgb = [B(AR, [8, 512], BF16) for _ in range(NWB)]
        wub = [B(AR, [8, 512], BF16) for _ in range(NWB)]
        wdb = [B(AR, [4, D], BF16) for _ in range(NWB)]
        for bb in wgb + wub + wdb:
            bb.th = [Tk(), Tk()]
        NST = CAP // 128
        xT = [B(A1, [8, CAP], BF16) for _ in range(2)]
        sgb = [B(A1, [CAP]) for _ in range(2)]
        actb = [B(A1, [4, CAP], BF16) for _ in range(2)]
        ybuf = [B(A2, [D]) for _ in range(2)]
        PF = 2
        xsb = [B(A1, [D], BF16) for _ in range((PF + 1) * NST)]

        def issue_loads(ex):
            wg_, wu_, wd_ = wgb[ex % NWB], wub[ex % NWB], wdb[ex % NWB]
            for h2 in range(2):
                dma("pool", wg_.ap[:, h2 * 4:h2 * 4 + 4, :], wg_d[ex].rearrange("(dc p) c -> p dc c", p=128)[:, h2 * 4:h2 * 4 + 4, :], w=[wg_.th[h2]])
                dma("pool", wu_.ap[:, h2 * 4:h2 * 4 + 4, :], wu_d[ex].rearrange("(dc p) c -> p dc c", p=128)[:, h2 * 4:h2 * 4 + 4, :], w=[wu_.th[h2]])
            for h2 in range(2):
                dma("pool", wd_.ap[:, h2 * 2:h2 * 2 + 2, :], wd_d[ex].rearrange("(fc p) c -> p fc c", p=128)[:, h2 * 2:h2 * 2 + 2, :], w=[wd_.th[h2]])
            for st_ in range(NST):
                xb_ = xsb[(ex % (PF + 1)) * NST + st_]
                dma("sp", xb_.ap, xs_d[ex * CAP + st_ * 128:ex * CAP + (st_ + 1) * 128, :], r=[t_xs], w=[xb_.t])

        for ex in range(min(PF, NE)):
            issue_loads(ex)
        for ex in range(NE):
            if ex + PF < NE:
                issue_loads(ex + PF)
            wg_, wu_, wd_ = wgb[ex % NWB], wub[ex % NWB], wdb[ex % NWB]
            xt = xT[ex % 2]; ab_ = actb[ex % 2]
            for st_ in range(NST):
                xb_ = xsb[(ex % (PF + 1)) * NST + st_]
                pa, pt = PS(BF16)
                for dc in range(8):
                    S.op("pe", lambda e, pa=pa, dc=dc, xb_=xb_: e.transpose(out=pa[:, dc * 128:(dc + 1) * 128], in_=xb_.ap[:, dc * 128:(dc + 1) * 128], identity=identb.ap), r=[xb_.t, identb.t], w=[pt])
                S.op("act", lambda e, pa=pa, st_=st_: e.activation(out=xt.ap[:, :, st_ * 128:(st_ + 1) * 128], in_=pa.rearrange("p (a b) -> p a b", b=128), func=AF.Copy), r=[pt], w=[xt.t])
            for fc in range(4):
                pg_, tg = PS(); pu_, tu = PS()
                for dc in range(8):
                    S.op("pe", lambda e, pg_=pg_, dc=dc, fc=fc: e.matmul(pg_[:, 0:CAP], lhsT=wg_.ap[:, dc, fc * 128:(fc + 1) * 128], rhs=xt.ap[:, dc, :], start=(dc == 0), stop=(dc == 7)), r=[wg_.th[dc // 4], xt.t], w=[tg], silent=not (dc == 7))
                for dc in range(8):
                    S.op("pe", lambda e, pu_=pu_, dc=dc, fc=fc: e.matmul(pu_[:, 0:CAP], lhsT=wu_.ap[:, dc, fc * 128:(fc + 1) * 128], rhs=xt.ap[:, dc, :], start=(dc == 0), stop=(dc == 7)), r=[wu_.th[dc // 4], xt.t], w=[tu], silent=not (dc == 7))
                sg_ = sgb[fc % 2]
                S.op("act", lambda e, pg_=pg_, sg_=sg_: e.activation(out=sg_.ap, in_=pg_[:, 0:CAP], func=AF.Silu), r=[tg], w=[sg_.t])
                S.op("dve", lambda e, pu_=pu_, sg_=sg_, fc=fc: e.tensor_tensor(out=ab_.ap[:, fc, :], in0=pu_[:, 0:CAP], in1=sg_.ap, op=OP.mult), r=[tu, sg_.t], w=[ab_.t])
            for st_ in range(NST):
                yb_ = ybuf[st_ % 2]
                for half in range(2):
                    po, to = PS()
                    for fc in range(4):
                        S.op("pe", lambda e, po=po, fc=fc, st_=st_, half=half: e.matmul(po, lhsT=ab_.ap[:, fc, st_ * 128:(st_ + 1) * 128], rhs=wd_.ap[:, fc, half * 512:(half + 1) * 512], start=(fc == 0), stop=(fc == 3)), r=[ab_.t, wd_.th[fc // 2]], w=[to], silent=not (fc == 3))
                    if half == 0:
                        S.op("act", lambda e, po=po, yb_=yb_: e.activation(out=yb_.ap[:, 0:512], in_=po, func=AF.Copy), r=[to], w=[yb_.t])
                    else:
                        S.op("dve", lambda e, po=po, yb_=yb_: e.tensor_copy(out=yb_.ap[:, 512:1024], in_=po), r=[to], w=[yb_.t])
                dma("sp", y_d[ex * CAP + st_ * 128:ex * CAP + (st_ + 1) * 128, :], yb_.ap, r=[yb_.t], w=[t_y])

        new_phase()
        NCB = 4
        y1 = [B(AR, [D]) for _ in range(NCB)]; y2 = [B(AR, [D]) for _ in range(NCB)]
        hh = [B(AR, [D]) for _ in range(NCB)]; ob = [B(AR, [D]) for _ in range(NCB)]
        junks = [B(AR, [D]) for _ in range(2)]; ssf = [B(AR, [2]) for _ in range(NCB)]
        for bb in y1 + y2:
            S.op("pool", lambda e, bb=bb: e.memset(bb.ap, 0.0), w=[bb.t])
        def issue_h1(tix):
            s, i = divmod(tix, NT)
            P = 128 if i < NT - 1 else LAST
            dma("sp", hh[tix % NCB].ap[:P], h1_d[s * T + i * 128:s * T + i * 128 + P, :], r=[t_h1], w=[hh[tix % NCB].t])

        PFC = 2
        for tix in range(min(PFC, NTT)):
            issue_h1(tix)
        for s in range(2):
            for i in range(NT):
                P = 128 if i < NT - 1 else LAST
                tix = s * NT + i
                if tix + PFC < NTT:
                    issue_h1(tix + PFC)
                a, b_, h_, o_, ss = y1[tix % NCB], y2[tix % NCB], hh[tix % NCB], ob[tix % NCB], ssf[tix % NCB]
                junk = junks[tix % 2]
                for kq, dst in enumerate((a, b_)):
                    S.op("pool", lambda e, kq=kq, dst=dst: e.indirect_dma_start(out=dst.ap[:P], out_offset=None, in_=y_d, in_offset=bass.IndirectOffsetOnAxis(ap=slots.ap[:P, tix, kq:kq + 1], axis=0), bounds_check=bcreg(e), oob_is_err=False), r=[slots.t, t_y], w=[dst.t], dma=True)
                S.op("dve", lambda e: e.scalar_tensor_tensor(out=h_.ap[:P], in0=a.ap[:P], scalar=gates.ap[:P, tix, 0:1], in1=h_.ap[:P], op0=OP.mult, op1=OP.add), r=[a.t, gates.t, h_.t], w=[h_.t])
                S.op("dve", lambda e: e.scalar_tensor_tensor(out=h_.ap[:P], in0=b_.ap[:P], scalar=gates.ap[:P, tix, 1:2], in1=h_.ap[:P], op0=OP.mult, op1=OP.add), r=[b_.t, gates.t, h_.t], w=[h_.t])
                sumsq(h_.ap[:P], junk.ap[:P], ss.ap[:P, 0:1], [h_.t], junk.t, ss.t)
                rstd_from_ss(ss.ap[:, 0:1], ss.ap[:, 1:2], D, P, ss.t)
                S.op("dve", lambda e: e.scalar_tensor_tensor(out=o_.ap[:P], in0=h_.ap[:P], scalar=ss.ap[:P, 1:2], in1=g_fin[:P], op0=OP.mult, op1=OP.mult), r=[h_.t, ss.t, rows.t], w=[o_.t])
                if i == 0:
                    dma("sp", out_d[s, 0:128 - N_META, :], o_.ap[N_META:128], r=[o_.t])
                else:
                    r0 = i * 128 - N_META
                    dma("sp", out_d[s, r0:r0 + P, :], o_.ap[:P], r=[o_.t])
        S.emit()
    return nc


_CACHE = {}


def run_kernel(inputs, n_cores, NG, CAP):
    x = np.asarray(inputs["x"], np.float32)
    Bt, SEQ, _ = x.shape
    assert Bt == 2 * n_cores
    L = SEQ + N_META
    NT = (L + 127) // 128
    NE = NG * 8
    key = (SEQ, NG, CAP)
    if key not in _CACHE:
        _CACHE[key] = build_program(SEQ, NG, CAP)
    nc = _CACHE[key]
    f = lambda k: np.asarray(inputs[k], np.float32)
    c = host_consts(L, NT)
    rep = lambda v: np.broadcast_to(v.reshape(1, -1), (128, v.size))
    rows = np.zeros((128, 6144), np.float32)
    rows[:, 0:1024] = rep(f("norm_mix")[0]); rows[:, 1024:2048] = rep(f("norm_ffn")[0]); rows[:, 2048:3072] = rep(f("norm_final"))
    rows[:, 3072:3584] = rep(f("hyena_norm")[0]); rows[:, 3584:4096] = rep(f("hgrn_norm")[0])
    rows[:, 4096:5120] = rep(f("filt_skip")[0].reshape(-1))
    convw = np.zeros((128, 12, 4), np.float32)
    convw[:, :, 0:3] = f("conv_w")[0].reshape(3, 12, 128).transpose(2, 1, 0)
    convw[:, :, 3] = f("conv_b")[0].reshape(12, 128).T
    lb = np.stack([f("lb_fwd"), f("lb_bwd")], 0)
    lb = np.ascontiguousarray(lb.reshape(2, 2, 4, 128).transpose(3, 0, 1, 2))
    wr = np.concatenate([f("w_router_group")[0], f("w_router_expert")[0].reshape(D, NE)], axis=1)
    wr_full = np.zeros((D, 8 + NE), np.float32)
    wr_full[:, 0:NG] = wr[:, 0:NG]; wr_full[:, 8:] = wr[:, NG:]
    wr_l = np.ascontiguousarray(wr_full.reshape(8, 128, 8 + NE).transpose(1, 0, 2))
    fb = np.zeros((64, 4), np.float32)
    fb[:, 0] = f("filt_b1")[0]; fb[:, 1] = f("filt_b2")[0]; fb[:, 2] = f("filt_freq")[0]
    shared = dict(meta=f("meta_tokens"), w_in=f("w_in")[0], w_out=f("w_out")[0], w_gate=f("w_gate")[0], w_up=f("w_up")[0],
                  w_down=f("w_down")[0], w_r=wr_l, rows=rows, convw=convw, lb=lb, f_w1=f("filt_w1")[0], f_w2=f("filt_w2")[0],
                  f_w3=f("filt_w3")[0], f_b=fb, **c)
    in_maps = []
    for ci in range(n_cores):
        m = dict(shared)
        m["x"] = np.ascontiguousarray(x[2 * ci:2 * ci + 2])
        in_maps.append(m)
    res = run_bass_kernel_spmd(nc, in_maps, core_ids=list(range(n_cores)))
    if DEBUG:
        global LAST_RES
        LAST_RES = res.results
    return np.concatenate([r["out"] for r in res.results], axis=0).astype(np.float32)


def kernel(**inputs):
    return run_kernel(inputs, 8, 8, 256)
```

```python
import math
import numpy as np
import ml_dtypes
import concourse.bass as bass
import concourse.mybir as mybir
from concourse.bass_utils import run_bass_kernel_spmd

F32 = mybir.dt.float32
BF16 = mybir.dt.bfloat16
I32 = mybir.dt.int32
AF = mybir.ActivationFunctionType
OP = mybir.AluOpType
AX = mybir.AxisListType

ENGS = ("pe", "act", "dve", "pool", "sp")


import types


def _freeze(fn):
    if fn.__closure__ is None:
        return fn
    cells = []
    for c in fn.__closure__:
        try:
            cells.append(types.CellType(c.cell_contents))
        except ValueError:
            cells.append(c)
    return types.FunctionType(fn.__code__, fn.__globals__, fn.__name__, fn.__defaults__, tuple(cells))


class Tk:
    __slots__ = ("w", "r", "name")

    def __init__(self, name=""):
        self.w = None
        self.r = []
        self.name = name


class Sched:
    def __init__(self, nc, n_dma_sems=24):
        self.nc = nc
        self.ops = {e: [] for e in ENGS}
        self.count = {e: 0 for e in ENGS}
        self.known = {e: {} for e in ENGS}
        self.n_hw = n_dma_sems
        self.n_grp1 = 12
        self.n_dma = n_dma_sems + self.n_grp1
        self.dma_cnt = [0] * self.n_dma
        self.dma_tok = [Tk("dmasem%d" % i) for i in range(self.n_dma)]
        self.dma_rr = 0
        self.rr1 = 0
        self.out_events = []

    def _need(self, eng, ev, waits):
        if ev is None:
            return
        key, val, clock = ev
        kn = self.known[eng]
        if key == eng and eng == "pe":
            return
        if kn.get(key, 0) >= val:
            return
        waits[key] = max(waits.get(key, 0), val)
        for k, v in clock.items():
            if kn.get(k, 0) < v:
                kn[k] = v
        if kn.get(key, 0) < val:
            kn[key] = val

    def op(self, eng, fn, r=(), w=(), dma=False, silent=False, grp=0):
        fn = _freeze(fn)
        silent = bool(silent) and eng == "pe" and not dma
        waits = {}
        slot = None
        toks_w = list(w)
        if dma:
            if grp == 1:
                slot = self.n_hw + self.rr1
                self.rr1 = (self.rr1 + 1) % self.n_grp1
            else:
                slot = self.dma_rr
                self.dma_rr = (self.dma_rr + 1) % self.n_hw
            toks_w.append(self.dma_tok[slot])
        for t in r:
            self._need(eng, t.w, waits)
        for t in toks_w:
            self._need(eng, t.w, waits)
            for ev in t.r:
                self._need(eng, ev, waits)
        if dma:
            self.dma_cnt[slot] += 16
            key, val = ("d", slot), self.dma_cnt[slot]
        elif silent:
            key, val = eng, self.count[eng] + 1
        else:
            self.count[eng] += 1
            key, val = eng, self.count[eng]
        clock = dict(self.known[eng])
        ev = (key, val, clock)
        if not dma and eng == "pe" and not silent:
            self.known[eng][eng] = val
        for t in r:
            t.r.append(ev)
        for t in toks_w:
            t.w = ev
            t.r = []
        self.ops[eng].append((sorted(waits.items(), key=str), fn, key, 16 if dma else (0 if silent else 1)))
        return ev

    def barrier(self):
        evs = []
        for e in ENGS:
            if self.count[e]:
                evs.append((e, self.count[e], {}))
        for s in range(self.n_dma):
            if self.dma_cnt[s]:
                evs.append((("d", s), self.dma_cnt[s], {}))
        for e in ENGS:
            waits = {}
            for ev in evs:
                if ev[0] != e:
                    self._need(e, ev, waits)
            if waits:
                self.ops[e].append((sorted(waits.items(), key=str), None, None, 0))

    def emit(self):
        nc = self.nc
        import contextlib
        with contextlib.ExitStack() as st:
            sems = {}
            for e in ENGS:
                sems[e] = st.enter_context(nc.semaphore("sem_" + e))
            for s in range(self.n_dma):
                sems[("d", s)] = st.enter_context(nc.semaphore("sem_d%d" % s))
            fin = {}
            for e in ENGS:
                if e != "sp" and self.count[e]:
                    fin[e] = self.count[e]
            for s in range(self.n_dma):
                if self.dma_cnt[s]:
                    fin[("d", s)] = self.dma_cnt[s]
            self.ops["sp"].append((sorted(fin.items(), key=str), None, None, 0))
            block = st.enter_context(nc.Block())

            def run(engname):
                def body(eng):
                    for waits, fn, key, inc in self.ops[engname]:
                        for k, v in waits:
                            eng.wait_ge(sems[k], v)
                        if fn is not None:
                            if inc:
                                fn(eng).then_inc(sems[key], inc)
                            else:
                                fn(eng)
                return body

            block.tensor(run("pe"))
            block.scalar(run("act"))
            block.vector(run("dve"))
            block.gpsimd(run("pool"))
            block.sync(run("sp"))


class Arena:
    def __init__(self, ap, nwords):
        self.ap = ap
        self.n = nwords
        self.top = 0

    def mark(self):
        return self.top

    def release(self, m):
        self.top = m

    def alloc(self, shape, dtype=F32):
        n = int(np.prod(shape))
        words = n if dtype in (F32, I32) else (n + 1) // 2
        words = (words + 7) // 8 * 8
        assert self.top + words <= self.n, "SBUF arena overflow %d + %d > %d" % (self.top, words, self.n)
        v = self.ap[:, self.top:self.top + words]
        self.top += words
        if dtype != F32:
            v = v.bitcast(dtype)
        v = v[:, 0:n]
        if len(shape) == 2:
            v = v.rearrange("p (a b) -> p a b", b=shape[1])
        elif len(shape) == 3:
            v = v.rearrange("p (a b c) -> p a b c", b=shape[1], c=shape[2])
        return v


N_META = 16
D = 1024
DH = 512
EPS = 1e-6
NWORDS = 52992


def host_consts(L, NT):
    T = NT * 128
    N = 2 * T
    t = np.arange(T, dtype=np.float64)
    f = np.arange(T, dtype=np.float64) + 0.5
    ang = 2.0 * np.pi / N * np.outer(t, f)
    valid = (t < L)[:, None]
    Cc = np.where(valid, np.cos(ang), 0.0)
    Cs = np.where(valid, np.sin(ang), 0.0)

    def lay(M):
        return np.ascontiguousarray(M.reshape(NT, 128, NT, 128).transpose(2, 1, 0, 3)).astype(ml_dtypes.bfloat16)

    c = {}
    c["dft_cc"] = lay(Cc)
    c["dft_cs"] = lay(Cs)
    c["dft_cct"] = lay(Cc.T.copy())
    c["dft_cst"] = lay(Cs.T.copy())
    pos = np.arange(L, dtype=np.float32)
    tt = pos / max(L - 1, 1)
    bands = np.linspace(1e-4, 15, 16, dtype=np.float32)
    a2 = (2.0 * math.pi / L) * pos[:, None] * bands[None, :]
    z = np.concatenate([tt[:, None], np.cos(a2), -np.sin(a2)], axis=-1).astype(np.float32)
    zT = np.zeros((33, T), np.float32)
    zT[:, :L] = z.T
    c["zfeat"] = zT
    deltas = np.abs(np.linspace(math.log(1e-2) / 1.5, math.log(1e-2) / 0.3, DH, dtype=np.float32))
    win = np.zeros((T, DH), np.float32)
    win[:L] = np.exp(-tt[:, None] * deltas[None, :])
    winb = win.copy()
    winb[0] = 0.0
    c["win_f"] = np.ascontiguousarray(win.reshape(NT, 128, DH).transpose(1, 0, 2))
    c["win_b"] = np.ascontiguousarray(winb.reshape(NT, 128, DH).transpose(1, 0, 2))
    i = np.arange(128)
    misc = np.zeros((128, 1024), np.float32)
    misc[:, 0:128] = np.eye(128)
    misc[:, 128:256] = (i[None, :] >= i[:, None])
    misc[:, 256:384] = (i[None, :] <= i[:, None])
    misc[:, 384:512] = (i[:, None] < i[None, :])
    misc[:, 512:640] = 1.0
    misc[:, 640:704] = np.arange(64)[None, :]
    misc[:, 704:712] = np.arange(8)[None, :]
    misc[:, 712] = i
    misc[:, 720:720 + NT] = 1.0
    misc[L - (NT - 1) * 128:, 720 + NT - 1] = 0.0
    c["misc"] = misc
    return c


DEBUG = False


def build_program(SEQ, NG, CAP):
    L = SEQ + N_META
    NT = (L + 127) // 128
    T = NT * 128
    LAST = L - (NT - 1) * 128
    NE = NG * 8
    NSLOT = NE * CAP
    NTT = 2 * NT
    NDFT = 2 * T
    nc = bass.Bass("TRN2", target_bir_lowering=False)

    def din(name, shape, dt=F32):
        return nc.dram_tensor(name, list(shape), dt, kind="ExternalInput").ap()

    x_d = din("x", [2, SEQ, D])
    meta_d = din("meta", [N_META, D])
    w_in_d = din("w_in", [D, 4096])
    w_out_d = din("w_out", [D, D])
    wg_d = din("w_gate", [NE, D, 512])
    wu_d = din("w_up", [NE, D, 512])
    wd_d = din("w_down", [NE, 512, D])
    wr_d = din("w_r", [128, 8, 8 + NE])
    rows_d = din("rows", [128, 6144])
    convw_d = din("convw", [128, 12, 4])
    lb_d = din("lb", [128, 2, 2, 4])
    f1_d = din("f_w1", [33, 64])
    f2_d = din("f_w2", [64, 64])
    f3_d = din("f_w3", [64, 2048])
    fb_d = din("f_b", [64, 4])
    misc_d = din("misc", [128, 1024])
    zfeat_d = din("zfeat", [33, T])
    winf_d = din("win_f", [128, NT, DH])
    winb_d = din("win_b", [128, NT, DH])
    cc_d = din("dft_cc", [NT, 128, NT, 128], BF16)
    cs_d = din("dft_cs", [NT, 128, NT, 128], BF16)
    cct_d = din("dft_cct", [NT, 128, NT, 128], BF16)
    cst_d = din("dft_cst", [NT, 128, NT, 128], BF16)
    out_d = nc.dram_tensor("out", [2, SEQ, D], F32, kind="ExternalOutput").ap()
    dk = dict(kind="ExternalOutput") if DEBUG else {}
    kspec_d = nc.dram_tensor("kspec", [2, 2, T, DH], F32, **dk).ap()
    h1_d = nc.dram_tensor("h1s", [2 * T, D], F32, **dk).ap()
    xs_d = nc.dram_tensor("xsort", [NSLOT, D], BF16, **dk).ap()
    y_d = nc.dram_tensor("ysort", [NSLOT, D], F32, **dk).ap()
    wgb_d = nc.dram_tensor("wg_bf", [NE, 128, 8, 512], BF16).ap()
    wub_d = nc.dram_tensor("wu_bf", [NE, 128, 8, 512], BF16).ap()
    wdb_d = nc.dram_tensor("wd_bf", [NE, 128, 4, D], BF16).ap()
    if DEBUG:
        dbg_mix = nc.dram_tensor("dbg_mix", [2, 128, 8, T], BF16, kind="ExternalOutput").ap()
        dbg_zx = nc.dram_tensor("dbg_zx", [2, 3, 128, NT, DH], BF16, kind="ExternalOutput").ap()
        dbg_aT = nc.dram_tensor("dbg_aT", [2, 128, 8, T], BF16, kind="ExternalOutput").ap()
        dbg_sg = nc.dram_tensor("dbg_sg", [128, NTT, 4], F32, kind="ExternalOutput").ap()

    import contextlib
    st = contextlib.ExitStack()
    with st:
        big = st.enter_context(nc.sbuf_tensor("arena", [128, NWORDS], F32))
        psb = [st.enter_context(nc.psum_tensor("ps%d" % i, [128, 512], F32)) for i in range(8)]
        S = Sched(nc)
        o0 = 0
        AC = Arena(big[:, o0:o0 + 8320], 8320); o0 += 8320
        A1 = Arena(big[:, o0:o0 + 8704], 8704); o0 += 8704
        A2 = Arena(big[:, o0:o0 + 8960], 8960); o0 += 8960
        AR = Arena(big[:, o0:NWORDS], NWORDS - o0)

        ps_tok = [Tk("ps%d" % i) for i in range(8)]
        ps_rr = [0]

        def PS(dtype=F32):
            i = ps_rr[0]
            ps_rr[0] = (i + 1) % 8
            ap = psb[i][:]
            if dtype != F32:
                ap = ap.bitcast(dtype)
            return ap, ps_tok[i]

        def new_phase():
            S.barrier()
            for a in (A1, A2, AR):
                a.release(0)

        class B:
            def __init__(self, arena, shape, dtype=F32, name=""):
                self.ap = arena.alloc(shape, dtype)
                self.t = Tk(name)

        regc = {}

        def bcreg(e):
            if "r" not in regc:
                regc["r"] = e.to_reg(NSLOT - 1)
            return regc["r"]

        def dma(q, out, in_, r=(), w=(), grp=0):
            S.op(q, lambda e: e.dma_start(out=out, in_=in_), r=r, w=w, dma=True, grp=grp)

        conv_tok = [[Tk(), Tk(), Tk()] for _ in range(NE)]
        conv_jobs = []
        for ex_ in range(NE):
            conv_jobs.append((wgb_d[ex_], wg_d[ex_].rearrange("(dc p) c -> p dc c", p=128), conv_tok[ex_][0]))
            conv_jobs.append((wub_d[ex_], wu_d[ex_].rearrange("(dc p) c -> p dc c", p=128), conv_tok[ex_][1]))
            conv_jobs.append((wdb_d[ex_], wd_d[ex_].rearrange("(fc p) c -> p fc c", p=128), conv_tok[ex_][2]))
        conv_pos = [0]

        def conv_some(n):
            return
            while n > 0 and conv_pos[0] < len(conv_jobs):
                o_, i_, tk_c = conv_jobs[conv_pos[0]]
                conv_pos[0] += 1
                n -= 1
                dma("pool", o_, i_, w=[tk_c], grp=1)

        def rstd_from_ss(ss, rs, n, npart, tk):
            S.op("act", lambda e: e.activation(out=rs[:npart], in_=ss[:npart], func=AF.Sqrt, scale=1.0 / n, bias=epsb.ap[:npart, 0:1]), r=[tk, epsb.t], w=[tk])
            S.op("dve", lambda e: e.reciprocal(out=rs[:npart], in_=rs[:npart]), r=[tk], w=[tk])

        def sumsq(src_ap, junk_ap, ss_ap, r, jt, st):
            S.op("act", lambda e: e.activation(out=junk_ap, in_=src_ap, func=AF.Square), r=r, w=[jt])
            S.op("dve", lambda e: e.tensor_reduce(out=ss_ap, in_=junk_ap, axis=AX.X, op=OP.add), r=[jt], w=[st])

        misc = B(AC, [1024]); dma("sp", misc.ap, misc_d, w=[misc.t])
        ident = misc.ap[:, 0:128]
        rows = B(AC, [5120]); dma("sp", rows.ap, rows_d[:, 0:5120], w=[rows.t])
        g_mix, g_ffn, g_fin = rows.ap[:, 0:1024], rows.ap[:, 1024:2048], rows.ap[:, 2048:3072]
        g_hy, g_hg = rows.ap[:, 3072:3584], rows.ap[:, 3584:4096]
        skipB = [rows.ap[:, 4096:4608], rows.ap[:, 4608:5120]]
        convw = B(AC, [12, 4]); dma("sp", convw.ap, convw_d, w=[convw.t])
        lbr = B(AC, [2, 2, 4]); dma("sp", lbr.ap, lb_d, w=[lbr.t])
        wr = B(AC, [8, 8 + NE]); dma("sp", wr.ap, wr_d, w=[wr.t])
        identb = B(AC, [128], BF16)
        S.op("dve", lambda e: e.tensor_copy(out=identb.ap, in_=ident), r=[misc.t], w=[identb.t])
        maskb = B(AC, [2, 128], BF16)
        S.op("dve", lambda e: e.tensor_copy(out=maskb.ap, in_=misc.ap[:, 128:384].rearrange("p (a b) -> p a b", b=128)), r=[misc.t], w=[maskb.t])
        ustrict = misc.ap[:, 384:512]
        ones = misc.ap[:, 512:640]
        iota64 = misc.ap[:, 640:704]
        iota8 = misc.ap[:, 704:712]
        iotap = misc.ap[:, 712:713]
        epsb = B(AC, [1])
        onesT = B(AC, [512])
        S.op("pool", lambda e: e.memset(onesT.ap, 1.0), w=[onesT.t])
        S.op("pool", lambda e: e.memset(epsb.ap, EPS), w=[epsb.t])
        lb = B(AC, [2, 4]); oml = B(AC, [2, 4])
        S.op("dve", lambda e: e.tensor_tensor(out=lb.ap, in0=lbr.ap[:, :, 0, :], in1=lbr.ap[:, :, 1, :], op=OP.subtract), r=[lbr.t], w=[lb.t])
        S.op("act", lambda e: e.activation(out=lb.ap, in_=lb.ap, func=AF.Sigmoid), r=[lb.t], w=[lb.t])
        S.op("dve", lambda e: e.tensor_scalar(out=oml.ap, in0=lb.ap, scalar1=-1.0, scalar2=1.0, op0=OP.mult, op1=OP.add), r=[lb.t], w=[oml.t])
        slots = B(AC, [NTT, 2], I32)
        gates = B(AC, [NTT, 2])
        S.op("pool", lambda e: e.memset(slots.ap, 0), w=[slots.t])
        S.op("pool", lambda e: e.memset(gates.ap, 0.0), w=[gates.t])
        cnt = B(AC, [NE])
        S.op("pool", lambda e: e.memset(cnt.ap, 0.0), w=[cnt.t])
        slotbase = B(AC, [NE])
        S.op("dve", lambda e: e.tensor_scalar(out=slotbase.ap, in0=iota64[:, 0:NE], scalar1=float(CAP), scalar2=None, op0=OP.mult), r=[misc.t], w=[slotbase.t])
        zero_bf = B(AC, [1024], BF16)
        S.op("pool", lambda e: e.memset(zero_bf.ap, 0.0), w=[zero_bf.t])
        t_xs = Tk("xsort"); t_y = Tk("ysort"); t_h1 = Tk("h1s"); t_ks = Tk("kspec")
        for r0 in range(0, NSLOT, 128):
            dma("sp", xs_d[r0:r0 + 128, :], zero_bf.ap, r=[zero_bf.t], w=[t_xs])

        def wrap(buf, tmp, npart):
            for _ in range(2):
                S.op("dve", lambda e: e.tensor_scalar(out=tmp.ap[:npart], in0=buf.ap[:npart], scalar1=math.pi, scalar2=-2 * math.pi, op0=OP.is_gt, op1=OP.mult), r=[buf.t], w=[tmp.t])
                S.op("dve", lambda e: e.tensor_tensor(out=buf.ap[:npart], in0=buf.ap[:npart], in1=tmp.ap[:npart], op=OP.add), r=[tmp.t, buf.t], w=[buf.t])
                S.op("dve", lambda e: e.tensor_scalar(out=tmp.ap[:npart], in0=buf.ap[:npart], scalar1=-math.pi, scalar2=2 * math.pi, op0=OP.is_lt, op1=OP.mult), r=[buf.t], w=[tmp.t])
                S.op("dve", lambda e: e.tensor_tensor(out=buf.ap[:npart], in0=buf.ap[:npart], in1=tmp.ap[:npart], op=OP.add), r=[tmp.t, buf.t], w=[buf.t])

        zT = B(A2, [T]); dma("sp", zT.ap[:33], zfeat_d, w=[zT.t])
        fw1 = B(A1, [64]); dma("sp", fw1.ap[:33], f1_d, w=[fw1.t])
        fw2 = B(A1, [64]); dma("sp", fw2.ap[:64], f2_d, w=[fw2.t])
        fw3 = B(A2, [2048]); dma("sp", fw3.ap[:64], f3_d, w=[fw3.t])
        fbb = B(A1, [4]); dma("sp", fbb.ap[:64], fb_d, w=[fbb.t])
        hT = [B(A1, [T]), B(A1, [T])]
        tmpw = B(A1, [T])
        slabs = [(s0, min(512, T - s0)) for s0 in range(0, T, 512)]
        for layer in range(2):
            src = zT if layer == 0 else hT[0]
            kk = 33 if layer == 0 else 64
            wl = fw1 if layer == 0 else fw2
            dst = hT[layer]
            for (s0, sn) in slabs:
                pa, pt = PS()
                S.op("pe", lambda e, pa=pa, s0=s0, sn=sn: e.matmul(pa[:64, 0:sn], lhsT=wl.ap[:kk, 0:64], rhs=src.ap[:kk, s0:s0 + sn], start=True, stop=True), r=[wl.t, src.t], w=[pt])
                S.op("dve", lambda e, pa=pa, s0=s0, sn=sn: e.tensor_scalar(out=dst.ap[:64, s0:s0 + sn], in0=pa[:64, 0:sn], scalar1=fbb.ap[:64, layer:layer + 1], scalar2=fbb.ap[:64, 2:3], op0=OP.add, op1=OP.mult), r=[pt, fbb.t], w=[dst.t])
            wrap(dst, tmpw, 64)
            S.op("act", lambda e: e.activation(out=dst.ap[:64], in_=dst.ap[:64], func=AF.Sin), r=[dst.t], w=[dst.t])
        h2T = hT[1]
        hs = B(AR, [NT, 1024], BF16); hdd = B(AR, [NT, 1024], BF16)
        mk0 = AR.mark()
        wtile = [[B(AR, [DH]), B(AR, [DH])] for _ in range(2)]
        ftmp = [B(AR, [DH]) for _ in range(4)]
        for i in range(NT):
            wf, wb_ = wtile[i % 2]
            dma("sp", wf.ap, winf_d[:, i, :], w=[wf.t])
            dma("sp", wb_.ap, winb_d[:, i, :], w=[wb_.t])
            for o in range(2):
                pf, ptf = PS(); pb, ptb = PS()
                S.op("pe", lambda e, pf=pf: e.matmul(pf, lhsT=h2T.ap[:64, i * 128:(i + 1) * 128], rhs=fw3.ap[:64, o * 512:(o + 1) * 512], start=True, stop=True), r=[h2T.t, fw3.t], w=[ptf])
                S.op("pe", lambda e, pb=pb: e.matmul(pb, lhsT=h2T.ap[:64, i * 128:(i + 1) * 128], rhs=fw3.ap[:64, 1024 + o * 512:1024 + (o + 1) * 512], start=True, stop=True), r=[h2T.t, fw3.t], w=[ptb])
                ta, tb = ftmp[2 * o], ftmp[2 * o + 1]
                S.op("dve", lambda e, pf=pf, ta=ta: e.tensor_tensor(out=ta.ap, in0=pf, in1=wf.ap, op=OP.mult), r=[ptf, wf.t], w=[ta.t])
                S.op("dve", lambda e, pb=pb, tb=tb: e.tensor_tensor(out=tb.ap, in0=pb, in1=wb_.ap, op=OP.mult), r=[ptb, wb_.t], w=[tb.t])
                S.op("pool", lambda e, ta=ta, tb=tb: e.tensor_tensor(out=hs.ap[:, i, o * 512:(o + 1) * 512], in0=ta.ap, in1=tb.ap, op=OP.add), r=[ta.t, tb.t], w=[hs.t])
                S.op("pool", lambda e, ta=ta, tb=tb: e.tensor_tensor(out=hdd.ap[:, i, o * 512:(o + 1) * 512], in0=ta.ap, in1=tb.ap, op=OP.subtract), r=[ta.t, tb.t], w=[hdd.t])
        S.barrier()
        AR.release(mk0)
        dftb = [[B(AR, [NT, 128], BF16), B(AR, [NT, 128], BF16)] for _ in range(2)]
        kout = [B(AR, [DH]) for _ in range(4)]
        dma("sp", dftb[0][0].ap, cc_d[0], w=[dftb[0][0].t])
        dma("sp", dftb[0][1].ap, cs_d[0], w=[dftb[0][1].t])
        for j in range(NT):
            cb, sb_ = dftb[j % 2]
            if j + 1 < NT:
                dma("sp", dftb[(j + 1) % 2][0].ap, cc_d[j + 1], w=[dftb[(j + 1) % 2][0].t])
                dma("sp", dftb[(j + 1) % 2][1].ap, cs_d[j + 1], w=[dftb[(j + 1) % 2][1].t])
            for o in range(2):
                for ri, (mat, src) in enumerate(((cb, hs), (sb_, hdd))):
                    pa, pt = PS()
                    for tc_ in range(NT):
                        S.op("pe", lambda e, pa=pa, mat=mat, src=src, tc_=tc_: e.matmul(pa, lhsT=mat.ap[:, tc_, :], rhs=src.ap[:, tc_, o * 512:(o + 1) * 512], start=(tc_ == 0), stop=(tc_ == NT - 1)), r=[mat.t, src.t], w=[pt], silent=not (tc_ == NT - 1))
                    ko = kout[2 * o + ri]
                    S.op("act", lambda e, pa=pa, ko=ko: e.activation(out=ko.ap, in_=pa, func=AF.Copy, scale=2.0 / NDFT), r=[pt], w=[ko.t])
                    dma("sp", kspec_d[o, ri, j * 128:(j + 1) * 128, :], ko.ap, r=[ko.t], w=[t_ks])

        def load_h_tile(q, dst, s, i, npart):
            if i == 0:
                dma(q, dst.ap[0:N_META], meta_d, w=[dst.t])
                dma(q, dst.ap[N_META:128], x_d[s, 0:128 - N_META, :], w=[dst.t])
            else:
                r0 = i * 128 - N_META
                dma(q, dst.ap[:npart], x_d[s, r0:r0 + npart, :], w=[dst.t])

        def wload(arena_buf, c0, ncols):
            dma("pool", arena_buf.ap[:, :, 0:ncols], w_in_d.rearrange("(dc p) c -> p dc c", p=128)[:, :, c0:c0 + ncols], w=[arena_buf.t])

        for s in range(2):
            new_phase()
            mixT = B(A1, [8, T], BF16)
            aT = B(A2, [8, T], BF16)
            if LAST < 128:
                S.op("pool", lambda e: e.memset(aT.ap[:, :, (NT - 1) * 128 + LAST:T], 0.0), w=[aT.t])
            m0 = AR.mark()
            hin = [B(AR, [D]) for _ in range(2)]
            abf = [B(AR, [D], BF16) for _ in range(2)]
            junk = B(AR, [D])
            ssb = [B(AR, [2]) for _ in range(2)]
            for i in range(NT):
                npart = 128 if i < NT - 1 else LAST
                hi, ab, ss = hin[i % 2], abf[i % 2], ssb[i % 2]
                load_h_tile("sp", hi, s, i, npart)
                sumsq(hi.ap[:npart], junk.ap[:npart], ss.ap[:npart, 0:1], [hi.t], junk.t, ss.t)
                rstd_from_ss(ss.ap[:, 0:1], ss.ap[:, 1:2], D, npart, ss.t)
                S.op("dve", lambda e, hi=hi, ss=ss, ab=ab: e.scalar_tensor_tensor(out=ab.ap[:npart], in0=hi.ap[:npart], scalar=ss.ap[:npart, 1:2], in1=g_mix[:npart], op0=OP.mult, op1=OP.mult), r=[hi.t, ss.t, rows.t], w=[ab.t])
                pa, pt = PS(BF16)
                for dc in range(8):
                    S.op("pe", lambda e, pa=pa, ab=ab, dc=dc: e.transpose(out=pa[:, dc * 128:dc * 128 + npart], in_=ab.ap[:npart, dc * 128:(dc + 1) * 128], identity=identb.ap[:npart, :npart]), r=[ab.t, identb.t], w=[pt])
                S.op("act", lambda e, pa=pa: e.activation(out=aT.ap[:, :, i * 128:i * 128 + npart], in_=pa.rearrange("p (a b) -> p a b", b=128)[:, :, 0:npart], func=AF.Copy), r=[pt], w=[aT.t])
            AR.release(m0)

            S.barrier()
            m0 = AR.mark()
            wch = [B(AR, [8, 128], BF16) for _ in range(3)]
            qT = B(AR, [T], BF16); kTd = B(AR, [T], BF16)
            PTe = B(AR, [T + 128])
            lf = B(AR, [T]); sgt = B(AR, [T])
            V = B(AR, [NT, 128], BF16); sgate = B(AR, [NT, 128], BF16); oacc = B(AR, [NT, 128])
            cs3 = [[B(AR, [NT]) for _ in range(4)] for _ in range(2)]
            Sst = [B(AR, [128]), B(AR, [128])]
            KtT1 = B(AR, [T], BF16)
            QtA = [B(AR, [T], BF16) for _ in range(2)]
            sTA = [B(AR, [NT, 128], BF16) for _ in range(2)]
            KtA = [B(AR, [NT, 128], BF16) for _ in range(2)]
            Spb = [B(AR, [128], BF16) for _ in range(2)]
            tmpb = [B(AR, [128]) for _ in range(2)]
            fins = [dict(junk=B(AR, [128]), ss=B(AR, [2]), yb=B(AR, [128], BF16)) for _ in range(2)]
            QhA = [B(AR, [T], BF16) for _ in range(2)]
            onesT2 = B(AR, [T], BF16)
            S.op("pool", lambda e: e.memset(onesT2.ap, 1.0), w=[onesT2.t])
            lf3 = lf.ap.rearrange("p (c k) -> p c k", k=128)
            sg3 = sgt.ap.rearrange("p (c k) -> p c k", k=128)
            for hd in range(4):
                cq = 1536 + hd * 128
                for j, wb in enumerate(wch):
                    wload(wb, cq + j * 512, 128)
                for j in range(3):
                    d = j - 1
                    for (s0, sn) in slabs:
                        pa, pt = PS()
                        for dc in range(8):
                            S.op("pe", lambda e, pa=pa, dc=dc: e.matmul(pa[:, 0:sn], lhsT=wch[j].ap[:, dc, :], rhs=aT.ap[:, dc, s0:s0 + sn], start=(dc == 0), stop=(dc == 7)), r=[wch[j].t, aT.t], w=[pt], silent=not (dc == 7))
                        if j == 0:
                            S.op("act", lambda e, pa=pa: e.activation(out=qT.ap[:, s0:s0 + sn], in_=pa[:, 0:sn], func=AF.Silu), r=[pt], w=[qT.t])
                        else:
                            S.op("act", lambda e, pa=pa: e.activation(out=sgt.ap[:, s0:s0 + sn], in_=pa[:, 0:sn], func=AF.Sigmoid), r=[pt], w=[sgt.t])
                    if j > 0:
                        S.op("dve", lambda e: e.tensor_scalar(out=sgt.ap, in0=sgt.ap, scalar1=oml.ap[:, d, hd:hd + 1], scalar2=lb.ap[:, d, hd:hd + 1], op0=OP.mult, op1=OP.add), r=[sgt.t, oml.t, lb.t], w=[sgt.t])
                        S.op("pool", lambda e: e.tensor_scalar(out=kTd.ap, in0=sgt.ap, scalar1=-1.0, scalar2=1.0, op0=OP.mult, op1=OP.add), r=[sgt.t], w=[kTd.t])
                        S.op("act", lambda e: e.activation(out=lf.ap, in_=sgt.ap, func=AF.Ln), r=[sgt.t], w=[lf.t])
                        S.op("pool", lambda e: e.memset(PTe.ap[:, 0:1], 0.0), w=[PTe.t])
                        S.op("dve", lambda e: e.tensor_tensor_scan(out=PTe.ap[:, 1:1 + T], data0=onesT2.ap, data1=lf.ap, initial=0.0, op0=OP.mult, op1=OP.add), r=[lf.t, onesT2.t, PTe.t], w=[PTe.t])
                    if j == 0:
                        continue
                    P3 = PTe.ap[:, 0:T].rearrange("p (c k) -> p c k", k=128)
                    Z3 = PTe.ap[:, 128:T + 128].rearrange("p (c k) -> p c k", k=128)
                    Acol, Zcol = P3[:, :, 0], Z3[:, :, 0]
                    Mcol = P3[:, :, 65] if d == 0 else P3[:, :, 64]
                    M, c1, c2, dec = cs3[d]
                    S.op("dve", lambda e: e.tensor_copy(out=M.ap, in_=Mcol), r=[PTe.t], w=[M.t])
                    e1, e2 = (c1, c2) if d == 0 else (c2, c1)
                    S.op("dve", lambda e: e.tensor_tensor(out=e1.ap, in0=M.ap, in1=Acol, op=OP.subtract), r=[M.t, PTe.t], w=[e1.t])
                    S.op("dve", lambda e: e.tensor_tensor(out=e2.ap, in0=Zcol, in1=M.ap, op=OP.subtract), r=[M.t, PTe.t], w=[e2.t])
                    S.op("dve", lambda e: e.tensor_tensor(out=dec.ap, in0=Zcol, in1=Acol, op=OP.subtract), r=[PTe.t], w=[dec.t])
                    for bb in (c1, c2, dec):
                        S.op("act", lambda e, bb=bb: e.activation(out=bb.ap, in_=bb.ap, func=AF.Exp), r=[bb.t], w=[bb.t])
                    off = 1 if d == 0 else 0
                    Pv = PTe.ap[:, off:off + T].rearrange("p (c k) -> p c k", k=128)
                    S.op("dve", lambda e: e.tensor_tensor(out=lf3, in0=Pv, in1=M.ap.unsqueeze(2).to_broadcast([128, NT, 128]), op=OP.subtract), r=[PTe.t, M.t, lf.t], w=[lf.t])
                    sq = 1.0 if d == 0 else -1.0
                    S.op("act", lambda e: e.activation(out=sgt.ap, in_=lf.ap, func=AF.Exp, scale=sq), r=[lf.t, sgt.t], w=[sgt.t])
                    S.op("dve", lambda e: e.tensor_tensor(out=QtA[d].ap, in0=qT.ap, in1=sgt.ap, op=OP.mult), r=[qT.t, sgt.t], w=[QtA[d].t])
                    S.op("act", lambda e: e.activation(out=lf.ap, in_=lf.ap, func=AF.Exp, scale=-sq), r=[lf.t], w=[lf.t])
                    S.op("pool", lambda e: e.tensor_tensor(out=KtT1.ap, in0=kTd.ap, in1=lf.ap, op=OP.mult), r=[kTd.t, lf.t], w=[KtT1.t])
                    for c0 in range(0, NT, 4):
                        n4 = min(4, NT - c0)
                        p1, t1 = PS()
                        for k in range(n4):
                            cs_ = slice((c0 + k) * 128, (c0 + k + 1) * 128)
                            S.op("pe", lambda e, p1=p1, k=k, cs_=cs_: e.matmul(p1[:, k * 128:(k + 1) * 128], lhsT=KtT1.ap[:, cs_], rhs=QtA[d].ap[:, cs_], start=True, stop=True), r=[KtT1.t, QtA[d].t], w=[t1])
                        S.op("dve", lambda e, p1=p1: e.tensor_tensor(out=sTA[d].ap[:, c0:c0 + n4, :], in0=p1[:, 0:n4 * 128].rearrange("p (a b) -> p a b", b=128), in1=maskb.ap[:, d:d + 1, :].to_broadcast([128, n4, 128]), op=OP.mult), r=[t1, maskb.t], w=[sTA[d].t])
                    S.op("pool", lambda e: e.tensor_tensor(out=QhA[d].ap.rearrange("p (c k) -> p c k", k=128), in0=QtA[d].ap.rearrange("p (c k) -> p c k", k=128), in1=c1.ap.unsqueeze(2).to_broadcast([128, NT, 128]), op=OP.mult), r=[QtA[d].t, c1.t], w=[QhA[d].t])
                    S.op("pool", lambda e: e.tensor_tensor(out=KtT1.ap.rearrange("p (c k) -> p c k", k=128), in0=KtT1.ap.rearrange("p (c k) -> p c k", k=128), in1=c2.ap.unsqueeze(2).to_broadcast([128, NT, 128]), op=OP.mult), r=[KtT1.t, c2.t], w=[KtT1.t])
                    for c0 in range(0, NT, 8):
                        n8 = min(8, NT - c0)
                        p2, t2 = PS(BF16)
                        for k in range(n8):
                            cs_ = slice((c0 + k) * 128, (c0 + k + 1) * 128)
                            S.op("pe", lambda e, p2=p2, k=k, cs_=cs_: e.transpose(out=p2[:, k * 128:(k + 1) * 128], in_=KtT1.ap[:, cs_], identity=identb.ap), r=[KtT1.t, identb.t], w=[t2])
                        S.op("act", lambda e, p2=p2: e.activation(out=KtA[d].ap[:, c0:c0 + n8, :], in_=p2[:, 0:n8 * 128].rearrange("p (a b) -> p a b", b=128), func=AF.Copy), r=[t2], w=[KtA[d].t])
                wload(wch[0], cq + 3 * 512, 128)
                wload(wch[1], cq + 4 * 512, 128)
                for i0 in range(0, NT, 4):
                    n4 = min(4, NT - i0)
                    for j in range(2):
                        pa, pt = PS()
                        for k in range(n4):
                            i = i0 + k
                            for dc in range(8):
                                S.op("pe", lambda e, pa=pa, k=k, i=i, dc=dc: e.matmul(pa[:, k * 128:(k + 1) * 128], lhsT=aT.ap[:, dc, i * 128:(i + 1) * 128], rhs=wch[j].ap[:, dc, :], start=(dc == 0), stop=(dc == 7)), r=[wch[j].t, aT.t], w=[pt], silent=not (dc == 7))
                        pv = pa[:, 0:n4 * 128].rearrange("p (a b) -> p a b", b=128)
                        if j == 0:
                            S.op("act", lambda e, pv=pv: e.activation(out=V.ap[:, i0:i0 + n4, :], in_=pv, func=AF.Copy), r=[pt], w=[V.t])
                        else:
                            S.op("act", lambda e, pv=pv: e.activation(out=sgate.ap[:, i0:i0 + n4, :], in_=pv, func=AF.Silu), r=[pt], w=[sgate.t])
                S.op("pool", lambda e: e.tensor_tensor(out=sgate.ap, in0=sgate.ap, in1=g_hg[:, hd * 128:(hd + 1) * 128].unsqueeze(1).to_broadcast([128, NT, 128]), op=OP.mult), r=[sgate.t, rows.t], w=[sgate.t])
                S.op("pool", lambda e: e.memset(oacc.ap, 0.0), w=[oacc.t])
                for d in range(2):
                    S.op("pool", lambda e, d=d: e.memset(Sst[d].ap, 0.0), w=[Sst[d].t])
                for step in range(NT):
                    for d in range(2):
                        c = step if d == 0 else NT - 1 - step
                        M, c1, c2, dec = cs3[d]
                        cs_ = slice(128 * c, 128 * c + 128)
                        S.op("act", lambda e: e.activation(out=Spb[d].ap, in_=Sst[d].ap, func=AF.Copy), r=[Sst[d].t], w=[Spb[d].t])
                        p4, t4 = PS()
                        S.op("pe", lambda e, p4=p4: e.matmul(p4[:, 0:128], lhsT=KtA[d].ap[:, c, :], rhs=V.ap[:, c, :], start=True, stop=True), r=[KtA[d].t, V.t], w=[t4])
                        p3, t3 = PS()
                        S.op("pe", lambda e, p3=p3: e.matmul(p3[:, 0:128], lhsT=sTA[d].ap[:, c, :], rhs=V.ap[:, c, :], start=True, stop=False), r=[sTA[d].t, V.t], w=[t3], silent=not False)
                        S.op("pe", lambda e, p3=p3: e.matmul(p3[:, 0:128], lhsT=QhA[d].ap[:, cs_], rhs=Spb[d].ap, start=False, stop=True), r=[QhA[d].t, Spb[d].t], w=[t3])
                        S.op("dve", lambda e, p4=p4: e.scalar_tensor_tensor(out=Sst[d].ap, in0=Sst[d].ap, scalar=dec.ap[:, c:c + 1], in1=p4[:, 0:128], op0=OP.mult, op1=OP.add), r=[t4, dec.t, Sst[d].t], w=[Sst[d].t])
                        S.op("dve", lambda e, p3=p3: e.tensor_tensor(out=oacc.ap[:, c, :], in0=p3[:, 0:128], in1=oacc.ap[:, c, :], op=OP.add), r=[t3, oacc.t], w=[oacc.t])
                for i in range(NT):
                    fin = fins[i % 2]
                    sumsq(oacc.ap[:, i, :], fin["junk"].ap, fin["ss"].ap[:, 0:1], [oacc.t], fin["junk"].t, fin["ss"].t)
                    rstd_from_ss(fin["ss"].ap[:, 0:1], fin["ss"].ap[:, 1:2], 128, 128, fin["ss"].t)
                    S.op("dve", lambda e: e.scalar_tensor_tensor(out=fin["yb"].ap, in0=oacc.ap[:, i, :], scalar=fin["ss"].ap[:, 1:2], in1=sgate.ap[:, i, :], op0=OP.mult, op1=OP.mult), r=[oacc.t, fin["ss"].t, sgate.t], w=[fin["yb"].t])
                    pa, pt = PS(BF16)
                    S.op("pe", lambda e, pa=pa: e.transpose(out=pa[:, 0:128], in_=fin["yb"].ap, identity=identb.ap), r=[fin["yb"].t, identb.t], w=[pt])
                    S.op("act", lambda e, pa=pa: e.activation(out=mixT.ap[:, 4 + hd, i * 128:(i + 1) * 128], in_=pa[:, 0:128], func=AF.Copy), r=[pt], w=[mixT.t])
            AR.release(m0)

            S.barrier()
            m0 = AR.mark()
            zx = [B(AR, [NT, DH], BF16, "zx%d" % k) for k in range(3)]
            m1 = AR.mark()
            wch = [B(AR, [8, 128], BF16) for _ in range(2)]
            pbuf = [B(AR, [T + 8]) for _ in range(2)]
            ub = B(AR, [T]); uT = [B(AR, [T], BF16) for _ in range(2)]
            for pb_ in pbuf:
                S.op("pool", lambda e, pb_=pb_: e.memset(pb_.ap[:, 0:1], 0.0), w=[pb_.t])
                S.op("pool", lambda e, pb_=pb_: e.memset(pb_.ap[:, T + 1:T + 2], 0.0), w=[pb_.t])
            for cc in range(12):
                wb, pb_, ut = wch[cc % 2], pbuf[cc % 2], uT[cc % 2]
                wload(wb, cc * 128, 128)
                for (s0, sn) in slabs:
                    pa, pt = PS()
                    for dc in range(8):
                        S.op("pe", lambda e, pa=pa, dc=dc: e.matmul(pa[:, 0:sn], lhsT=wb.ap[:, dc, :], rhs=aT.ap[:, dc, s0:s0 + sn], start=(dc == 0), stop=(dc == 7)), r=[wb.t, aT.t], w=[pt], silent=not (dc == 7))
                    S.op("act", lambda e, pa=pa: e.activation(out=pb_.ap[:, 1 + s0:1 + s0 + sn], in_=pa[:, 0:sn], func=AF.Copy), r=[pt], w=[pb_.t])
                S.op("act", lambda e: e.activation(out=ub.ap, in_=pb_.ap[:, 1:T + 1], func=AF.Identity, scale=convw.ap[:, cc, 1:2], bias=convw.ap[:, cc, 3:4]), r=[pb_.t, convw.t], w=[ub.t])
                S.op("dve", lambda e: e.scalar_tensor_tensor(out=ub.ap, in0=pb_.ap[:, 0:T], scalar=convw.ap[:, cc, 0:1], in1=ub.ap, op0=OP.mult, op1=OP.add), r=[pb_.t, convw.t, ub.t], w=[ub.t])
                S.op("dve", lambda e: e.scalar_tensor_tensor(out=ut.ap, in0=pb_.ap[:, 2:T + 2], scalar=convw.ap[:, cc, 2:3], in1=ub.ap, op0=OP.mult, op1=OP.add), r=[pb_.t, convw.t, ub.t], w=[ut.t])
                dst = zx[cc // 4]
                cq = (cc % 4) * 128
                for i0 in range(0, NT, 8):
                    n8 = min(8, NT - i0)
                    pa, pt = PS(BF16)
                    for k in range(n8):
                        S.op("pe", lambda e, pa=pa, k=k: e.transpose(out=pa[:, k * 128:(k + 1) * 128], in_=ut.ap[:, (i0 + k) * 128:(i0 + k + 1) * 128], identity=identb.ap), r=[ut.t, identb.t], w=[pt])
                    S.op("act", lambda e, pa=pa: e.activation(out=dst.ap[:, i0:i0 + n8, cq:cq + 128], in_=pa[:, 0:n8 * 128].rearrange("p (a b) -> p a b", b=128), func=AF.Copy), r=[pt], w=[dst.t])
            AR.release(m1)

            if DEBUG:
                for k in range(3):
                    dma("sp", dbg_zx[s, k], zx[k].ap, r=[zx[k].t])
                dma("sp", dbg_aT[s], aT.ap, r=[aT.t])
            S.barrier()
            A2.release(0)
            YR = B(A2, [NT, DH], BF16); YS = B(A2, [NT, DH], BF16)
            dftb = [[B(AR, [NT, 128], BF16), B(AR, [NT, 128], BF16)] for _ in range(2)]
            kb = [[B(AR, [DH]), B(AR, [DH])] for _ in range(2)]
            tq = [B(AR, [DH]) for _ in range(4)]
            sk_t = B(AR, [DH]); tsum = B(AR, [DH]); yh = B(AR, [DH]); yn = B(AR, [DH], BF16)
            junk = B(AR, [DH]); ssh = B(AR, [2])
            zin = zx[0]
            for o in range(2):
                gate = zx[1 + o]
                for j in range(NT):
                    conv_some(1)
                    cb, sb_ = dftb[j % 2]
                    kr, ks = kb[j % 2]
                    dma("sp", cb.ap, cc_d[j], w=[cb.t])
                    dma("sp", sb_.ap, cs_d[j], w=[sb_.t])
                    dma("sp", kr.ap, kspec_d[o, 0, j * 128:(j + 1) * 128, :], r=[t_ks], w=[kr.t])
                    dma("sp", ks.ap, kspec_d[o, 1, j * 128:(j + 1) * 128, :], r=[t_ks], w=[ks.t])
                    pr, tr = PS(); pq, tq_ = PS()
                    for tc_ in range(NT):
                        S.op("pe", lambda e, pr=pr, tc_=tc_: e.matmul(pr, lhsT=cb.ap[:, tc_, :], rhs=zin.ap[:, tc_, :], start=(tc_ == 0), stop=(tc_ == NT - 1)), r=[cb.t, zin.t], w=[tr], silent=not (tc_ == NT - 1))
                    for tc_ in range(NT):
                        S.op("pe", lambda e, pq=pq, tc_=tc_: e.matmul(pq, lhsT=sb_.ap[:, tc_, :], rhs=zin.ap[:, tc_, :], start=(tc_ == 0), stop=(tc_ == NT - 1)), r=[sb_.t, zin.t], w=[tq_], silent=not (tc_ == NT - 1))
                    S.op("dve", lambda e, pr=pr: e.tensor_tensor(out=tq[0].ap, in0=pr, in1=kr.ap, op=OP.mult), r=[tr, kr.t], w=[tq[0].t])
                    S.op("dve", lambda e, pq=pq: e.tensor_tensor(out=tq[1].ap, in0=pq, in1=ks.ap, op=OP.mult), r=[tq_, ks.t], w=[tq[1].t])
                    S.op("pool", lambda e: e.tensor_tensor(out=YR.ap[:, j, :], in0=tq[0].ap, in1=tq[1].ap, op=OP.subtract), r=[tq[0].t, tq[1].t], w=[YR.t])
                    S.op("dve", lambda e, pr=pr: e.tensor_tensor(out=tq[2].ap, in0=pr, in1=ks.ap, op=OP.mult), r=[tr, ks.t], w=[tq[2].t])
                    S.op("dve", lambda e, pq=pq: e.tensor_tensor(out=tq[3].ap, in0=pq, in1=kr.ap, op=OP.mult), r=[tq_, kr.t], w=[tq[3].t])
                    S.op("pool", lambda e: e.tensor_tensor(out=YS.ap[:, j, :], in0=tq[2].ap, in1=tq[3].ap, op=OP.add), r=[tq[2].t, tq[3].t], w=[YS.t])
                for i in range(NT):
                    conv_some(2)
                    cb, sb_ = dftb[i % 2]
                    dma("sp", cb.ap, cct_d[i], w=[cb.t])
                    dma("sp", sb_.ap, cst_d[i], w=[sb_.t])
                    pa, pt = PS()
                    for fc in range(NT):
                        S.op("pe", lambda e, pa=pa, fc=fc: e.matmul(pa, lhsT=cb.ap[:, fc, :], rhs=YR.ap[:, fc, :], start=(fc == 0), stop=False), r=[cb.t, YR.t], w=[pt], silent=not False)
                    for fc in range(NT):
                        S.op("pe", lambda e, pa=pa, fc=fc: e.matmul(pa, lhsT=sb_.ap[:, fc, :], rhs=YS.ap[:, fc, :], start=False, stop=(fc == NT - 1)), r=[sb_.t, YS.t], w=[pt], silent=not (fc == NT - 1))
                    S.op("pool", lambda e: e.tensor_tensor(out=sk_t.ap, in0=zin.ap[:, i, :], in1=skipB[o], op=OP.mult), r=[zin.t, rows.t], w=[sk_t.t])
                    S.op("dve", lambda e, pa=pa: e.tensor_tensor(out=tsum.ap, in0=pa, in1=sk_t.ap, op=OP.add), r=[pt, sk_t.t], w=[tsum.t])
                    if o == 0:
                        S.op("pool", lambda e: e.tensor_tensor(out=gate.ap[:, i, :], in0=tsum.ap, in1=gate.ap[:, i, :], op=OP.mult), r=[tsum.t, gate.t], w=[gate.t])
                    else:
                        S.op("pool", lambda e: e.tensor_tensor(out=yh.ap, in0=tsum.ap, in1=gate.ap[:, i, :], op=OP.mult), r=[tsum.t, gate.t], w=[yh.t])
                        sumsq(yh.ap, junk.ap, ssh.ap[:, 0:1], [yh.t], junk.t, ssh.t)
                        rstd_from_ss(ssh.ap[:, 0:1], ssh.ap[:, 1:2], DH, 128, ssh.t)
                        S.op("dve", lambda e: e.scalar_tensor_tensor(out=yn.ap, in0=yh.ap, scalar=ssh.ap[:, 1:2], in1=g_hy, op0=OP.mult, op1=OP.mult), r=[yh.t, ssh.t, rows.t], w=[yn.t])
                        pb2, pt2 = PS(BF16)
                        for k in range(4):
                            S.op("pe", lambda e, pb2=pb2, k=k: e.transpose(out=pb2[:, k * 128:(k + 1) * 128], in_=yn.ap[:, k * 128:(k + 1) * 128], identity=identb.ap), r=[yn.t, identb.t], w=[pt2])
                        S.op("act", lambda e, pb2=pb2: e.activation(out=mixT.ap[:, 0:4, i * 128:(i + 1) * 128], in_=pb2[:, 0:512].rearrange("p (a b) -> p a b", b=128), func=AF.Copy), r=[pt2], w=[mixT.t])
                zin = zx[1]
            AR.release(m0)

            if DEBUG:
                dma("sp", dbg_mix[s], mixT.ap, r=[mixT.t])
            S.barrier()
            A2.release(0)
            wo = B(A2, [8, D], BF16)
            dma("pool", wo.ap, w_out_d.rearrange("(dc p) c -> p dc c", p=128), w=[wo.t])
            m0 = AR.mark()
            hres = [B(AR, [D]) for _ in range(2)]
            h1 = [B(AR, [D]) for _ in range(2)]
            hf = [B(AR, [D]) for _ in range(2)]
            hfTs = [B(AR, [8, 128]) for _ in range(2)]
            ssr = [B(AR, [2]) for _ in range(2)]
            hfball = B(AR, [NT, D], BF16)
            lgall = B(AR, [NT, 8 + NE])
            S.op("pool", lambda e: e.memset(lgall.ap, 0.0), w=[lgall.t])
            for i in range(NT):
                npart = 128 if i < NT - 1 else LAST
                tix = s * NT + i
                hr, h1_, hf_, ss = hres[i % 2], h1[i % 2], hf[i % 2], ssr[i % 2]
                load_h_tile("sp", hr, s, i, npart)
                for half in range(2):
                    pa, pt = PS()
                    for cc in range(8):
                        S.op("pe", lambda e, pa=pa, cc=cc: e.matmul(pa[:npart], lhsT=mixT.ap[:, cc, i * 128:i * 128 + npart], rhs=wo.ap[:, cc, half * 512:(half + 1) * 512], start=(cc == 0), stop=(cc == 7)), r=[mixT.t, wo.t], w=[pt], silent=not (cc == 7))
                    S.op("dve", lambda e, pa=pa: e.tensor_tensor(out=h1_.ap[:npart, half * 512:(half + 1) * 512], in0=pa[:npart], in1=hr.ap[:npart, half * 512:(half + 1) * 512], op=OP.add), r=[pt, hr.t], w=[h1_.t])
                dma("sp", h1_d[s * T + i * 128:s * T + i * 128 + npart, :], h1_.ap[:npart], r=[h1_.t], w=[t_h1])
                hfT = hfTs[i % 2]
                sumsq(h1_.ap[:npart], hf_.ap[:npart], ss.ap[:npart, 0:1], [h1_.t], hf_.t, ss.t)
                rstd_from_ss(ss.ap[:, 0:1], ss.ap[:, 1:2], D, npart, ss.t)
                S.op("dve", lambda e: e.scalar_tensor_tensor(out=hf_.ap[:npart], in0=h1_.ap[:npart], scalar=ss.ap[:npart, 1:2], in1=g_ffn[:npart], op0=OP.mult, op1=OP.mult), r=[h1_.t, ss.t, rows.t], w=[hf_.t])
                S.op("pool", lambda e: e.tensor_copy(out=hfball.ap[:npart, i, :], in_=hf_.ap[:npart]), r=[hf_.t], w=[hfball.t])
                for q4 in range(2):
                    pa, pt = PS()
                    for k in range(4):
                        dc = q4 * 4 + k
                        S.op("pe", lambda e, pa=pa, k=k, dc=dc: e.transpose(out=pa[:, k * 128:k * 128 + npart], in_=hf_.ap[:npart, dc * 128:(dc + 1) * 128], identity=ident[:npart, :npart]), r=[hf_.t, misc.t], w=[pt])
                    S.op("act", lambda e, pa=pa: e.activation(out=hfT.ap[:, q4 * 4:q4 * 4 + 4, 0:npart], in_=pa.rearrange("p (a b) -> p a b", b=128)[:, :, 0:npart], func=AF.Copy), r=[pt], w=[hfT.t])
                pr, tr = PS()
                for dc in range(8):
                    S.op("pe", lambda e, pr=pr, dc=dc: e.matmul(pr[:npart, 0:8 + NE], lhsT=hfT.ap[:, dc, 0:npart], rhs=wr.ap[:, dc, :], start=(dc == 0), stop=(dc == 7)), r=[hfT.t, wr.t], w=[tr], silent=not (dc == 7))
                S.op("act", lambda e, pr=pr: e.activation(out=lgall.ap[:npart, i, :], in_=pr[:npart, 0:8 + NE], func=AF.Copy), r=[tr], w=[lgall.t])
            G = NG
            b2 = lambda: B(AR, [NT]).ap
            gmax, sume, pg, m1, m2, dd, sig, gidx, e1, e2, ei1, ei2 = (b2() for _ in range(12))
            rr = [b2(), b2()]
            b8 = lambda: B(AR, [NT, 8]).ap
            t8, ohg, el, el2, oh1, oh2 = (b8() for _ in range(6))
            O2 = B(AR, [NT, NE]).ap; Oa = B(AR, [NT, NE]).ap; rk = B(AR, [NT, NE]).ap; sv = B(AR, [NT, NE]).ap
            prod4 = B(A2, [NT, NE]).ap; O1 = B(A2, [NT, NE]).ap; csum = B(A2, [NT, NE]).ap; cum = B(A2, [NT, NE]).ap
            slf = B(AR, [NT, 2]).ap
            RT = Tk("route")
            iotaTE = B(AR, [NT, NE])
            for t_ in range(NT):
                S.op("pool", lambda e: e.tensor_copy(out=iotaTE.ap[:, t_, :], in_=iota64[:, 0:NE]), r=[misc.t], w=[iotaTE.t])
            valid = misc.ap[:, 720:720 + NT]
            bc = lambda a2, n: a2.unsqueeze(2).to_broadcast([128, NT, n])

            def dvb(fn, er=(), ew=()):
                S.op("dve", fn, r=[RT] + list(er), w=[RT] + list(ew))
            lg3 = lgall.ap[:, :, 0:G]
            le4 = lgall.ap[:, :, 8:8 + NE].rearrange("p t (g k) -> p t g k", k=8)
            dvb(lambda e: e.tensor_reduce(out=gmax, in_=lg3, axis=AX.X, op=OP.max), er=[lgall.t])
            dvb(lambda e: e.tensor_tensor(out=t8[:, :, 0:G], in0=lg3, in1=bc(gmax, G), op=OP.subtract), er=[lgall.t])
            dvb(lambda e: e.tensor_scalar(out=ohg[:, :, 0:G], in0=t8[:, :, 0:G], scalar1=0.0, scalar2=None, op0=OP.is_equal))
            S.op("act", lambda e: e.activation(out=t8[:, :, 0:G], in_=t8[:, :, 0:G], func=AF.Exp), r=[RT], w=[RT])
            dvb(lambda e: e.tensor_reduce(out=sume, in_=t8[:, :, 0:G], axis=AX.X, op=OP.add))
            dvb(lambda e: e.reciprocal(out=pg, in_=sume))
            p4v = prod4.rearrange("p t (g k) -> p t g k", k=8)
            dvb(lambda e: e.tensor_tensor(out=p4v, in0=le4, in1=ohg[:, :, 0:G].unsqueeze(3).to_broadcast([128, NT, G, 8]), op=OP.mult), er=[lgall.t])
            dvb(lambda e: e.tensor_reduce(out=el, in_=p4v.rearrange("p t g k -> p t k g"), axis=AX.X, op=OP.add))
            dvb(lambda e: e.tensor_reduce(out=m1, in_=el, axis=AX.X, op=OP.max))
            dvb(lambda e: e.tensor_tensor(out=oh1, in0=el, in1=bc(m1, 8), op=OP.is_equal))
            dvb(lambda e: e.scalar_tensor_tensor(out=el2, in0=oh1, scalar=-1e30, in1=el, op0=OP.mult, op1=OP.add))
            dvb(lambda e: e.tensor_reduce(out=m2, in_=el2, axis=AX.X, op=OP.max))
            dvb(lambda e: e.tensor_tensor(out=oh2, in0=el2, in1=bc(m2, 8), op=OP.is_equal))
            dvb(lambda e: e.tensor_tensor(out=dd, in0=m1, in1=m2, op=OP.subtract))
            S.op("act", lambda e: e.activation(out=sig, in_=dd, func=AF.Sigmoid), r=[RT], w=[RT])
            for (oh, dst, n) in ((ohg, gidx, G), (oh1, e1, 8), (oh2, e2, 8)):
                dvb(lambda e, oh=oh, n=n: e.tensor_tensor(out=t8[:, :, 0:n], in0=oh[:, :, 0:n], in1=iota8[:, 0:n].unsqueeze(1).to_broadcast([128, NT, n]), op=OP.mult), er=[misc.t])
                dvb(lambda e, dst=dst, n=n: e.tensor_reduce(out=dst, in_=t8[:, :, 0:n], axis=AX.X, op=OP.add))
            for (ek, eik, Ok) in ((e1, ei1, O1), (e2, ei2, O2)):
                dvb(lambda e, ek=ek, eik=eik: e.scalar_tensor_tensor(out=eik, in0=gidx, scalar=8.0, in1=ek, op0=OP.mult, op1=OP.add))
                dvb(lambda e, eik=eik, Ok=Ok: e.tensor_tensor(out=Ok, in0=iotaTE.ap, in1=bc(eik, NE), op=OP.is_equal), er=[iotaTE.t])
                dvb(lambda e, Ok=Ok: e.tensor_tensor(out=Ok, in0=Ok, in1=bc(valid, NE), op=OP.mult), er=[misc.t])
            dvb(lambda e: e.tensor_tensor(out=Oa, in0=O1, in1=O2, op=OP.add))
            NCOL = NT * NE
            fl = lambda a: a.rearrange("p t e -> p (t e)")
            for c0 in range(0, NCOL, 512):
                cn = min(512, NCOL - c0)
                pk, tk_ = PS()
                S.op("pe", lambda e, pk=pk: e.matmul(pk[:, 0:cn], lhsT=ustrict, rhs=fl(Oa)[:, c0:c0 + cn], start=True, stop=True), r=[RT, misc.t], w=[tk_])
                dvb(lambda e, pk=pk: e.tensor_copy(out=fl(rk)[:, c0:c0 + cn], in_=pk[:, 0:cn]), er=[tk_])
                pc, tc2 = PS()
                S.op("pe", lambda e, pc=pc: e.matmul(pc[:, 0:cn], lhsT=ones, rhs=fl(Oa)[:, c0:c0 + cn], start=True, stop=True), r=[RT, misc.t], w=[tc2])
                dvb(lambda e, pc=pc: e.tensor_copy(out=fl(csum)[:, c0:c0 + cn], in_=pc[:, 0:cn]), er=[tc2])
            dvb(lambda e: e.tensor_copy(out=cum[:, 0, :], in_=cnt.ap), er=[cnt.t])
            for t_ in range(1, NT):
                dvb(lambda e: e.tensor_tensor(out=cum[:, t_, :], in0=cum[:, t_ - 1, :], in1=csum[:, t_ - 1, :], op=OP.add))
            dvb(lambda e: e.tensor_tensor(out=cnt.ap, in0=cum[:, NT - 1, :], in1=csum[:, NT - 1, :], op=OP.add), er=[cnt.t], ew=[cnt.t])
            dvb(lambda e: e.tensor_tensor(out=rk, in0=rk, in1=cum, op=OP.add))
            dvb(lambda e: e.tensor_tensor(out=sv, in0=rk, in1=slotbase.ap.unsqueeze(1).to_broadcast([128, NT, NE]), op=OP.add), er=[slotbase.t])
            for kq, Ok in enumerate((O1, O2)):
                dvb(lambda e, Ok=Ok: e.tensor_tensor(out=Oa, in0=Ok, in1=rk, op=OP.mult))
                dvb(lambda e, kq=kq: e.tensor_reduce(out=rr[kq], in_=Oa, axis=AX.X, op=OP.add))
                dvb(lambda e, Ok=Ok: e.tensor_tensor(out=Oa, in0=Ok, in1=sv, op=OP.mult))
                dvb(lambda e, kq=kq: e.tensor_reduce(out=slf[:, :, kq], in_=Oa, axis=AX.X, op=OP.add))
                dvb(lambda e, kq=kq: e.tensor_scalar(out=rr[kq], in0=rr[kq], scalar1=float(CAP) - 0.5, scalar2=None, op0=OP.is_gt))
                dvb(lambda e, kq=kq: e.scalar_tensor_tensor(out=slf[:, :, kq], in0=rr[kq], scalar=float(NSLOT), in1=slf[:, :, kq], op0=OP.mult, op1=OP.add))
                dvb(lambda e, kq=kq: e.tensor_scalar(out=rr[kq], in0=rr[kq], scalar1=-1.0, scalar2=1.0, op0=OP.mult, op1=OP.add))
            gs = gates.ap[:, s * NT:(s + 1) * NT, :]
            dvb(lambda e: e.tensor_tensor(out=gs[:, :, 0], in0=pg, in1=sig, op=OP.mult), er=[gates.t], ew=[gates.t])
            dvb(lambda e: e.tensor_tensor(out=gs[:, :, 1], in0=pg, in1=gs[:, :, 0], op=OP.subtract), er=[gates.t], ew=[gates.t])
            for kq in range(2):
                dvb(lambda e, kq=kq: e.tensor_tensor(out=gs[:, :, kq], in0=gs[:, :, kq], in1=rr[kq], op=OP.mult), er=[gates.t], ew=[gates.t])
            dvb(lambda e: e.tensor_copy(out=slots.ap[:, s * NT:(s + 1) * NT, :], in_=slf), er=[slots.t], ew=[slots.t])
            for i in range(NT):
                P = 128 if i < NT - 1 else LAST
                tix = s * NT + i
                for kq in range(2):
                    S.op("pool", lambda e, kq=kq: e.indirect_dma_start(out=xs_d, out_offset=bass.IndirectOffsetOnAxis(ap=slots.ap[:P, tix, kq:kq + 1], axis=0), in_=hfball.ap[:P, i, :], in_offset=None, bounds_check=bcreg(e), oob_is_err=False), r=[slots.t, hfball.t], w=[t_xs], dma=True)
            AR.release(m0)

        conv_some(10 ** 9)
        if DEBUG:
            dma("sp", dbg_sg[:, :, 0:2], gates.ap, r=[gates.t])
            dma("sp", dbg_sg[:, :, 2:4].bitcast(I32), slots.ap, r=[slots.t])
        new_phase()
        NWB = 4
        wgb = [B(AR, [8, 512], BF16) for _ in range(NWB)]
        wub = [B(AR, [8, 512], BF16) for _ in range(NWB)]
        wdb = [B(AR, [4, D], BF16) for _ in range(NWB)]
        for bb in wgb + wub + wdb:
            bb.th = [Tk(), Tk()]
        NST = CAP // 128
        xT = [B(A1, [8, CAP], BF16) for _ in range(2)]
        sgb = [B(A1, [CAP]) for _ in range(2)]
        actb = [B(A1, [4, CAP], BF16) for _ in range(2)]
        ybuf = [B(A2, [D]) for _ in range(2)]
        PF = 2
        xsb = [B(A1, [D], BF16) for _ in range((PF + 1) * NST)]

        def issue_loads(ex):
            wg_, wu_, wd_ = wgb[ex % NWB], wub[ex % NWB], wdb[ex % NWB]
            for h2 in range(2):
                dma("pool", wg_.ap[:, h2 * 4:h2 * 4 + 4, :], wg_d[ex].rearrange("(dc p) c -> p dc c", p=128)[:, h2 * 4:h2 * 4 + 4, :], w=[wg_.th[h2]])
                dma("pool", wu_.ap[:, h2 * 4:h2 * 4 + 4, :], wu_d[ex].rearrange("(dc p) c -> p dc c", p=128)[:, h2 * 4:h2 * 4 + 4, :], w=[wu_.th[h2]])
            for h2 in range(2):
                dma("pool", wd_.ap[:, h2 * 2:h2 * 2 + 2, :], wd_d[ex].rearrange("(fc p) c -> p fc c", p=128)[:, h2 * 2:h2 * 2 + 2, :], w=[wd_.th[h2]])
            for st_ in range(NST):
                xb_ = xsb[(ex % (PF + 1)) * NST + st_]
                dma("sp", xb_.ap, xs_d[ex * CAP + st_ * 128:ex * CAP + (st_ + 1) * 128, :], r=[t_xs], w=[xb_.t])

        for ex in range(min(PF, NE)):
            issue_loads(ex)
        for ex in range(NE):
            if ex + PF < NE:
                issue_loads(ex + PF)
            wg_, wu_, wd_ = wgb[ex % NWB], wub[ex % NWB], wdb[ex % NWB]
            xt = xT[ex % 2]; ab_ = actb[ex % 2]
            for st_ in range(NST):
                xb_ = xsb[(ex % (PF + 1)) * NST + st_]
                pa, pt = PS(BF16)
                for dc in range(8):
                    S.op("pe", lambda e, pa=pa, dc=dc, xb_=xb_: e.transpose(out=pa[:, dc * 128:(dc + 1) * 128], in_=xb_.ap[:, dc * 128:(dc + 1) * 128], identity=identb.ap), r=[xb_.t, identb.t], w=[pt])
                S.op("act", lambda e, pa=pa, st_=st_: e.activation(out=xt.ap[:, :, st_ * 128:(st_ + 1) * 128], in_=pa.rearrange("p (a b) -> p a b", b=128), func=AF.Copy), r=[pt], w=[xt.t])
            for fc in range(4):
                pg_, tg = PS(); pu_, tu = PS()
                for dc in range(8):
                    S.op("pe", lambda e, pg_=pg_, dc=dc, fc=fc: e.matmul(pg_[:, 0:CAP], lhsT=wg_.ap[:, dc, fc * 128:(fc + 1) * 128], rhs=xt.ap[:, dc, :], start=(dc == 0), stop=(dc == 7)), r=[wg_.th[dc // 4], xt.t], w=[tg], silent=not (dc == 7))
                for dc in range(8):
                    S.op("pe", lambda e, pu_=pu_, dc=dc, fc=fc: e.matmul(pu_[:, 0:CAP], lhsT=wu_.ap[:, dc, fc * 128:(fc + 1) * 128], rhs=xt.ap[:, dc, :], start=(dc == 0), stop=(dc == 7)), r=[wu_.th[dc // 4], xt.t], w=[tu], silent=not (dc == 7))
                sg_ = sgb[fc % 2]
                S.op("act", lambda e, pg_=pg_, sg_=sg_: e.activation(out=sg_.ap, in_=pg_[:, 0:CAP], func=AF.Silu), r=[tg], w=[sg_.t])
                S.op("dve", lambda e, pu_=pu_, sg_=sg_, fc=fc: e.tensor_tensor(out=ab_.ap[:, fc, :], in0=pu_[:, 0:CAP], in1=sg_.ap, op=OP.mult), r=[tu, sg_.t], w=[ab_.t])
            for st_ in range(NST):
                yb_ = ybuf[st_ % 2]
                for half in range(2):
                    po, to = PS()
                    for fc in range(4):
                        S.op("pe", lambda e, po=po, fc=fc, st_=st_, half=half: e.matmul(po, lhsT=ab_.ap[:, fc, st_ * 128:(st_ + 1) * 128], rhs=wd_.ap[:, fc, half * 512:(half + 1) * 512], start=(fc == 0), stop=(fc == 3)), r=[ab_.t, wd_.th[fc // 2]], w=[to], silent=not (fc == 3))
                    if half == 0:
                        S.op("act", lambda e, po=po, yb_=yb_: e.activation(out=yb_.ap[:, 0:512], in_=po, func=AF.Copy), r=[to], w=[yb_.t])
                    else:
                        S.op("dve", lambda e, po=po, yb_=yb_: e.tensor_copy(out=yb_.ap[:, 512:1024], in_=po), r=[to], w=[yb_.t])
                dma("sp", y_d[ex * CAP + st_ * 128:ex * CAP + (st_ + 1) * 128, :], yb_.ap, r=[yb_.t], w=[t_y])

        new_phase()
        NCB = 4
        y1 = [B(AR, [D]) for _ in range(NCB)]; y2 = [B(AR, [D]) for _ in range(NCB)]
        hh = [B(AR, [D]) for _ in range(NCB)]; ob = [B(AR, [D]) for _ in range(NCB)]
        junks = [B(AR, [D]) for _ in range(2)]; ssf = [B(AR, [2]) for _ in range(NCB)]
        for bb in y1 + y2:
            S.op("pool", lambda e, bb=bb: e.memset(bb.ap, 0.0), w=[bb.t])
        def issue_h1(tix):
            s, i = divmod(tix, NT)
            P = 128 if i < NT - 1 else LAST
            dma("sp", hh[tix % NCB].ap[:P], h1_d[s * T + i * 128:s * T + i * 128 + P, :], r=[t_h1], w=[hh[tix % NCB].t])

        PFC = 2
        for tix in range(min(PFC, NTT)):
            issue_h1(tix)
        for s in range(2):
            for i in range(NT):
                P = 128 if i < NT - 1 else LAST
                tix = s * NT + i
                if tix + PFC < NTT:
                    issue_h1(tix + PFC)
                a, b_, h_, o_, ss = y1[tix % NCB], y2[tix % NCB], hh[tix % NCB], ob[tix % NCB], ssf[tix % NCB]
                junk = junks[tix % 2]
                for kq, dst in enumerate((a, b_)):
                    S.op("pool", lambda e, kq=kq, dst=dst: e.indirect_dma_start(out=dst.ap[:P], out_offset=None, in_=y_d, in_offset=bass.IndirectOffsetOnAxis(ap=slots.ap[:P, tix, kq:kq + 1], axis=0), bounds_check=bcreg(e), oob_is_err=False), r=[slots.t, t_y], w=[dst.t], dma=True)
                S.op("dve", lambda e: e.scalar_tensor_tensor(out=h_.ap[:P], in0=a.ap[:P], scalar=gates.ap[:P, tix, 0:1], in1=h_.ap[:P], op0=OP.mult, op1=OP.add), r=[a.t, gates.t, h_.t], w=[h_.t])
                S.op("dve", lambda e: e.scalar_tensor_tensor(out=h_.ap[:P], in0=b_.ap[:P], scalar=gates.ap[:P, tix, 1:2], in1=h_.ap[:P], op0=OP.mult, op1=OP.add), r=[b_.t, gates.t, h_.t], w=[h_.t])
                sumsq(h_.ap[:P], junk.ap[:P], ss.ap[:P, 0:1], [h_.t], junk.t, ss.t)
                rstd_from_ss(ss.ap[:, 0:1], ss.ap[:, 1:2], D, P, ss.t)
                S.op("dve", lambda e: e.scalar_tensor_tensor(out=o_.ap[:P], in0=h_.ap[:P], scalar=ss.ap[:P, 1:2], in1=g_fin[:P], op0=OP.mult, op1=OP.mult), r=[h_.t, ss.t, rows.t], w=[o_.t])
                if i == 0:
                    dma("sp", out_d[s, 0:128 - N_META, :], o_.ap[N_META:128], r=[o_.t])
                else:
                    r0 = i * 128 - N_META
                    dma("sp", out_d[s, r0:r0 + P, :], o_.ap[:P], r=[o_.t])
        S.emit()
    return nc


_CACHE = {}


def run_kernel(inputs, n_cores, NG, CAP):
    x = np.asarray(inputs["x"], np.float32)
    Bt, SEQ, _ = x.shape
    assert Bt == 2 * n_cores
    L = SEQ + N_META
    NT = (L + 127) // 128
    NE = NG * 8
    key = (SEQ, NG, CAP)
    if key not in _CACHE:
        _CACHE[key] = build_program(SEQ, NG, CAP)
    nc = _CACHE[key]
    f = lambda k: np.asarray(inputs[k], np.float32)
    c = host_consts(L, NT)
    rep = lambda v: np.broadcast_to(v.reshape(1, -1), (128, v.size))
    rows = np.zeros((128, 6144), np.float32)
    rows[:, 0:1024] = rep(f("norm_mix")[0]); rows[:, 1024:2048] = rep(f("norm_ffn")[0]); rows[:, 2048:3072] = rep(f("norm_final"))
    rows[:, 3072:3584] = rep(f("hyena_norm")[0]); rows[:, 3584:4096] = rep(f("hgrn_norm")[0])
    rows[:, 4096:5120] = rep(f("filt_skip")[0].reshape(-1))
    convw = np.zeros((128, 12, 4), np.float32)
    convw[:, :, 0:3] = f("conv_w")[0].reshape(3, 12, 128).transpose(2, 1, 0)
    convw[:, :, 3] = f("conv_b")[0].reshape(12, 128).T
    lb = np.stack([f("lb_fwd"), f("lb_bwd")], 0)
    lb = np.ascontiguousarray(lb.reshape(2, 2, 4, 128).transpose(3, 0, 1, 2))
    wr = np.concatenate([f("w_router_group")[0], f("w_router_expert")[0].reshape(D, NE)], axis=1)
    wr_full = np.zeros((D, 8 + NE), np.float32)
    wr_full[:, 0:NG] = wr[:, 0:NG]; wr_full[:, 8:] = wr[:, NG:]
    wr_l = np.ascontiguousarray(wr_full.reshape(8, 128, 8 + NE).transpose(1, 0, 2))
    fb = np.zeros((64, 4), np.float32)
    fb[:, 0] = f("filt_b1")[0]; fb[:, 1] = f("filt_b2")[0]; fb[:, 2] = f("filt_freq")[0]
    shared = dict(meta=f("meta_tokens"), w_in=f("w_in")[0], w_out=f("w_out")[0], w_gate=f("w_gate")[0], w_up=f("w_up")[0],
                  w_down=f("w_down")[0], w_r=wr_l, rows=rows, convw=convw, lb=lb, f_w1=f("filt_w1")[0], f_w2=f("filt_w2")[0],
                  f_w3=f("filt_w3")[0], f_b=fb, **c)
    in_maps = []
    for ci in range(n_cores):
        m = dict(shared)
        m["x"] = np.ascontiguousarray(x[2 * ci:2 * ci + 2])
        in_maps.append(m)
    res = run_bass_kernel_spmd(nc, in_maps, core_ids=list(range(n_cores)))
    if DEBUG:
        global LAST_RES
        LAST_RES = res.results
    return np.concatenate([r["out"] for r in res.results], axis=0).astype(np.float32)


def kernel(**inputs):
    return run_kernel(inputs, 8, 8, 256)
```

```python
import math
import numpy as np
import ml_dtypes
import concourse.bass as bass
import concourse.mybir as mybir
from concourse.bass_utils import run_bass_kernel_spmd

F32 = mybir.dt.float32
BF16 = mybir.dt.bfloat16
I32 = mybir.dt.int32
AF = mybir.ActivationFunctionType
OP = mybir.AluOpType
AX = mybir.AxisListType

ENGS = ("pe", "act", "dve", "pool", "sp")


import types


def _freeze(fn):
    if fn.__closure__ is None:
        return fn
    cells = []
    for c in fn.__closure__:
        try:
            cells.append(types.CellType(c.cell_contents))
        except ValueError:
            cells.append(c)
    return types.FunctionType(fn.__code__, fn.__globals__, fn.__name__, fn.__defaults__, tuple(cells))


class Tk:
    __slots__ = ("w", "r", "name")

    def __init__(self, name=""):
        self.w = None
        self.r = []
        self.name = name


class Sched:
    def __init__(self, nc, n_dma_sems=24):
        self.nc = nc
        self.ops = {e: [] for e in ENGS}
        self.count = {e: 0 for e in ENGS}
        self.known = {e: {} for e in ENGS}
        self.n_hw = n_dma_sems
        self.n_grp1 = 12
        self.n_dma = n_dma_sems + self.n_grp1
        self.dma_cnt = [0] * self.n_dma
        self.dma_tok = [Tk("dmasem%d" % i) for i in range(self.n_dma)]
        self.dma_rr = 0
        self.rr1 = 0
        self.out_events = []

    def _need(self, eng, ev, waits):
        if ev is None:
            return
        key, val, clock = ev
        kn = self.known[eng]
        if key == eng and eng == "pe":
            return
        if kn.get(key, 0) >= val:
            return
        waits[key] = max(waits.get(key, 0), val)
        for k, v in clock.items():
            if kn.get(k, 0) < v:
                kn[k] = v
        if kn.get(key, 0) < val:
            kn[key] = val

    def op(self, eng, fn, r=(), w=(), dma=False, silent=False, grp=0):
        fn = _freeze(fn)
        silent = bool(silent) and eng == "pe" and not dma
        waits = {}
        slot = None
        toks_w = list(w)
        if dma:
            if grp == 1:
                slot = self.n_hw + self.rr1
                self.rr1 = (self.rr1 + 1) % self.n_grp1
            else:
                slot = self.dma_rr
                self.dma_rr = (self.dma_rr + 1) % self.n_hw
            toks_w.append(self.dma_tok[slot])
        for t in r:
            self._need(eng, t.w, waits)
        for t in toks_w:
            self._need(eng, t.w, waits)
            for ev in t.r:
                self._need(eng, ev, waits)
        if dma:
            self.dma_cnt[slot] += 16
            key, val = ("d", slot), self.dma_cnt[slot]
        elif silent:
            key, val = eng, self.count[eng] + 1
        else:
            self.count[eng] += 1
            key, val = eng, self.count[eng]
        clock = dict(self.known[eng])
        ev = (key, val, clock)
        if not dma and eng == "pe" and not silent:
            self.known[eng][eng] = val
        for t in r:
            t.r.append(ev)
        for t in toks_w:
            t.w = ev
            t.r = []
        self.ops[eng].append((sorted(waits.items(), key=str), fn, key, 16 if dma else (0 if silent else 1)))
        return ev

    def barrier(self):
        evs = []
        for e in ENGS:
            if self.count[e]:
                evs.append((e, self.count[e], {}))
        for s in range(self.n_dma):
            if self.dma_cnt[s]:
                evs.append((("d", s), self.dma_cnt[s], {}))
        for e in ENGS:
            waits = {}
            for ev in evs:
                if ev[0] != e:
                    self._need(e, ev, waits)
            if waits:
                self.ops[e].append((sorted(waits.items(), key=str), None, None, 0))

    def emit(self):
        nc = self.nc
        import contextlib
        with contextlib.ExitStack() as st:
            sems = {}
            for e in ENGS:
                sems[e] = st.enter_context(nc.semaphore("sem_" + e))
            for s in range(self.n_dma):
                sems[("d", s)] = st.enter_context(nc.semaphore("sem_d%d" % s))
            fin = {}
            for e in ENGS:
                if e != "sp" and self.count[e]:
                    fin[e] = self.count[e]
            for s in range(self.n_dma):
                if self.dma_cnt[s]:
                    fin[("d", s)] = self.dma_cnt[s]
            self.ops["sp"].append((sorted(fin.items(), key=str), None, None, 0))
            block = st.enter_context(nc.Block())

            def run(engname):
                def body(eng):
                    for waits, fn, key, inc in self.ops[engname]:
                        for k, v in waits:
                            eng.wait_ge(sems[k], v)
                        if fn is not None:
                            if inc:
                                fn(eng).then_inc(sems[key], inc)
                            else:
                                fn(eng)
                return body

            block.tensor(run("pe"))
            block.scalar(run("act"))
            block.vector(run("dve"))
            block.gpsimd(run("pool"))
            block.sync(run("sp"))


class Arena:
    def __init__(self, ap, nwords):
        self.ap = ap
        self.n = nwords
        self.top = 0

    def mark(self):
        return self.top

    def release(self, m):
        self.top = m

    def alloc(self, shape, dtype=F32):
        n = int(np.prod(shape))
        words = n if dtype in (F32, I32) else (n + 1) // 2
        words = (words + 7) // 8 * 8
        assert self.top + words <= self.n, "SBUF arena overflow %d + %d > %d" % (self.top, words, self.n)
        v = self.ap[:, self.top:self.top + words]
        self.top += words
        if dtype != F32:
            v = v.bitcast(dtype)
        v = v[:, 0:n]
        if len(shape) == 2:
            v = v.rearrange("p (a b) -> p a b", b=shape[1])
        elif len(shape) == 3:
            v = v.rearrange("p (a b c) -> p a b c", b=shape[1], c=shape[2])
        return v


N_META = 16
D = 1024
DH = 512
EPS = 1e-6
NWORDS = 52992


def host_consts(L, NT):
    T = NT * 128
    N = 2 * T
    t = np.arange(T, dtype=np.float64)
    f = np.arange(T, dtype=np.float64) + 0.5
    ang = 2.0 * np.pi / N * np.outer(t, f)
    valid = (t < L)[:, None]
    Cc = np.where(valid, np.cos(ang), 0.0)
    Cs = np.where(valid, np.sin(ang), 0.0)

    def lay(M):
        return np.ascontiguousarray(M.reshape(NT, 128, NT, 128).transpose(2, 1, 0, 3)).astype(ml_dtypes.bfloat16)

    c = {}
    c["dft_cc"] = lay(Cc)
    c["dft_cs"] = lay(Cs)
    c["dft_cct"] = lay(Cc.T.copy())
    c["dft_cst"] = lay(Cs.T.copy())
    pos = np.arange(L, dtype=np.float32)
    tt = pos / max(L - 1, 1)
    bands = np.linspace(1e-4, 15, 16, dtype=np.float32)
    a2 = (2.0 * math.pi / L) * pos[:, None] * bands[None, :]
    z = np.concatenate([tt[:, None], np.cos(a2), -np.sin(a2)], axis=-1).astype(np.float32)
    zT = np.zeros((33, T), np.float32)
    zT[:, :L] = z.T
    c["zfeat"] = zT
    deltas = np.abs(np.linspace(math.log(1e-2) / 1.5, math.log(1e-2) / 0.3, DH, dtype=np.float32))
    win = np.zeros((T, DH), np.float32)
    win[:L] = np.exp(-tt[:, None] * deltas[None, :])
    winb = win.copy()
    winb[0] = 0.0
    c["win_f"] = np.ascontiguousarray(win.reshape(NT, 128, DH).transpose(1, 0, 2))
    c["win_b"] = np.ascontiguousarray(winb.reshape(NT, 128, DH).transpose(1, 0, 2))
    i = np.arange(128)
    misc = np.zeros((128, 1024), np.float32)
    misc[:, 0:128] = np.eye(128)
    misc[:, 128:256] = (i[None, :] >= i[:, None])
    misc[:, 256:384] = (i[None, :] <= i[:, None])
    misc[:, 384:512] = (i[:, None] < i[None, :])
    misc[:, 512:640] = 1.0
    misc[:, 640:704] = np.arange(64)[None, :]
    misc[:, 704:712] = np.arange(8)[None, :]
    misc[:, 712] = i
    misc[:, 720:720 + NT] = 1.0
    misc[L - (NT - 1) * 128:, 720 + NT - 1] = 0.0
    c["misc"] = misc
    return c


DEBUG = False


def build_program(SEQ, NG, CAP):
    L = SEQ + N_META
    NT = (L + 127) // 128
    T = NT * 128
    LAST = L - (NT - 1) * 128
    NE = NG * 8
    NSLOT = NE * CAP
    NTT = 2 * NT
    NDFT = 2 * T
    nc = bass.Bass("TRN2", target_bir_lowering=False)

    def din(name, shape, dt=F32):
        return nc.dram_tensor(name, list(shape), dt, kind="ExternalInput").ap()

    x_d = din("x", [2, SEQ, D])
    meta_d = din("meta", [N_META, D])
    w_in_d = din("w_in", [D, 4096])
    w_out_d = din("w_out", [D, D])
    wg_d = din("w_gate", [NE, D, 512])
    wu_d = din("w_up", [NE, D, 512])
    wd_d = din("w_down", [NE, 512, D])
    wr_d = din("w_r", [128, 8, 8 + NE])
    rows_d = din("rows", [128, 6144])
    convw_d = din("convw", [128, 12, 4])
    lb_d = din("lb", [128, 2, 2, 4])
    f1_d = din("f_w1", [33, 64])
    f2_d = din("f_w2", [64, 64])
    f3_d = din("f_w3", [64, 2048])
    fb_d = din("f_b", [64, 4])
    misc_d = din("misc", [128, 1024])
    zfeat_d = din("zfeat", [33, T])
    winf_d = din("win_f", [128, NT, DH])
    winb_d = din("win_b", [128, NT, DH])
    cc_d = din("dft_cc", [NT, 128, NT, 128], BF16)
    cs_d = din("dft_cs", [NT, 128, NT, 128], BF16)
    cct_d = din("dft_cct", [NT, 128, NT, 128], BF16)
    cst_d = din("dft_cst", [NT, 128, NT, 128], BF16)
    out_d = nc.dram_tensor("out", [2, SEQ, D], F32, kind="ExternalOutput").ap()
    dk = dict(kind="ExternalOutput") if DEBUG else {}
    kspec_d = nc.dram_tensor("kspec", [2, 2, T, DH], F32, **dk).ap()
    h1_d = nc.dram_tensor("h1s", [2 * T, D], F32, **dk).ap()
    xs_d = nc.dram_tensor("xsort", [NSLOT, D], BF16, **dk).ap()
    y_d = nc.dram_tensor("ysort", [NSLOT, D], F32, **dk).ap()
    wgb_d = nc.dram_tensor("wg_bf", [NE, 128, 8, 512], BF16).ap()
    wub_d = nc.dram_tensor("wu_bf", [NE, 128, 8, 512], BF16).ap()
    wdb_d = nc.dram_tensor("wd_bf", [NE, 128, 4, D], BF16).ap()
    if DEBUG:
        dbg_mix = nc.dram_tensor("dbg_mix", [2, 128, 8, T], BF16, kind="ExternalOutput").ap()
        dbg_zx = nc.dram_tensor("dbg_zx", [2, 3, 128, NT, DH], BF16, kind="ExternalOutput").ap()
        dbg_aT = nc.dram_tensor("dbg_aT", [2, 128, 8, T], BF16, kind="ExternalOutput").ap()
        dbg_sg = nc.dram_tensor("dbg_sg", [128, NTT, 4], F32, kind="ExternalOutput").ap()

    import contextlib
    st = contextlib.ExitStack()
    with st:
        big = st.enter_context(nc.sbuf_tensor("arena", [128, NWORDS], F32))
        psb = [st.enter_context(nc.psum_tensor("ps%d" % i, [128, 512], F32)) for i in range(8)]
        S = Sched(nc)
        o0 = 0
        AC = Arena(big[:, o0:o0 + 8320], 8320); o0 += 8320
        A1 = Arena(big[:, o0:o0 + 8704], 8704); o0 += 8704
        A2 = Arena(big[:, o0:o0 + 8960], 8960); o0 += 8960
        AR = Arena(big[:, o0:NWORDS], NWORDS - o0)

        ps_tok = [Tk("ps%d" % i) for i in range(8)]
        ps_rr = [0]

        def PS(dtype=F32):
            i = ps_rr[0]
            ps_rr[0] = (i + 1) % 8
            ap = psb[i][:]
            if dtype != F32:
                ap = ap.bitcast(dtype)
            return ap, ps_tok[i]

        def new_phase():
            S.barrier()
            for a in (A1, A2, AR):
                a.release(0)

        class B:
            def __init__(self, arena, shape, dtype=F32, name=""):
                self.ap = arena.alloc(shape, dtype)
                self.t = Tk(name)

        regc = {}

        def bcreg(e):
            if "r" not in regc:
                regc["r"] = e.to_reg(NSLOT - 1)
            return regc["r"]

        def dma(q, out, in_, r=(), w=(), grp=0):
            S.op(q, lambda e: e.dma_start(out=out, in_=in_), r=r, w=w, dma=True, grp=grp)

        conv_tok = [[Tk(), Tk(), Tk()] for _ in range(NE)]
        conv_jobs = []
        for ex_ in range(NE):
            conv_jobs.append((wgb_d[ex_], wg_d[ex_].rearrange("(dc p) c -> p dc c", p=128), conv_tok[ex_][0]))
            conv_jobs.append((wub_d[ex_], wu_d[ex_].rearrange("(dc p) c -> p dc c", p=128), conv_tok[ex_][1]))
            conv_jobs.append((wdb_d[ex_], wd_d[ex_].rearrange("(fc p) c -> p fc c", p=128), conv_tok[ex_][2]))
        conv_pos = [0]

        def conv_some(n):
            return
            while n > 0 and conv_pos[0] < len(conv_jobs):
                o_, i_, tk_c = conv_jobs[conv_pos[0]]
                conv_pos[0] += 1
                n -= 1
                dma("pool", o_, i_, w=[tk_c], grp=1)

        def rstd_from_ss(ss, rs, n, npart, tk):
            S.op("act", lambda e: e.activation(out=rs[:npart], in_=ss[:npart], func=AF.Sqrt, scale=1.0 / n, bias=epsb.ap[:npart, 0:1]), r=[tk, epsb.t], w=[tk])
            S.op("dve", lambda e: e.reciprocal(out=rs[:npart], in_=rs[:npart]), r=[tk], w=[tk])

        def sumsq(src_ap, junk_ap, ss_ap, r, jt, st):
            S.op("act", lambda e: e.activation(out=junk_ap, in_=src_ap, func=AF.Square), r=r, w=[jt])
            S.op("dve", lambda e: e.tensor_reduce(out=ss_ap, in_=junk_ap, axis=AX.X, op=OP.add), r=[jt], w=[st])

        misc = B(AC, [1024]); dma("sp", misc.ap, misc_d, w=[misc.t])
        ident = misc.ap[:, 0:128]
        rows = B(AC, [5120]); dma("sp", rows.ap, rows_d[:, 0:5120], w=[rows.t])
        g_mix, g_ffn, g_fin = rows.ap[:, 0:1024], rows.ap[:, 1024:2048], rows.ap[:, 2048:3072]
        g_hy, g_hg = rows.ap[:, 3072:3584], rows.ap[:, 3584:4096]
        skipB = [rows.ap[:, 4096:4608], rows.ap[:, 4608:5120]]
        convw = B(AC, [12, 4]); dma("sp", convw.ap, convw_d, w=[convw.t])
        lbr = B(AC, [2, 2, 4]); dma("sp", lbr.ap, lb_d, w=[lbr.t])
        wr = B(AC, [8, 8 + NE]); dma("sp", wr.ap, wr_d, w=[wr.t])
        identb = B(AC, [128], BF16)
        S.op("dve", lambda e: e.tensor_copy(out=identb.ap, in_=ident), r=[misc.t], w=[identb.t])
        maskb = B(AC, [2, 128], BF16)
        S.op("dve", lambda e: e.tensor_copy(out=maskb.ap, in_=misc.ap[:, 128:384].rearrange("p (a b) -> p a b", b=128)), r=[misc.t], w=[maskb.t])
        ustrict = misc.ap[:, 384:512]
        ones = misc.ap[:, 512:640]
        iota64 = misc.ap[:, 640:704]
        iota8 = misc.ap[:, 704:712]
        iotap = misc.ap[:, 712:713]
        epsb = B(AC, [1])
        onesT = B(AC, [512])
        S.op("pool", lambda e: e.memset(onesT.ap, 1.0), w=[onesT.t])
        S.op("pool", lambda e: e.memset(epsb.ap, EPS), w=[epsb.t])
        lb = B(AC, [2, 4]); oml = B(AC, [2, 4])
        S.op("dve", lambda e: e.tensor_tensor(out=lb.ap, in0=lbr.ap[:, :, 0, :], in1=lbr.ap[:, :, 1, :], op=OP.subtract), r=[lbr.t], w=[lb.t])
        S.op("act", lambda e: e.activation(out=lb.ap, in_=lb.ap, func=AF.Sigmoid), r=[lb.t], w=[lb.t])
        S.op("dve", lambda e: e.tensor_scalar(out=oml.ap, in0=lb.ap, scalar1=-1.0, scalar2=1.0, op0=OP.mult, op1=OP.add), r=[lb.t], w=[oml.t])
        slots = B(AC, [NTT, 2], I32)
        gates = B(AC, [NTT, 2])
        S.op("pool", lambda e: e.memset(slots.ap, 0), w=[slots.t])
        S.op("pool", lambda e: e.memset(gates.ap, 0.0), w=[gates.t])
        cnt = B(AC, [NE])
        S.op("pool", lambda e: e.memset(cnt.ap, 0.0), w=[cnt.t])
        slotbase = B(AC, [NE])
        S.op("dve", lambda e: e.tensor_scalar(out=slotbase.ap, in0=iota64[:, 0:NE], scalar1=float(CAP), scalar2=None, op0=OP.mult), r=[misc.t], w=[slotbase.t])
        zero_bf = B(AC, [1024], BF16)
        S.op("pool", lambda e: e.memset(zero_bf.ap, 0.0), w=[zero_bf.t])
        t_xs = Tk("xsort"); t_y = Tk("ysort"); t_h1 = Tk("h1s"); t_ks = Tk("kspec")
        for r0 in range(0, NSLOT, 128):
            dma("sp", xs_d[r0:r0 + 128, :], zero_bf.ap, r=[zero_bf.t], w=[t_xs])

        def wrap(buf, tmp, npart):
            for _ in range(2):
                S.op("dve", lambda e: e.tensor_scalar(out=tmp.ap[:npart], in0=buf.ap[:npart], scalar1=math.pi, scalar2=-2 * math.pi, op0=OP.is_gt, op1=OP.mult), r=[buf.t], w=[tmp.t])
                S.op("dve", lambda e: e.tensor_tensor(out=buf.ap[:npart], in0=buf.ap[:npart], in1=tmp.ap[:npart], op=OP.add), r=[tmp.t, buf.t], w=[buf.t])
                S.op("dve", lambda e: e.tensor_scalar(out=tmp.ap[:npart], in0=buf.ap[:npart], scalar1=-math.pi, scalar2=2 * math.pi, op0=OP.is_lt, op1=OP.mult), r=[buf.t], w=[tmp.t])
                S.op("dve", lambda e: e.tensor_tensor(out=buf.ap[:npart], in0=buf.ap[:npart], in1=tmp.ap[:npart], op=OP.add), r=[tmp.t, buf.t], w=[buf.t])

        zT = B(A2, [T]); dma("sp", zT.ap[:33], zfeat_d, w=[zT.t])
        fw1 = B(A1, [64]); dma("sp", fw1.ap[:33], f1_d, w=[fw1.t])
        fw2 = B(A1, [64]); dma("sp", fw2.ap[:64], f2_d, w=[fw2.t])
        fw3 = B(A2, [2048]); dma("sp", fw3.ap[:64], f3_d, w=[fw3.t])
        fbb = B(A1, [4]); dma("sp", fbb.ap[:64], fb_d, w=[fbb.t])
        hT = [B(A1, [T]), B(A1, [T])]
        tmpw = B(A1, [T])
        slabs = [(s0, min(512, T - s0)) for s0 in range(0, T, 512)]
        for layer in range(2):
            src = zT if layer == 0 else hT[0]
            kk = 33 if layer == 0 else 64
            wl = fw1 if layer == 0 else fw2
            dst = hT[layer]
            for (s0, sn) in slabs:
                pa, pt = PS()
                S.op("pe", lambda e, pa=pa, s0=s0, sn=sn: e.matmul(pa[:64, 0:sn], lhsT=wl.ap[:kk, 0:64], rhs=src.ap[:kk, s0:s0 + sn], start=True, stop=True), r=[wl.t, src.t], w=[pt])
                S.op("dve", lambda e, pa=pa, s0=s0, sn=sn: e.tensor_scalar(out=dst.ap[:64, s0:s0 + sn], in0=pa[:64, 0:sn], scalar1=fbb.ap[:64, layer:layer + 1], scalar2=fbb.ap[:64, 2:3], op0=OP.add, op1=OP.mult), r=[pt, fbb.t], w=[dst.t])
            wrap(dst, tmpw, 64)
            S.op("act", lambda e: e.activation(out=dst.ap[:64], in_=dst.ap[:64], func=AF.Sin), r=[dst.t], w=[dst.t])
        h2T = hT[1]
        hs = B(AR, [NT, 1024], BF16); hdd = B(AR, [NT, 1024], BF16)
        mk0 = AR.mark()
        wtile = [[B(AR, [DH]), B(AR, [DH])] for _ in range(2)]
        ftmp = [B(AR, [DH]) for _ in range(8)]
        for i in range(NT):
            wf, wb_ = wtile[i % 2]
            dma("sp", wf.ap, winf_d[:, i, :], w=[wf.t])
            dma("sp", wb_.ap, winb_d[:, i, :], w=[wb_.t])
            for o in range(2):
                pf, ptf = PS(); pb, ptb = PS()
                S.op("pe", lambda e, pf=pf: e.matmul(pf, lhsT=h2T.ap[:64, i * 128:(i + 1) * 128], rhs=fw3.ap[:64, o * 512:(o + 1) * 512], start=True, stop=True), r=[h2T.t, fw3.t], w=[ptf])
                S.op("pe", lambda e, pb=pb: e.matmul(pb, lhsT=h2T.ap[:64, i * 128:(i + 1) * 128], rhs=fw3.ap[:64, 1024 + o * 512:1024 + (o + 1) * 512], start=True, stop=True), r=[h2T.t, fw3.t], w=[ptb])
                ta, tb = ftmp[4 * (i % 2) + 2 * o], ftmp[4 * (i % 2) + 2 * o + 1]
                S.op("dve", lambda e, pf=pf, ta=ta: e.tensor_tensor(out=ta.ap, in0=pf, in1=wf.ap, op=OP.mult), r=[ptf, wf.t], w=[ta.t])
                S.op("dve", lambda e, pb=pb, tb=tb: e.tensor_tensor(out=tb.ap, in0=pb, in1=wb_.ap, op=OP.mult), r=[ptb, wb_.t], w=[tb.t])
                S.op("pool", lambda e, ta=ta, tb=tb: e.tensor_tensor(out=hs.ap[:, i, o * 512:(o + 1) * 512], in0=ta.ap, in1=tb.ap, op=OP.add), r=[ta.t, tb.t], w=[hs.t])
                S.op("pool", lambda e, ta=ta, tb=tb: e.tensor_tensor(out=hdd.ap[:, i, o * 512:(o + 1) * 512], in0=ta.ap, in1=tb.ap, op=OP.subtract), r=[ta.t, tb.t], w=[hdd.t])
        S.barrier()
        AR.release(mk0)
        dftb = [[B(AR, [NT, 128], BF16), B(AR, [NT, 128], BF16)] for _ in range(2)]
        kout = [B(AR, [DH]) for _ in range(4)]
        dma("sp", dftb[0][0].ap, cc_d[0], w=[dftb[0][0].t])
        dma("sp", dftb[0][1].ap, cs_d[0], w=[dftb[0][1].t])
        for j in range(NT):
            cb, sb_ = dftb[j % 2]
            if j + 1 < NT:
                dma("sp", dftb[(j + 1) % 2][0].ap, cc_d[j + 1], w=[dftb[(j + 1) % 2][0].t])
                dma("sp", dftb[(j + 1) % 2][1].ap, cs_d[j + 1], w=[dftb[(j + 1) % 2][1].t])
            for o in range(2):
                for ri, (mat, src) in enumerate(((cb, hs), (sb_, hdd))):
                    pa, pt = PS()
                    for tc_ in range(NT):
                        S.op("pe", lambda e, pa=pa, mat=mat, src=src, tc_=tc_: e.matmul(pa, lhsT=mat.ap[:, tc_, :], rhs=src.ap[:, tc_, o * 512:(o + 1) * 512], start=(tc_ == 0), stop=(tc_ == NT - 1)), r=[mat.t, src.t], w=[pt], silent=not (tc_ == NT - 1))
                    ko = kout[2 * o + ri]
                    S.op("act", lambda e, pa=pa, ko=ko: e.activation(out=ko.ap, in_=pa, func=AF.Copy, scale=2.0 / NDFT), r=[pt], w=[ko.t])
                    dma("sp", kspec_d[o, ri, j * 128:(j + 1) * 128, :], ko.ap, r=[ko.t], w=[t_ks])

        def load_h_tile(q, dst, s, i, npart):
            if i == 0:
                dma(q, dst.ap[0:N_META], meta_d, w=[dst.t])
                dma(q, dst.ap[N_META:128], x_d[s, 0:128 - N_META, :], w=[dst.t])
            else:
                r0 = i * 128 - N_META
                dma(q, dst.ap[:npart], x_d[s, r0:r0 + npart, :], w=[dst.t])

        def wload(arena_buf, c0, ncols):
            dma("pool", arena_buf.ap[:, :, 0:ncols], w_in_d.rearrange("(dc p) c -> p dc c", p=128)[:, :, c0:c0 + ncols], w=[arena_buf.t])

        for s in range(2):
            new_phase()
            mixT = B(A1, [8, T], BF16)
            aT = B(A2, [8, T], BF16)
            if LAST < 128:
                S.op("pool", lambda e: e.memset(aT.ap[:, :, (NT - 1) * 128 + LAST:T], 0.0), w=[aT.t])
            m0 = AR.mark()
            hin = [B(AR, [D]) for _ in range(2)]
            abf = [B(AR, [D], BF16) for _ in range(2)]
            junk = B(AR, [D])
            ssb = [B(AR, [2]) for _ in range(2)]
            for i in range(NT):
                npart = 128 if i < NT - 1 else LAST
                hi, ab, ss = hin[i % 2], abf[i % 2], ssb[i % 2]
                load_h_tile("sp", hi, s, i, npart)
                sumsq(hi.ap[:npart], junk.ap[:npart], ss.ap[:npart, 0:1], [hi.t], junk.t, ss.t)
                rstd_from_ss(ss.ap[:, 0:1], ss.ap[:, 1:2], D, npart, ss.t)
                S.op("dve", lambda e, hi=hi, ss=ss, ab=ab: e.scalar_tensor_tensor(out=ab.ap[:npart], in0=hi.ap[:npart], scalar=ss.ap[:npart, 1:2], in1=g_mix[:npart], op0=OP.mult, op1=OP.mult), r=[hi.t, ss.t, rows.t], w=[ab.t])
                pa, pt = PS(BF16)
                for dc in range(8):
                    S.op("pe", lambda e, pa=pa, ab=ab, dc=dc: e.transpose(out=pa[:, dc * 128:dc * 128 + npart], in_=ab.ap[:npart, dc * 128:(dc + 1) * 128], identity=identb.ap[:npart, :npart]), r=[ab.t, identb.t], w=[pt])
                S.op("act", lambda e, pa=pa: e.activation(out=aT.ap[:, :, i * 128:i * 128 + npart], in_=pa.rearrange("p (a b) -> p a b", b=128)[:, :, 0:npart], func=AF.Copy), r=[pt], w=[aT.t])
            AR.release(m0)

            S.barrier()
            m0 = AR.mark()
            wch = [B(AR, [8, 128], BF16) for _ in range(3)]
            qT = B(AR, [T], BF16); kTd = B(AR, [T], BF16)
            PTe = B(AR, [T + 128])
            lf = B(AR, [T]); sgt = B(AR, [T])
            V = B(AR, [NT, 128], BF16); sgate = B(AR, [NT, 128], BF16); oacc = B(AR, [NT, 128])
            cs3 = [[B(AR, [NT]) for _ in range(4)] for _ in range(2)]
            Sst = [B(AR, [128]), B(AR, [128])]
            KtT1 = B(AR, [T], BF16)
            QtA = [B(AR, [T], BF16) for _ in range(2)]
            sTA = [B(AR, [NT, 128], BF16) for _ in range(2)]
            KtA = [B(AR, [NT, 128], BF16) for _ in range(2)]
            Spb = [B(AR, [128], BF16) for _ in range(2)]
            tmpb = [B(AR, [128]) for _ in range(2)]
            fins = [dict(junk=B(AR, [128]), ss=B(AR, [2]), yb=B(AR, [128], BF16)) for _ in range(2)]
            QhA = [B(AR, [T], BF16) for _ in range(2)]
            onesT2 = B(AR, [T], BF16)
            S.op("pool", lambda e: e.memset(onesT2.ap, 1.0), w=[onesT2.t])
            lf3 = lf.ap.rearrange("p (c k) -> p c k", k=128)
            sg3 = sgt.ap.rearrange("p (c k) -> p c k", k=128)
            for hd in range(4):
                cq = 1536 + hd * 128
                for j, wb in enumerate(wch):
                    wload(wb, cq + j * 512, 128)
                for j in range(3):
                    d = j - 1
                    for (s0, sn) in slabs:
                        pa, pt = PS()
                        for dc in range(8):
                            S.op("pe", lambda e, pa=pa, dc=dc: e.matmul(pa[:, 0:sn], lhsT=wch[j].ap[:, dc, :], rhs=aT.ap[:, dc, s0:s0 + sn], start=(dc == 0), stop=(dc == 7)), r=[wch[j].t, aT.t], w=[pt], silent=not (dc == 7))
                        if j == 0:
                            S.op("act", lambda e, pa=pa: e.activation(out=qT.ap[:, s0:s0 + sn], in_=pa[:, 0:sn], func=AF.Silu), r=[pt], w=[qT.t])
                        else:
                            S.op("act", lambda e, pa=pa: e.activation(out=sgt.ap[:, s0:s0 + sn], in_=pa[:, 0:sn], func=AF.Sigmoid), r=[pt], w=[sgt.t])
                    if j > 0:
                        S.op("dve", lambda e: e.tensor_scalar(out=sgt.ap, in0=sgt.ap, scalar1=oml.ap[:, d, hd:hd + 1], scalar2=lb.ap[:, d, hd:hd + 1], op0=OP.mult, op1=OP.add), r=[sgt.t, oml.t, lb.t], w=[sgt.t])
                        S.op("pool", lambda e: e.tensor_scalar(out=kTd.ap, in0=sgt.ap, scalar1=-1.0, scalar2=1.0, op0=OP.mult, op1=OP.add), r=[sgt.t], w=[kTd.t])
                        S.op("act", lambda e: e.activation(out=lf.ap, in_=sgt.ap, func=AF.Ln), r=[sgt.t], w=[lf.t])
                        S.op("pool", lambda e: e.memset(PTe.ap[:, 0:1], 0.0), w=[PTe.t])
                        S.op("dve", lambda e: e.tensor_tensor_scan(out=PTe.ap[:, 1:1 + T], data0=onesT2.ap, data1=lf.ap, initial=0.0, op0=OP.mult, op1=OP.add), r=[lf.t, onesT2.t, PTe.t], w=[PTe.t])
                    if j == 0:
                        continue
                    P3 = PTe.ap[:, 0:T].rearrange("p (c k) -> p c k", k=128)
                    Z3 = PTe.ap[:, 128:T + 128].rearrange("p (c k) -> p c k", k=128)
                    Acol, Zcol = P3[:, :, 0], Z3[:, :, 0]
                    Mcol = P3[:, :, 65] if d == 0 else P3[:, :, 64]
                    M, c1, c2, dec = cs3[d]
                    S.op("dve", lambda e: e.tensor_copy(out=M.ap, in_=Mcol), r=[PTe.t], w=[M.t])
                    e1, e2 = (c1, c2) if d == 0 else (c2, c1)
                    S.op("dve", lambda e: e.tensor_tensor(out=e1.ap, in0=M.ap, in1=Acol, op=OP.subtract), r=[M.t, PTe.t], w=[e1.t])
                    S.op("dve", lambda e: e.tensor_tensor(out=e2.ap, in0=Zcol, in1=M.ap, op=OP.subtract), r=[M.t, PTe.t], w=[e2.t])
                    S.op("dve", lambda e: e.tensor_tensor(out=dec.ap, in0=Zcol, in1=Acol, op=OP.subtract), r=[PTe.t], w=[dec.t])
                    for bb in (c1, c2, dec):
                        S.op("act", lambda e, bb=bb: e.activation(out=bb.ap, in_=bb.ap, func=AF.Exp), r=[bb.t], w=[bb.t])
                    off = 1 if d == 0 else 0
                    Pv = PTe.ap[:, off:off + T].rearrange("p (c k) -> p c k", k=128)
                    S.op("dve", lambda e: e.tensor_tensor(out=lf3, in0=Pv, in1=M.ap.unsqueeze(2).to_broadcast([128, NT, 128]), op=OP.subtract), r=[PTe.t, M.t, lf.t], w=[lf.t])
                    sq = 1.0 if d == 0 else -1.0
                    S.op("act", lambda e: e.activation(out=sgt.ap, in_=lf.ap, func=AF.Exp, scale=sq), r=[lf.t, sgt.t], w=[sgt.t])
                    S.op("dve", lambda e: e.tensor_tensor(out=QtA[d].ap, in0=qT.ap, in1=sgt.ap, op=OP.mult), r=[qT.t, sgt.t], w=[QtA[d].t])
                    S.op("act", lambda e: e.activation(out=lf.ap, in_=lf.ap, func=AF.Exp, scale=-sq), r=[lf.t], w=[lf.t])
                    S.op("pool", lambda e: e.tensor_tensor(out=KtT1.ap, in0=kTd.ap, in1=lf.ap, op=OP.mult), r=[kTd.t, lf.t], w=[KtT1.t])
                    for c0 in range(0, NT, 4):
                        n4 = min(4, NT - c0)
                        p1, t1 = PS()
                        for k in range(n4):
                            cs_ = slice((c0 + k) * 128, (c0 + k + 1) * 128)
                            S.op("pe", lambda e, p1=p1, k=k, cs_=cs_: e.matmul(p1[:, k * 128:(k + 1) * 128], lhsT=KtT1.ap[:, cs_], rhs=QtA[d].ap[:, cs_], start=True, stop=True), r=[KtT1.t, QtA[d].t], w=[t1])
                        S.op("dve", lambda e, p1=p1: e.tensor_tensor(out=sTA[d].ap[:, c0:c0 + n4, :], in0=p1[:, 0:n4 * 128].rearrange("p (a b) -> p a b", b=128), in1=maskb.ap[:, d:d + 1, :].to_broadcast([128, n4, 128]), op=OP.mult), r=[t1, maskb.t], w=[sTA[d].t])
                    S.op("pool", lambda e: e.tensor_tensor(out=QhA[d].ap.rearrange("p (c k) -> p c k", k=128), in0=QtA[d].ap.rearrange("p (c k) -> p c k", k=128), in1=c1.ap.unsqueeze(2).to_broadcast([128, NT, 128]), op=OP.mult), r=[QtA[d].t, c1.t], w=[QhA[d].t])
                    S.op("pool", lambda e: e.tensor_tensor(out=KtT1.ap.rearrange("p (c k) -> p c k", k=128), in0=KtT1.ap.rearrange("p (c k) -> p c k", k=128), in1=c2.ap.unsqueeze(2).to_broadcast([128, NT, 128]), op=OP.mult), r=[KtT1.t, c2.t], w=[KtT1.t])
                    for c0 in range(0, NT, 8):
                        n8 = min(8, NT - c0)
                        p2, t2 = PS(BF16)
                        for k in range(n8):
                            cs_ = slice((c0 + k) * 128, (c0 + k + 1) * 128)
                            S.op("pe", lambda e, p2=p2, k=k, cs_=cs_: e.transpose(out=p2[:, k * 128:(k + 1) * 128], in_=KtT1.ap[:, cs_], identity=identb.ap), r=[KtT1.t, identb.t], w=[t2])
                        S.op("act", lambda e, p2=p2: e.activation(out=KtA[d].ap[:, c0:c0 + n8, :], in_=p2[:, 0:n8 * 128].rearrange("p (a b) -> p a b", b=128), func=AF.Copy), r=[t2], w=[KtA[d].t])
                wload(wch[0], cq + 3 * 512, 128)
                wload(wch[1], cq + 4 * 512, 128)
                for i0 in range(0, NT, 4):
                    n4 = min(4, NT - i0)
                    for j in range(2):
                        pa, pt = PS()
                        for k in range(n4):
                            i = i0 + k
                            for dc in range(8):
                                S.op("pe", lambda e, pa=pa, k=k, i=i, dc=dc: e.matmul(pa[:, k * 128:(k + 1) * 128], lhsT=aT.ap[:, dc, i * 128:(i + 1) * 128], rhs=wch[j].ap[:, dc, :], start=(dc == 0), stop=(dc == 7)), r=[wch[j].t, aT.t], w=[pt], silent=not (dc == 7))
                        pv = pa[:, 0:n4 * 128].rearrange("p (a b) -> p a b", b=128)
                        if j == 0:
                            S.op("act", lambda e, pv=pv: e.activation(out=V.ap[:, i0:i0 + n4, :], in_=pv, func=AF.Copy), r=[pt], w=[V.t])
                        else:
                            S.op("act", lambda e, pv=pv: e.activation(out=sgate.ap[:, i0:i0 + n4, :], in_=pv, func=AF.Silu), r=[pt], w=[sgate.t])
                S.op("pool", lambda e: e.tensor_tensor(out=sgate.ap, in0=sgate.ap, in1=g_hg[:, hd * 128:(hd + 1) * 128].unsqueeze(1).to_broadcast([128, NT, 128]), op=OP.mult), r=[sgate.t, rows.t], w=[sgate.t])
                S.op("pool", lambda e: e.memset(oacc.ap, 0.0), w=[oacc.t])
                for d in range(2):
                    S.op("pool", lambda e, d=d: e.memset(Sst[d].ap, 0.0), w=[Sst[d].t])
                for step in range(NT):
                    for d in range(2):
                        c = step if d == 0 else NT - 1 - step
                        M, c1, c2, dec = cs3[d]
                        cs_ = slice(128 * c, 128 * c + 128)
                        S.op("act", lambda e: e.activation(out=Spb[d].ap, in_=Sst[d].ap, func=AF.Copy), r=[Sst[d].t], w=[Spb[d].t])
                        p4, t4 = PS()
                        S.op("pe", lambda e, p4=p4: e.matmul(p4[:, 0:128], lhsT=KtA[d].ap[:, c, :], rhs=V.ap[:, c, :], start=True, stop=True), r=[KtA[d].t, V.t], w=[t4])
                        p3, t3 = PS()
                        S.op("pe", lambda e, p3=p3: e.matmul(p3[:, 0:128], lhsT=sTA[d].ap[:, c, :], rhs=V.ap[:, c, :], start=True, stop=False), r=[sTA[d].t, V.t], w=[t3], silent=not False)
                        S.op("pe", lambda e, p3=p3: e.matmul(p3[:, 0:128], lhsT=QhA[d].ap[:, cs_], rhs=Spb[d].ap, start=False, stop=True), r=[QhA[d].t, Spb[d].t], w=[t3])
                        S.op("dve", lambda e, p4=p4: e.scalar_tensor_tensor(out=Sst[d].ap, in0=Sst[d].ap, scalar=dec.ap[:, c:c + 1], in1=p4[:, 0:128], op0=OP.mult, op1=OP.add), r=[t4, dec.t, Sst[d].t], w=[Sst[d].t])
                        S.op("dve", lambda e, p3=p3: e.tensor_tensor(out=oacc.ap[:, c, :], in0=p3[:, 0:128], in1=oacc.ap[:, c, :], op=OP.add), r=[t3, oacc.t], w=[oacc.t])
                for i in range(NT):
                    fin = fins[i % 2]
                    sumsq(oacc.ap[:, i, :], fin["junk"].ap, fin["ss"].ap[:, 0:1], [oacc.t], fin["junk"].t, fin["ss"].t)
                    rstd_from_ss(fin["ss"].ap[:, 0:1], fin["ss"].ap[:, 1:2], 128, 128, fin["ss"].t)
                    S.op("dve", lambda e: e.scalar_tensor_tensor(out=fin["yb"].ap, in0=oacc.ap[:, i, :], scalar=fin["ss"].ap[:, 1:2], in1=sgate.ap[:, i, :], op0=OP.mult, op1=OP.mult), r=[oacc.t, fin["ss"].t, sgate.t], w=[fin["yb"].t])
                    pa, pt = PS(BF16)
                    S.op("pe", lambda e, pa=pa: e.transpose(out=pa[:, 0:128], in_=fin["yb"].ap, identity=identb.ap), r=[fin["yb"].t, identb.t], w=[pt])
                    S.op("act", lambda e, pa=pa: e.activation(out=mixT.ap[:, 4 + hd, i * 128:(i + 1) * 128], in_=pa[:, 0:128], func=AF.Copy), r=[pt], w=[mixT.t])
            AR.release(m0)

            S.barrier()
            m0 = AR.mark()
            zx = [B(AR, [NT, DH], BF16, "zx%d" % k) for k in range(3)]
            m1 = AR.mark()
            wch = [B(AR, [8, 128], BF16) for _ in range(2)]
            pbuf = [B(AR, [T + 8]) for _ in range(2)]
            ub = B(AR, [T]); uT = [B(AR, [T], BF16) for _ in range(2)]
            for pb_ in pbuf:
                S.op("pool", lambda e, pb_=pb_: e.memset(pb_.ap[:, 0:1], 0.0), w=[pb_.t])
                S.op("pool", lambda e, pb_=pb_: e.memset(pb_.ap[:, T + 1:T + 2], 0.0), w=[pb_.t])
            for cc in range(12):
                wb, pb_, ut = wch[cc % 2], pbuf[cc % 2], uT[cc % 2]
                wload(wb, cc * 128, 128)
                for (s0, sn) in slabs:
                    pa, pt = PS()
                    for dc in range(8):
                        S.op("pe", lambda e, pa=pa, dc=dc: e.matmul(pa[:, 0:sn], lhsT=wb.ap[:, dc, :], rhs=aT.ap[:, dc, s0:s0 + sn], start=(dc == 0), stop=(dc == 7)), r=[wb.t, aT.t], w=[pt], silent=not (dc == 7))
                    S.op("act", lambda e, pa=pa: e.activation(out=pb_.ap[:, 1 + s0:1 + s0 + sn], in_=pa[:, 0:sn], func=AF.Copy), r=[pt], w=[pb_.t])
                S.op("act", lambda e: e.activation(out=ub.ap, in_=pb_.ap[:, 1:T + 1], func=AF.Identity, scale=convw.ap[:, cc, 1:2], bias=convw.ap[:, cc, 3:4]), r=[pb_.t, convw.t], w=[ub.t])
                S.op("dve", lambda e: e.scalar_tensor_tensor(out=ub.ap, in0=pb_.ap[:, 0:T], scalar=convw.ap[:, cc, 0:1], in1=ub.ap, op0=OP.mult, op1=OP.add), r=[pb_.t, convw.t, ub.t], w=[ub.t])
                S.op("dve", lambda e: e.scalar_tensor_tensor(out=ut.ap, in0=pb_.ap[:, 2:T + 2], scalar=convw.ap[:, cc, 2:3], in1=ub.ap, op0=OP.mult, op1=OP.add), r=[pb_.t, convw.t, ub.t], w=[ut.t])
                dst = zx[cc // 4]
                cq = (cc % 4) * 128
                for i0 in range(0, NT, 8):
                    n8 = min(8, NT - i0)
                    pa, pt = PS(BF16)
                    for k in range(n8):
                        S.op("pe", lambda e, pa=pa, k=k: e.transpose(out=pa[:, k * 128:(k + 1) * 128], in_=ut.ap[:, (i0 + k) * 128:(i0 + k + 1) * 128], identity=identb.ap), r=[ut.t, identb.t], w=[pt])
                    S.op("act", lambda e, pa=pa: e.activation(out=dst.ap[:, i0:i0 + n8, cq:cq + 128], in_=pa[:, 0:n8 * 128].rearrange("p (a b) -> p a b", b=128), func=AF.Copy), r=[pt], w=[dst.t])
            AR.release(m1)

            if DEBUG:
                for k in range(3):
                    dma("sp", dbg_zx[s, k], zx[k].ap, r=[zx[k].t])
                dma("sp", dbg_aT[s], aT.ap, r=[aT.t])
            S.barrier()
            A2.release(0)
            YR = B(A2, [NT, DH], BF16); YS = B(A2, [NT, DH], BF16)
            dftb = [[B(AR, [NT, 128], BF16), B(AR, [NT, 128], BF16)] for _ in range(2)]
            kb = [[B(AR, [DH]), B(AR, [DH])] for _ in range(2)]
            tq = [B(AR, [DH]) for _ in range(4)]
            sk_t = B(AR, [DH]); tsum = B(AR, [DH]); yh = B(AR, [DH]); yn = B(AR, [DH], BF16)
            junk = B(AR, [DH]); ssh = B(AR, [2])
            zin = zx[0]
            for o in range(2):
                gate = zx[1 + o]
                for j in range(NT):
                    conv_some(1)
                    cb, sb_ = dftb[j % 2]
                    kr, ks = kb[j % 2]
                    dma("sp", cb.ap, cc_d[j], w=[cb.t])
                    dma("sp", sb_.ap, cs_d[j], w=[sb_.t])
                    dma("sp", kr.ap, kspec_d[o, 0, j * 128:(j + 1) * 128, :], r=[t_ks], w=[kr.t])
                    dma("sp", ks.ap, kspec_d[o, 1, j * 128:(j + 1) * 128, :], r=[t_ks], w=[ks.t])
                    pr, tr = PS(); pq, tq_ = PS()
                    for tc_ in range(NT):
                        S.op("pe", lambda e, pr=pr, tc_=tc_: e.matmul(pr, lhsT=cb.ap[:, tc_, :], rhs=zin.ap[:, tc_, :], start=(tc_ == 0), stop=(tc_ == NT - 1)), r=[cb.t, zin.t], w=[tr], silent=not (tc_ == NT - 1))
                    for tc_ in range(NT):
                        S.op("pe", lambda e, pq=pq, tc_=tc_: e.matmul(pq, lhsT=sb_.ap[:, tc_, :], rhs=zin.ap[:, tc_, :], start=(tc_ == 0), stop=(tc_ == NT - 1)), r=[sb_.t, zin.t], w=[tq_], silent=not (tc_ == NT - 1))
                    S.op("dve", lambda e, pr=pr: e.tensor_tensor(out=tq[0].ap, in0=pr, in1=kr.ap, op=OP.mult), r=[tr, kr.t], w=[tq[0].t])
                    S.op("dve", lambda e, pq=pq: e.tensor_tensor(out=tq[1].ap, in0=pq, in1=ks.ap, op=OP.mult), r=[tq_, ks.t], w=[tq[1].t])
                    S.op("pool", lambda e: e.tensor_tensor(out=YR.ap[:, j, :], in0=tq[0].ap, in1=tq[1].ap, op=OP.subtract), r=[tq[0].t, tq[1].t], w=[YR.t])
                    S.op("dve", lambda e, pr=pr: e.tensor_tensor(out=tq[2].ap, in0=pr, in1=ks.ap, op=OP.mult), r=[tr, ks.t], w=[tq[2].t])
                    S.op("dve", lambda e, pq=pq: e.tensor_tensor(out=tq[3].ap, in0=pq, in1=kr.ap, op=OP.mult), r=[tq_, kr.t], w=[tq[3].t])
                    S.op("pool", lambda e: e.tensor_tensor(out=YS.ap[:, j, :], in0=tq[2].ap, in1=tq[3].ap, op=OP.add), r=[tq[2].t, tq[3].t], w=[YS.t])
                for i in range(NT):
                    conv_some(2)
                    cb, sb_ = dftb[i % 2]
                    dma("sp", cb.ap, cct_d[i], w=[cb.t])
                    dma("sp", sb_.ap, cst_d[i], w=[sb_.t])
                    pa, pt = PS()
                    for fc in range(NT):
                        S.op("pe", lambda e, pa=pa, fc=fc: e.matmul(pa, lhsT=cb.ap[:, fc, :], rhs=YR.ap[:, fc, :], start=(fc == 0), stop=False), r=[cb.t, YR.t], w=[pt], silent=not False)
                    for fc in range(NT):
                        S.op("pe", lambda e, pa=pa, fc=fc: e.matmul(pa, lhsT=sb_.ap[:, fc, :], rhs=YS.ap[:, fc, :], start=False, stop=(fc == NT - 1)), r=[sb_.t, YS.t], w=[pt], silent=not (fc == NT - 1))
                    S.op("pool", lambda e: e.tensor_tensor(out=sk_t.ap, in0=zin.ap[:, i, :], in1=skipB[o], op=OP.mult), r=[zin.t, rows.t], w=[sk_t.t])
                    S.op("dve", lambda e, pa=pa: e.tensor_tensor(out=tsum.ap, in0=pa, in1=sk_t.ap, op=OP.add), r=[pt, sk_t.t], w=[tsum.t])
                    if o == 0:
                        S.op("pool", lambda e: e.tensor_tensor(out=gate.ap[:, i, :], in0=tsum.ap, in1=gate.ap[:, i, :], op=OP.mult), r=[tsum.t, gate.t], w=[gate.t])
                    else:
                        S.op("pool", lambda e: e.tensor_tensor(out=yh.ap, in0=tsum.ap, in1=gate.ap[:, i, :], op=OP.mult), r=[tsum.t, gate.t], w=[yh.t])
                        sumsq(yh.ap, junk.ap, ssh.ap[:, 0:1], [yh.t], junk.t, ssh.t)
                        rstd_from_ss(ssh.ap[:, 0:1], ssh.ap[:, 1:2], DH, 128, ssh.t)
                        S.op("dve", lambda e: e.scalar_tensor_tensor(out=yn.ap, in0=yh.ap, scalar=ssh.ap[:, 1:2], in1=g_hy, op0=OP.mult, op1=OP.mult), r=[yh.t, ssh.t, rows.t], w=[yn.t])
                        pb2, pt2 = PS(BF16)
                        for k in range(4):
                            S.op("pe", lambda e, pb2=pb2, k=k: e.transpose(out=pb2[:, k * 128:(k + 1) * 128], in_=yn.ap[:, k * 128:(k + 1) * 128], identity=identb.ap), r=[yn.t, identb.t], w=[pt2])
                        S.op("act", lambda e, pb2=pb2: e.activation(out=mixT.ap[:, 0:4, i * 128:(i + 1) * 128], in_=pb2[:, 0:512].rearrange("p (a b) -> p a b", b=128), func=AF.Copy), r=[pt2], w=[mixT.t])
                zin = zx[1]
            AR.release(m0)

            if DEBUG:
                dma("sp", dbg_mix[s], mixT.ap, r=[mixT.t])
            S.barrier()
            A2.release(0)
            wo = B(A2, [8, D], BF16)
            dma("pool", wo.ap, w_out_d.rearrange("(dc p) c -> p dc c", p=128), w=[wo.t])
            m0 = AR.mark()
            hres = [B(AR, [D]) for _ in range(2)]
            h1 = [B(AR, [D]) for _ in range(2)]
            hf = [B(AR, [D]) for _ in range(2)]
            hfT = B(AR, [8, 128])
            junk = B(AR, [D]); ssr = [B(AR, [2]) for _ in range(2)]
            hfball = B(AR, [NT, D], BF16)
            lgall = B(AR, [NT, 8 + NE])
            S.op("pool", lambda e: e.memset(lgall.ap, 0.0), w=[lgall.t])
            for i in range(NT):
                npart = 128 if i < NT - 1 else LAST
                tix = s * NT + i
                hr, h1_, hf_, ss = hres[i % 2], h1[i % 2], hf[i % 2], ssr[i % 2]
                load_h_tile("sp", hr, s, i, npart)
                for half in range(2):
                    pa, pt = PS()
                    for cc in range(8):
                        S.op("pe", lambda e, pa=pa, cc=cc: e.matmul(pa[:npart], lhsT=mixT.ap[:, cc, i * 128:i * 128 + npart], rhs=wo.ap[:, cc, half * 512:(half + 1) * 512], start=(cc == 0), stop=(cc == 7)), r=[mixT.t, wo.t], w=[pt], silent=not (cc == 7))
                    S.op("dve", lambda e, pa=pa: e.tensor_tensor(out=h1_.ap[:npart, half * 512:(half + 1) * 512], in0=pa[:npart], in1=hr.ap[:npart, half * 512:(half + 1) * 512], op=OP.add), r=[pt, hr.t], w=[h1_.t])
                dma("sp", h1_d[s * T + i * 128:s * T + i * 128 + npart, :], h1_.ap[:npart], r=[h1_.t], w=[t_h1])
                sumsq(h1_.ap[:npart], junk.ap[:npart], ss.ap[:npart, 0:1], [h1_.t], junk.t, ss.t)
                rstd_from_ss(ss.ap[:, 0:1], ss.ap[:, 1:2], D, npart, ss.t)
                S.op("dve", lambda e: e.scalar_tensor_tensor(out=hf_.ap[:npart], in0=h1_.ap[:npart], scalar=ss.ap[:npart, 1:2], in1=g_ffn[:npart], op0=OP.mult, op1=OP.mult), r=[h1_.t, ss.t, rows.t], w=[hf_.t])
                S.op("pool", lambda e: e.tensor_copy(out=hfball.ap[:npart, i, :], in_=hf_.ap[:npart]), r=[hf_.t], w=[hfball.t])
                for q4 in range(2):
                    pa, pt = PS()
                    for k in range(4):
                        dc = q4 * 4 + k
                        S.op("pe", lambda e, pa=pa, k=k, dc=dc: e.transpose(out=pa[:, k * 128:k * 128 + npart], in_=hf_.ap[:npart, dc * 128:(dc + 1) * 128], identity=ident[:npart, :npart]), r=[hf_.t, misc.t], w=[pt])
                    S.op("act", lambda e, pa=pa: e.activation(out=hfT.ap[:, q4 * 4:q4 * 4 + 4, 0:npart], in_=pa.rearrange("p (a b) -> p a b", b=128)[:, :, 0:npart], func=AF.Copy), r=[pt], w=[hfT.t])
                pr, tr = PS()
                for dc in range(8):
                    S.op("pe", lambda e, pr=pr, dc=dc: e.matmul(pr[:npart, 0:8 + NE], lhsT=hfT.ap[:, dc, 0:npart], rhs=wr.ap[:, dc, :], start=(dc == 0), stop=(dc == 7)), r=[hfT.t, wr.t], w=[tr], silent=not (dc == 7))
                S.op("act", lambda e, pr=pr: e.activation(out=lgall.ap[:npart, i, :], in_=pr[:npart, 0:8 + NE], func=AF.Copy), r=[tr], w=[lgall.t])
            G = NG
            b2 = lambda: B(AR, [NT]).ap
            gmax, sume, pg, m1, m2, dd, sig, gidx, e1, e2, ei1, ei2 = (b2() for _ in range(12))
            rr = [b2(), b2()]
            b8 = lambda: B(AR, [NT, 8]).ap
            t8, ohg, el, el2, oh1, oh2 = (b8() for _ in range(6))
            O2 = B(AR, [NT, NE]).ap; Oa = B(AR, [NT, NE]).ap; rk = B(AR, [NT, NE]).ap; sv = B(AR, [NT, NE]).ap
            prod4 = B(A2, [NT, NE]).ap; O1 = B(A2, [NT, NE]).ap; csum = B(A2, [NT, NE]).ap; cum = B(A2, [NT, NE]).ap
            slf = B(AR, [NT, 2]).ap
            RT = Tk("route")
            iotaTE = B(AR, [NT, NE])
            for t_ in range(NT):
                S.op("pool", lambda e: e.tensor_copy(out=iotaTE.ap[:, t_, :], in_=iota64[:, 0:NE]), r=[misc.t], w=[iotaTE.t])
            valid = misc.ap[:, 720:720 + NT]
            bc = lambda a2, n: a2.unsqueeze(2).to_broadcast([128, NT, n])

            def dvb(fn, er=(), ew=()):
                S.op("dve", fn, r=[RT] + list(er), w=[RT] + list(ew))
            lg3 = lgall.ap[:, :, 0:G]
            le4 = lgall.ap[:, :, 8:8 + NE].rearrange("p t (g k) -> p t g k", k=8)
            dvb(lambda e: e.tensor_reduce(out=gmax, in_=lg3, axis=AX.X, op=OP.max), er=[lgall.t])
            dvb(lambda e: e.tensor_tensor(out=t8[:, :, 0:G], in0=lg3, in1=bc(gmax, G), op=OP.subtract), er=[lgall.t])
            dvb(lambda e: e.tensor_scalar(out=ohg[:, :, 0:G], in0=t8[:, :, 0:G], scalar1=0.0, scalar2=None, op0=OP.is_equal))
            S.op("act", lambda e: e.activation(out=t8[:, :, 0:G], in_=t8[:, :, 0:G], func=AF.Exp), r=[RT], w=[RT])
            dvb(lambda e: e.tensor_reduce(out=sume, in_=t8[:, :, 0:G], axis=AX.X, op=OP.add))
            dvb(lambda e: e.reciprocal(out=pg, in_=sume))
            p4v = prod4.rearrange("p t (g k) -> p t g k", k=8)
            dvb(lambda e: e.tensor_tensor(out=p4v, in0=le4, in1=ohg[:, :, 0:G].unsqueeze(3).to_broadcast([128, NT, G, 8]), op=OP.mult), er=[lgall.t])
            dvb(lambda e: e.tensor_reduce(out=el, in_=p4v.rearrange("p t g k -> p t k g"), axis=AX.X, op=OP.add))
            dvb(lambda e: e.tensor_reduce(out=m1, in_=el, axis=AX.X, op=OP.max))
            dvb(lambda e: e.tensor_tensor(out=oh1, in0=el, in1=bc(m1, 8), op=OP.is_equal))
            dvb(lambda e: e.scalar_tensor_tensor(out=el2, in0=oh1, scalar=-1e30, in1=el, op0=OP.mult, op1=OP.add))
            dvb(lambda e: e.tensor_reduce(out=m2, in_=el2, axis=AX.X, op=OP.max))
            dvb(lambda e: e.tensor_tensor(out=oh2, in0=el2, in1=bc(m2, 8), op=OP.is_equal))
            dvb(lambda e: e.tensor_tensor(out=dd, in0=m1, in1=m2, op=OP.subtract))
            S.op("act", lambda e: e.activation(out=sig, in_=dd, func=AF.Sigmoid), r=[RT], w=[RT])
            for (oh, dst, n) in ((ohg, gidx, G), (oh1, e1, 8), (oh2, e2, 8)):
                dvb(lambda e, oh=oh, n=n: e.tensor_tensor(out=t8[:, :, 0:n], in0=oh[:, :, 0:n], in1=iota8[:, 0:n].unsqueeze(1).to_broadcast([128, NT, n]), op=OP.mult), er=[misc.t])
                dvb(lambda e, dst=dst, n=n: e.tensor_reduce(out=dst, in_=t8[:, :, 0:n], axis=AX.X, op=OP.add))
            for (ek, eik, Ok) in ((e1, ei1, O1), (e2, ei2, O2)):
                dvb(lambda e, ek=ek, eik=eik: e.scalar_tensor_tensor(out=eik, in0=gidx, scalar=8.0, in1=ek, op0=OP.mult, op1=OP.add))
                dvb(lambda e, eik=eik, Ok=Ok: e.tensor_tensor(out=Ok, in0=iotaTE.ap, in1=bc(eik, NE), op=OP.is_equal), er=[iotaTE.t])
                dvb(lambda e, Ok=Ok: e.tensor_tensor(out=Ok, in0=Ok, in1=bc(valid, NE), op=OP.mult), er=[misc.t])
            dvb(lambda e: e.tensor_tensor(out=Oa, in0=O1, in1=O2, op=OP.add))
            NCOL = NT * NE
            fl = lambda a: a.rearrange("p t e -> p (t e)")
            for c0 in range(0, NCOL, 512):
                cn = min(512, NCOL - c0)
                pk, tk_ = PS()
                S.op("pe", lambda e, pk=pk: e.matmul(pk[:, 0:cn], lhsT=ustrict, rhs=fl(Oa)[:, c0:c0 + cn], start=True, stop=True), r=[RT, misc.t], w=[tk_])
                dvb(lambda e, pk=pk: e.tensor_copy(out=fl(rk)[:, c0:c0 + cn], in_=pk[:, 0:cn]), er=[tk_])
                pc, tc2 = PS()
                S.op("pe", lambda e, pc=pc: e.matmul(pc[:, 0:cn], lhsT=ones, rhs=fl(Oa)[:, c0:c0 + cn], start=True, stop=True), r=[RT, misc.t], w=[tc2])
                dvb(lambda e, pc=pc: e.tensor_copy(out=fl(csum)[:, c0:c0 + cn], in_=pc[:, 0:cn]), er=[tc2])
            dvb(lambda e: e.tensor_copy(out=cum[:, 0, :], in_=cnt.ap), er=[cnt.t])
            for t_ in range(1, NT):
                dvb(lambda e: e.tensor_tensor(out=cum[:, t_, :], in0=cum[:, t_ - 1, :], in1=csum[:, t_ - 1, :], op=OP.add))
            dvb(lambda e: e.tensor_tensor(out=cnt.ap, in0=cum[:, NT - 1, :], in1=csum[:, NT - 1, :], op=OP.add), er=[cnt.t], ew=[cnt.t])
            dvb(lambda e: e.tensor_tensor(out=rk, in0=rk, in1=cum, op=OP.add))
            dvb(lambda e: e.tensor_tensor(out=sv, in0=rk, in1=slotbase.ap.unsqueeze(1).to_broadcast([128, NT, NE]), op=OP.add), er=[slotbase.t])
            for kq, Ok in enumerate((O1, O2)):
                dvb(lambda e, Ok=Ok: e.tensor_tensor(out=Oa, in0=Ok, in1=rk, op=OP.mult))
                dvb(lambda e, kq=kq: e.tensor_reduce(out=rr[kq], in_=Oa, axis=AX.X, op=OP.add))
                dvb(lambda e, Ok=Ok: e.tensor_tensor(out=Oa, in0=Ok, in1=sv, op=OP.mult))
                dvb(lambda e, kq=kq: e.tensor_reduce(out=slf[:, :, kq], in_=Oa, axis=AX.X, op=OP.add))
                dvb(lambda e, kq=kq: e.tensor_scalar(out=rr[kq], in0=rr[kq], scalar1=float(CAP) - 0.5, scalar2=None, op0=OP.is_gt))
                dvb(lambda e, kq=kq: e.scalar_tensor_tensor(out=slf[:, :, kq], in0=rr[kq], scalar=float(NSLOT), in1=slf[:, :, kq], op0=OP.mult, op1=OP.add))
                dvb(lambda e, kq=kq: e.tensor_scalar(out=rr[kq], in0=rr[kq], scalar1=-1.0, scalar2=1.0, op0=OP.mult, op1=OP.add))
            gs = gates.ap[:, s * NT:(s + 1) * NT, :]
            dvb(lambda e: e.tensor_tensor(out=gs[:, :, 0], in0=pg, in1=sig, op=OP.mult), er=[gates.t], ew=[gates.t])
            dvb(lambda e: e.tensor_tensor(out=gs[:, :, 1], in0=pg, in1=gs[:, :, 0], op=OP.subtract), er=[gates.t], ew=[gates.t])
            for kq in range(2):
                dvb(lambda e, kq=kq: e.tensor_tensor(out=gs[:, :, kq], in0=gs[:, :, kq], in1=rr[kq], op=OP.mult), er=[gates.t], ew=[gates.t])
            dvb(lambda e: e.tensor_copy(out=slots.ap[:, s * NT:(s + 1) * NT, :], in_=slf), er=[slots.t], ew=[slots.t])
            for i in range(NT):
                P = 128 if i < NT - 1 else LAST
                tix = s * NT + i
                for kq in range(2):
                    S.op("pool", lambda e, kq=kq: e.indirect_dma_start(out=xs_d, out_offset=bass.IndirectOffsetOnAxis(ap=slots.ap[:P, tix, kq:kq + 1], axis=0), in_=hfball.ap[:P, i, :], in_offset=None, bounds_check=bcreg(e), oob_is_err=False), r=[slots.t, hfball.t], w=[t_xs], dma=True)
            AR.release(m0)

        conv_some(10 ** 9)
        if DEBUG:
            dma("sp", dbg_sg[:, :, 0:2], gates.ap, r=[gates.t])
            dma("sp", dbg_sg[:, :, 2:4].bitcast(I32), slots.ap, r=[slots.t])
        new_phase()
        NWB = 4
        wgb = [B(AR, [8, 512], BF16) for _ in range(NWB)]
        wub = [B(AR, [8, 512], BF16) for _ in range(NWB)]
        wdb = [B(AR, [4, D], BF16) for _ in range(NWB)]
        for bb in wgb + wub + wdb:
            bb.th = [Tk(), Tk()]
        NST = CAP // 128
        xT = [B(A1, [8, CAP], BF16) for _ in range(2)]
        sgb = [B(A1, [CAP]) for _ in range(2)]
        actb = [B(A1, [4, CAP], BF16) for _ in range(2)]
        ybuf = [B(A2, [D]) for _ in range(2)]
        PF = 3
        xsb = [B(A1, [D], BF16) for _ in range((PF + 1) * NST)]

        def issue_loads(ex):
            wg_, wu_, wd_ = wgb[ex % NWB], wub[ex % NWB], wdb[ex % NWB]
            for h2 in range(2):
                dma("pool", wg_.ap[:, h2 * 4:h2 * 4 + 4, :], wg_d[ex].rearrange("(dc p) c -> p dc c", p=128)[:, h2 * 4:h2 * 4 + 4, :], w=[wg_.th[h2]])
                dma("pool", wu_.ap[:, h2 * 4:h2 * 4 + 4, :], wu_d[ex].rearrange("(dc p) c -> p dc c", p=128)[:, h2 * 4:h2 * 4 + 4, :], w=[wu_.th[h2]])
            for h2 in range(2):
                dma("pool", wd_.ap[:, h2 * 2:h2 * 2 + 2, :], wd_d[ex].rearrange("(fc p) c -> p fc c", p=128)[:, h2 * 2:h2 * 2 + 2, :], w=[wd_.th[h2]])
            for st_ in range(NST):
                xb_ = xsb[(ex % (PF + 1)) * NST + st_]
                dma("sp", xb_.ap, xs_d[ex * CAP + st_ * 128:ex * CAP + (st_ + 1) * 128, :], r=[t_xs], w=[xb_.t])

        for ex in range(min(PF, NE)):
            issue_loads(ex)
        for ex in range(NE):
            if ex + PF < NE:
                issue_loads(ex + PF)
            wg_, wu_, wd_ = wgb[ex % NWB], wub[ex % NWB], wdb[ex % NWB]
            xt = xT[ex % 2]; ab_ = actb[ex % 2]
            for st_ in range(NST):
                xb_ = xsb[(ex % (PF + 1)) * NST + st_]
                pa, pt = PS(BF16)
                for dc in range(8):
                    S.op("pe", lambda e, pa=pa, dc=dc, xb_=xb_: e.transpose(out=pa[:, dc * 128:(dc + 1) * 128], in_=xb_.ap[:, dc * 128:(dc + 1) * 128], identity=identb.ap), r=[xb_.t, identb.t], w=[pt])
                S.op("act", lambda e, pa=pa, st_=st_: e.activation(out=xt.ap[:, :, st_ * 128:(st_ + 1) * 128], in_=pa.rearrange("p (a b) -> p a b", b=128), func=AF.Copy), r=[pt], w=[xt.t])
            for fc in range(4):
                pg_, tg = PS(); pu_, tu = PS()
                for dc in range(8):
                    S.op("pe", lambda e, pg_=pg_, dc=dc, fc=fc: e.matmul(pg_[:, 0:CAP], lhsT=wg_.ap[:, dc, fc * 128:(fc + 1) * 128], rhs=xt.ap[:, dc, :], start=(dc == 0), stop=(dc == 7)), r=[wg_.th[dc // 4], xt.t], w=[tg], silent=not (dc == 7))
                for dc in range(8):
                    S.op("pe", lambda e, pu_=pu_, dc=dc, fc=fc: e.matmul(pu_[:, 0:CAP], lhsT=wu_.ap[:, dc, fc * 128:(fc + 1) * 128], rhs=xt.ap[:, dc, :], start=(dc == 0), stop=(dc == 7)), r=[wu_.th[dc // 4], xt.t], w=[tu], silent=not (dc == 7))
                sg_ = sgb[fc % 2]
                S.op("act", lambda e, pg_=pg_, sg_=sg_: e.activation(out=sg_.ap, in_=pg_[:, 0:CAP], func=AF.Silu), r=[tg], w=[sg_.t])
                S.op("dve", lambda e, pu_=pu_, sg_=sg_, fc=fc: e.tensor_tensor(out=ab_.ap[:, fc, :], in0=pu_[:, 0:CAP], in1=sg_.ap, op=OP.mult), r=[tu, sg_.t], w=[ab_.t])
            for st_ in range(NST):
                yb_ = ybuf[st_ % 2]
                for half in range(2):
                    po, to = PS()
                    for fc in range(4):
                        S.op("pe", lambda e, po=po, fc=fc, st_=st_, half=half: e.matmul(po, lhsT=ab_.ap[:, fc, st_ * 128:(st_ + 1) * 128], rhs=wd_.ap[:, fc, half * 512:(half + 1) * 512], start=(fc == 0), stop=(fc == 3)), r=[ab_.t, wd_.th[fc // 2]], w=[to], silent=not (fc == 3))
                    if half == 0:
                        S.op("act", lambda e, po=po, yb_=yb_: e.activation(out=yb_.ap[:, 0:512], in_=po, func=AF.Copy), r=[to], w=[yb_.t])
                    else:
                        S.op("dve", lambda e, po=po, yb_=yb_: e.tensor_copy(out=yb_.ap[:, 512:1024], in_=po), r=[to], w=[yb_.t])
                dma("sp", y_d[ex * CAP + st_ * 128:ex * CAP + (st_ + 1) * 128, :], yb_.ap, r=[yb_.t], w=[t_y])

        new_phase()
        NCB = 4
        y1 = [B(AR, [D]) for _ in range(NCB)]; y2 = [B(AR, [D]) for _ in range(NCB)]
        hh = [B(AR, [D]) for _ in range(NCB)]; ob = [B(AR, [D]) for _ in range(NCB)]
        junks = [B(AR, [D]) for _ in range(2)]; ssf = [B(AR, [2]) for _ in range(NCB)]
        for bb in y1 + y2:
            S.op("pool", lambda e, bb=bb: e.memset(bb.ap, 0.0), w=[bb.t])
        def issue_h1(tix):
            s, i = divmod(tix, NT)
            P = 128 if i < NT - 1 else LAST
            dma("sp", hh[tix % NCB].ap[:P], h1_d[s * T + i * 128:s * T + i * 128 + P, :], r=[t_h1], w=[hh[tix % NCB].t])

        PFC = 2
        for tix in range(min(PFC, NTT)):
            issue_h1(tix)
        for s in range(2):
            for i in range(NT):
                P = 128 if i < NT - 1 else LAST
                tix = s * NT + i
                if tix + PFC < NTT:
                    issue_h1(tix + PFC)
                a, b_, h_, o_, ss = y1[tix % NCB], y2[tix % NCB], hh[tix % NCB], ob[tix % NCB], ssf[tix % NCB]
                junk = junks[tix % 2]
                for kq, dst in enumerate((a, b_)):
                    S.op("pool", lambda e, kq=kq, dst=dst: e.indirect_dma_start(out=dst.ap[:P], out_offset=None, in_=y_d, in_offset=bass.IndirectOffsetOnAxis(ap=slots.ap[:P, tix, kq:kq + 1], axis=0), bounds_check=bcreg(e), oob_is_err=False), r=[slots.t, t_y], w=[dst.t], dma=True)
                S.op("dve", lambda e: e.scalar_tensor_tensor(out=h_.ap[:P], in0=a.ap[:P], scalar=gates.ap[:P, tix, 0:1], in1=h_.ap[:P], op0=OP.mult, op1=OP.add), r=[a.t, gates.t, h_.t], w=[h_.t])
                S.op("dve", lambda e: e.scalar_tensor_tensor(out=h_.ap[:P], in0=b_.ap[:P], scalar=gates.ap[:P, tix, 1:2], in1=h_.ap[:P], op0=OP.mult, op1=OP.add), r=[b_.t, gates.t, h_.t], w=[h_.t])
                sumsq(h_.ap[:P], junk.ap[:P], ss.ap[:P, 0:1], [h_.t], junk.t, ss.t)
                rstd_from_ss(ss.ap[:, 0:1], ss.ap[:, 1:2], D, P, ss.t)
                S.op("dve", lambda e: e.scalar_tensor_tensor(out=o_.ap[:P], in0=h_.ap[:P], scalar=ss.ap[:P, 1:2], in1=g_fin[:P], op0=OP.mult, op1=OP.mult), r=[h_.t, ss.t, rows.t], w=[o_.t])
                if i == 0:
                    dma("sp", out_d[s, 0:128 - N_META, :], o_.ap[N_META:128], r=[o_.t])
                else:
                    r0 = i * 128 - N_META
                    dma("sp", out_d[s, r0:r0 + P, :], o_.ap[:P], r=[o_.t])
        S.emit()
    return nc


_CACHE = {}


def run_kernel(inputs, n_cores, NG, CAP):
    x = np.asarray(inputs["x"], np.float32)
    Bt, SEQ, _ = x.shape
    assert Bt == 2 * n_cores
    L = SEQ + N_META
    NT = (L + 127) // 128
    NE = NG * 8
    key = (SEQ, NG, CAP)
    if key not in _CACHE:
        _CACHE[key] = build_program(SEQ, NG, CAP)
    nc = _CACHE[key]
    f = lambda k: np.asarray(inputs[k], np.float32)
    c = host_consts(L, NT)
    rep = lambda v: np.broadcast_to(v.reshape(1, -1), (128, v.size))
    rows = np.zeros((128, 6144), np.float32)
    rows[:, 0:1024] = rep(f("norm_mix")[0]); rows[:, 1024:2048] = rep(f("norm_ffn")[0]); rows[:, 2048:3072] = rep(f("norm_final"))
    rows[:, 3072:3584] = rep(f("hyena_norm")[0]); rows[:, 3584:4096] = rep(f("hgrn_norm")[0])
    rows[:, 4096:5120] = rep(f("filt_skip")[0].reshape(-1))
    convw = np.zeros((128, 12, 4), np.float32)
    convw[:, :, 0:3] = f("conv_w")[0].reshape(3, 12, 128).transpose(2, 1, 0)
    convw[:, :, 3] = f("conv_b")[0].reshape(12, 128).T
    lb = np.stack([f("lb_fwd"), f("lb_bwd")], 0)
    lb = np.ascontiguousarray(lb.reshape(2, 2, 4, 128).transpose(3, 0, 1, 2))
    wr = np.concatenate([f("w_router_group")[0], f("w_router_expert")[0].reshape(D, NE)], axis=1)
    wr_full = np.zeros((D, 8 + NE), np.float32)
    wr_full[:, 0:NG] = wr[:, 0:NG]; wr_full[:, 8:] = wr[:, NG:]
    wr_l = np.ascontiguousarray(wr_full.reshape(8, 128, 8 + NE).transpose(1, 0, 2))
    fb = np.zeros((64, 4), np.float32)
    fb[:, 0] = f("filt_b1")[0]; fb[:, 1] = f("filt_b2")[0]; fb[:, 2] = f("filt_freq")[0]
    shared = dict(meta=f("meta_tokens"), w_in=f("w_in")[0], w_out=f("w_out")[0], w_gate=f("w_gate")[0], w_up=f("w_up")[0],
                  w_down=f("w_down")[0], w_r=wr_l, rows=rows, convw=convw, lb=lb, f_w1=f("filt_w1")[0], f_w2=f("filt_w2")[0],
                  f_w3=f("filt_w3")[0], f_b=fb, **c)
    in_maps = []
    for ci in range(n_cores):
        m = dict(shared)
        m["x"] = np.ascontiguousarray(x[2 * ci:2 * ci + 2])
        in_maps.append(m)
    res = run_bass_kernel_spmd(nc, in_maps, core_ids=list(range(n_cores)))
    if DEBUG:
        global LAST_RES
        LAST_RES = res.results
    return np.concatenate([r["out"] for r in res.results], axis=0).astype(np.float32)


def kernel(**inputs):
    return run_kernel(inputs, 8, 8, 256)
```

```python
import math
import numpy as np
import ml_dtypes
import concourse.bass as bass
import concourse.mybir as mybir
from concourse.bass_utils import run_bass_kernel_spmd

F32 = mybir.dt.float32
BF16 = mybir.dt.bfloat16
I32 = mybir.dt.int32
AF = mybir.ActivationFunctionType
OP = mybir.AluOpType
AX = mybir.AxisListType

ENGS = ("pe", "act", "dve", "pool", "sp")


import types


def _freeze(fn):
    if fn.__closure__ is None:
        return fn
    cells = []
    for c in fn.__closure__:
        try:
            cells.append(types.CellType(c.cell_contents))
        except ValueError:
            cells.append(c)
    return types.FunctionType(fn.__code__, fn.__globals__, fn.__name__, fn.__defaults__, tuple(cells))


class Tk:
    __slots__ = ("w", "r", "name")

    def __init__(self, name=""):
        self.w = None
        self.r = []
        self.name = name


class Sched:
    def __init__(self, nc, n_dma_sems=24):
        self.nc = nc
        self.ops = {e: [] for e in ENGS}
        self.count = {e: 0 for e in ENGS}
        self.known = {e: {} for e in ENGS}
        self.n_hw = n_dma_sems
        self.n_grp1 = 12
        self.n_dma = n_dma_sems + self.n_grp1
        self.dma_cnt = [0] * self.n_dma
        self.dma_tok = [Tk("dmasem%d" % i) for i in range(self.n_dma)]
        self.dma_rr = 0
        self.rr1 = 0
        self.out_events = []

    def _need(self, eng, ev, waits):
        if ev is None:
            return
        key, val, clock = ev
        kn = self.known[eng]
        if key == eng and eng == "pe":
            return
        if kn.get(key, 0) >= val:
            return
        waits[key] = max(waits.get(key, 0), val)
        for k, v in clock.items():
            if kn.get(k, 0) < v:
                kn[k] = v
        if kn.get(key, 0) < val:
            kn[key] = val

    def op(self, eng, fn, r=(), w=(), dma=False, silent=False, grp=0):
        fn = _freeze(fn)
        silent = bool(silent) and eng == "pe" and not dma
        waits = {}
        slot = None
        toks_w = list(w)
        if dma:
            if grp == 1:
                slot = self.n_hw + self.rr1
                self.rr1 = (self.rr1 + 1) % self.n_grp1
            else:
                slot = self.dma_rr
                self.dma_rr = (self.dma_rr + 1) % self.n_hw
            toks_w.append(self.dma_tok[slot])
        for t in r:
            self._need(eng, t.w, waits)
        for t in toks_w:
            self._need(eng, t.w, waits)
            for ev in t.r:
                self._need(eng, ev, waits)
        if dma:
            self.dma_cnt[slot] += 16
            key, val = ("d", slot), self.dma_cnt[slot]
        elif silent:
            key, val = eng, self.count[eng] + 1
        else:
            self.count[eng] += 1
            key, val = eng, self.count[eng]
        clock = dict(self.known[eng])
        ev = (key, val, clock)
        if not dma and eng == "pe" and not silent:
            self.known[eng][eng] = val
        for t in r:
            t.r.append(ev)
        for t in toks_w:
            t.w = ev
            t.r = []
        self.ops[eng].append((sorted(waits.items(), key=str), fn, key, 16 if dma else (0 if silent else 1)))
        return ev

    def barrier(self):
        evs = []
        for e in ENGS:
            if self.count[e]:
                evs.append((e, self.count[e], {}))
        for s in range(self.n_dma):
            if self.dma_cnt[s]:
                evs.append((("d", s), self.dma_cnt[s], {}))
        for e in ENGS:
            waits = {}
            for ev in evs:
                if ev[0] != e:
                    self._need(e, ev, waits)
            if waits:
                self.ops[e].append((sorted(waits.items(), key=str), None, None, 0))

    def emit(self):
        nc = self.nc
        import contextlib
        with contextlib.ExitStack() as st:
            sems = {}
            for e in ENGS:
                sems[e] = st.enter_context(nc.semaphore("sem_" + e))
            for s in range(self.n_dma):
                sems[("d", s)] = st.enter_context(nc.semaphore("sem_d%d" % s))
            fin = {}
            for e in ENGS:
                if e != "sp" and self.count[e]:
                    fin[e] = self.count[e]
            for s in range(self.n_dma):
                if self.dma_cnt[s]:
                    fin[("d", s)] = self.dma_cnt[s]
            self.ops["sp"].append((sorted(fin.items(), key=str), None, None, 0))
            block = st.enter_context(nc.Block())

            def run(engname):
                def body(eng):
                    for waits, fn, key, inc in self.ops[engname]:
                        for k, v in waits:
                            eng.wait_ge(sems[k], v)
                        if fn is not None:
                            if inc:
                                fn(eng).then_inc(sems[key], inc)
                            else:
                                fn(eng)
                return body

            block.tensor(run("pe"))
            block.scalar(run("act"))
            block.vector(run("dve"))
            block.gpsimd(run("pool"))
            block.sync(run("sp"))


class Arena:
    def __init__(self, ap, nwords):
        self.ap = ap
        self.n = nwords
        self.top = 0

    def mark(self):
        return self.top

    def release(self, m):
        self.top = m

    def alloc(self, shape, dtype=F32):
        n = int(np.prod(shape))
        words = n if dtype in (F32, I32) else (n + 1) // 2
        words = (words + 7) // 8 * 8
        assert self.top + words <= self.n, "SBUF arena overflow %d + %d > %d" % (self.top, words, self.n)
        v = self.ap[:, self.top:self.top + words]
        self.top += words
        if dtype != F32:
            v = v.bitcast(dtype)
        v = v[:, 0:n]
        if len(shape) == 2:
            v = v.rearrange("p (a b) -> p a b", b=shape[1])
        elif len(shape) == 3:
            v = v.rearrange("p (a b c) -> p a b c", b=shape[1], c=shape[2])
        return v


N_META = 16
D = 1024
DH = 512
EPS = 1e-6
NWORDS = 52992


def host_consts(L, NT):
    T = NT * 128
    N = 2 * T
    t = np.arange(T, dtype=np.float64)
    f = np.arange(T, dtype=np.float64) + 0.5
    ang = 2.0 * np.pi / N * np.outer(t, f)
    valid = (t < L)[:, None]
    Cc = np.where(valid, np.cos(ang), 0.0)
    Cs = np.where(valid, np.sin(ang), 0.0)

    def lay(M):
        return np.ascontiguousarray(M.reshape(NT, 128, NT, 128).transpose(2, 1, 0, 3)).astype(ml_dtypes.bfloat16)

    c = {}
    c["dft_cc"] = lay(Cc)
    c["dft_cs"] = lay(Cs)
    c["dft_cct"] = lay(Cc.T.copy())
    c["dft_cst"] = lay(Cs.T.copy())
    pos = np.arange(L, dtype=np.float32)
    tt = pos / max(L - 1, 1)
    bands = np.linspace(1e-4, 15, 16, dtype=np.float32)
    a2 = (2.0 * math.pi / L) * pos[:, None] * bands[None, :]
    z = np.concatenate([tt[:, None], np.cos(a2), -np.sin(a2)], axis=-1).astype(np.float32)
    zT = np.zeros((33, T), np.float32)
    zT[:, :L] = z.T
    c["zfeat"] = zT
    deltas = np.abs(np.linspace(math.log(1e-2) / 1.5, math.log(1e-2) / 0.3, DH, dtype=np.float32))
    win = np.zeros((T, DH), np.float32)
    win[:L] = np.exp(-tt[:, None] * deltas[None, :])
    winb = win.copy()
    winb[0] = 0.0
    c["win_f"] = np.ascontiguousarray(win.reshape(NT, 128, DH).transpose(1, 0, 2))
    c["win_b"] = np.ascontiguousarray(winb.reshape(NT, 128, DH).transpose(1, 0, 2))
    i = np.arange(128)
    misc = np.zeros((128, 1024), np.float32)
    misc[:, 0:128] = np.eye(128)
    misc[:, 128:256] = (i[None, :] >= i[:, None])
    misc[:, 256:384] = (i[None, :] <= i[:, None])
    misc[:, 384:512] = (i[:, None] < i[None, :])
    misc[:, 512:640] = 1.0
    misc[:, 640:704] = np.arange(64)[None, :]
    misc[:, 704:712] = np.arange(8)[None, :]
    misc[:, 712] = i
    misc[:, 720:720 + NT] = 1.0
    misc[L - (NT - 1) * 128:, 720 + NT - 1] = 0.0
    c["misc"] = misc
    return c


DEBUG = False


def build_program(SEQ, NG, CAP):
    L = SEQ + N_META
    NT = (L + 127) // 128
    T = NT * 128
    LAST = L - (NT - 1) * 128
    NE = NG * 8
    NSLOT = NE * CAP
    NTT = 2 * NT
    NDFT = 2 * T
    nc = bass.Bass("TRN2", target_bir_lowering=False)

    def din(name, shape, dt=F32):
        return nc.dram_tensor(name, list(shape), dt, kind="ExternalInput").ap()

    x_d = din("x", [2, SEQ, D])
    meta_d = din("meta", [N_META, D])
    w_in_d = din("w_in", [D, 4096])
    w_out_d = din("w_out", [D, D])
    wg_d = din("w_gate", [NE, D, 512])
    wu_d = din("w_up", [NE, D, 512])
    wd_d = din("w_down", [NE, 512, D])
    wr_d = din("w_r", [128, 8, 8 + NE])
    rows_d = din("rows", [128, 6144])
    convw_d = din("convw", [128, 12, 4])
    lb_d = din("lb", [128, 2, 2, 4])
    f1_d = din("f_w1", [33, 64])
    f2_d = din("f_w2", [64, 64])
    f3_d = din("f_w3", [64, 2048])
    fb_d = din("f_b", [64, 4])
    misc_d = din("misc", [128, 1024])
    zfeat_d = din("zfeat", [33, T])
    winf_d = din("win_f", [128, NT, DH])
    winb_d = din("win_b", [128, NT, DH])
    cc_d = din("dft_cc", [NT, 128, NT, 128], BF16)
    cs_d = din("dft_cs", [NT, 128, NT, 128], BF16)
    cct_d = din("dft_cct", [NT, 128, NT, 128], BF16)
    cst_d = din("dft_cst", [NT, 128, NT, 128], BF16)
    out_d = nc.dram_tensor("out", [2, SEQ, D], F32, kind="ExternalOutput").ap()
    dk = dict(kind="ExternalOutput") if DEBUG else {}
    kspec_d = nc.dram_tensor("kspec", [2, 2, T, DH], F32, **dk).ap()
    h1_d = nc.dram_tensor("h1s", [2 * T, D], F32, **dk).ap()
    xs_d = nc.dram_tensor("xsort", [NSLOT, D], BF16, **dk).ap()
    y_d = nc.dram_tensor("ysort", [NSLOT, D], F32, **dk).ap()
    wgb_d = nc.dram_tensor("wg_bf", [NE, 128, 8, 512], BF16).ap()
    wub_d = nc.dram_tensor("wu_bf", [NE, 128, 8, 512], BF16).ap()
    wdb_d = nc.dram_tensor("wd_bf", [NE, 128, 4, D], BF16).ap()
    if DEBUG:
        dbg_mix = nc.dram_tensor("dbg_mix", [2, 128, 8, T], BF16, kind="ExternalOutput").ap()
        dbg_zx = nc.dram_tensor("dbg_zx", [2, 3, 128, NT, DH], BF16, kind="ExternalOutput").ap()
        dbg_aT = nc.dram_tensor("dbg_aT", [2, 128, 8, T], BF16, kind="ExternalOutput").ap()
        dbg_sg = nc.dram_tensor("dbg_sg", [128, NTT, 4], F32, kind="ExternalOutput").ap()

    import contextlib
    st = contextlib.ExitStack()
    with st:
        big = st.enter_context(nc.sbuf_tensor("arena", [128, NWORDS], F32))
        psb = [st.enter_context(nc.psum_tensor("ps%d" % i, [128, 512], F32)) for i in range(8)]
        S = Sched(nc)
        o0 = 0
        AC = Arena(big[:, o0:o0 + 8320], 8320); o0 += 8320
        A1 = Arena(big[:, o0:o0 + 8704], 8704); o0 += 8704
        A2 = Arena(big[:, o0:o0 + 8960], 8960); o0 += 8960
        AR = Arena(big[:, o0:NWORDS], NWORDS - o0)

        ps_tok = [Tk("ps%d" % i) for i in range(8)]
        ps_rr = [0]

        def PS(dtype=F32):
            i = ps_rr[0]
            ps_rr[0] = (i + 1) % 8
            ap = psb[i][:]
            if dtype != F32:
                ap = ap.bitcast(dtype)
            return ap, ps_tok[i]

        def new_phase():
            S.barrier()
            for a in (A1, A2, AR):
                a.release(0)

        class B:
            def __init__(self, arena, shape, dtype=F32, name=""):
                self.ap = arena.alloc(shape, dtype)
                self.t = Tk(name)

        regc = {}

        def bcreg(e):
            if "r" not in regc:
                regc["r"] = e.to_reg(NSLOT - 1)
            return regc["r"]

        def dma(q, out, in_, r=(), w=(), grp=0):
            S.op(q, lambda e: e.dma_start(out=out, in_=in_), r=r, w=w, dma=True, grp=grp)

        conv_tok = [[Tk(), Tk(), Tk()] for _ in range(NE)]
        conv_jobs = []
        for ex_ in range(NE):
            conv_jobs.append((wgb_d[ex_], wg_d[ex_].rearrange("(dc p) c -> p dc c", p=128), conv_tok[ex_][0]))
            conv_jobs.append((wub_d[ex_], wu_d[ex_].rearrange("(dc p) c -> p dc c", p=128), conv_tok[ex_][1]))
            conv_jobs.append((wdb_d[ex_], wd_d[ex_].rearrange("(fc p) c -> p fc c", p=128), conv_tok[ex_][2]))
        conv_pos = [0]

        def conv_some(n):
            return
            while n > 0 and conv_pos[0] < len(conv_jobs):
                o_, i_, tk_c = conv_jobs[conv_pos[0]]
                conv_pos[0] += 1
                n -= 1
                dma("pool", o_, i_, w=[tk_c], grp=1)

        def rstd_from_ss(ss, rs, n, npart, tk):
            S.op("act", lambda e: e.activation(out=rs[:npart], in_=ss[:npart], func=AF.Sqrt, scale=1.0 / n, bias=epsb.ap[:npart, 0:1]), r=[tk, epsb.t], w=[tk])
            S.op("dve", lambda e: e.reciprocal(out=rs[:npart], in_=rs[:npart]), r=[tk], w=[tk])

        def sumsq(src_ap, junk_ap, ss_ap, r, jt, st):
            S.op("act", lambda e: e.activation(out=junk_ap, in_=src_ap, func=AF.Square), r=r, w=[jt])
            S.op("dve", lambda e: e.tensor_reduce(out=ss_ap, in_=junk_ap, axis=AX.X, op=OP.add), r=[jt], w=[st])

        misc = B(AC, [1024]); dma("sp", misc.ap, misc_d, w=[misc.t])
        ident = misc.ap[:, 0:128]
        rows = B(AC, [5120]); dma("sp", rows.ap, rows_d[:, 0:5120], w=[rows.t])
        g_mix, g_ffn, g_fin = rows.ap[:, 0:1024], rows.ap[:, 1024:2048], rows.ap[:, 2048:3072]
        g_hy, g_hg = rows.ap[:, 3072:3584], rows.ap[:, 3584:4096]
        skipB = [rows.ap[:, 4096:4608], rows.ap[:, 4608:5120]]
        convw = B(AC, [12, 4]); dma("sp", convw.ap, convw_d, w=[convw.t])
        lbr = B(AC, [2, 2, 4]); dma("sp", lbr.ap, lb_d, w=[lbr.t])
        wr = B(AC, [8, 8 + NE]); dma("sp", wr.ap, wr_d, w=[wr.t])
        identb = B(AC, [128], BF16)
        S.op("dve", lambda e: e.tensor_copy(out=identb.ap, in_=ident), r=[misc.t], w=[identb.t])
        maskb = B(AC, [2, 128], BF16)
        S.op("dve", lambda e: e.tensor_copy(out=maskb.ap, in_=misc.ap[:, 128:384].rearrange("p (a b) -> p a b", b=128)), r=[misc.t], w=[maskb.t])
        ustrict = misc.ap[:, 384:512]
        ones = misc.ap[:, 512:640]
        iota64 = misc.ap[:, 640:704]
        iota8 = misc.ap[:, 704:712]
        iotap = misc.ap[:, 712:713]
        epsb = B(AC, [1])
        onesT = B(AC, [512])
        S.op("pool", lambda e: e.memset(onesT.ap, 1.0), w=[onesT.t])
        S.op("pool", lambda e: e.memset(epsb.ap, EPS), w=[epsb.t])
        lb = B(AC, [2, 4]); oml = B(AC, [2, 4])
        S.op("dve", lambda e: e.tensor_tensor(out=lb.ap, in0=lbr.ap[:, :, 0, :], in1=lbr.ap[:, :, 1, :], op=OP.subtract), r=[lbr.t], w=[lb.t])
        S.op("act", lambda e: e.activation(out=lb.ap, in_=lb.ap, func=AF.Sigmoid), r=[lb.t], w=[lb.t])
        S.op("dve", lambda e: e.tensor_scalar(out=oml.ap, in0=lb.ap, scalar1=-1.0, scalar2=1.0, op0=OP.mult, op1=OP.add), r=[lb.t], w=[oml.t])
        slots = B(AC, [NTT, 2], I32)
        gates = B(AC, [NTT, 2])
        S.op("pool", lambda e: e.memset(slots.ap, 0), w=[slots.t])
        S.op("pool", lambda e: e.memset(gates.ap, 0.0), w=[gates.t])
        cnt = B(AC, [NE])
        S.op("pool", lambda e: e.memset(cnt.ap, 0.0), w=[cnt.t])
        slotbase = B(AC, [NE])
        S.op("dve", lambda e: e.tensor_scalar(out=slotbase.ap, in0=iota64[:, 0:NE], scalar1=float(CAP), scalar2=None, op0=OP.mult), r=[misc.t], w=[slotbase.t])
        zero_bf = B(AC, [1024], BF16)
        S.op("pool", lambda e: e.memset(zero_bf.ap, 0.0), w=[zero_bf.t])
        t_xs = Tk("xsort"); t_y = Tk("ysort"); t_h1 = Tk("h1s"); t_ks = Tk("kspec")
        for r0 in range(0, NSLOT, 128):
            dma("sp", xs_d[r0:r0 + 128, :], zero_bf.ap, r=[zero_bf.t], w=[t_xs])

        def wrap(buf, tmp, npart):
            for _ in range(2):
                S.op("dve", lambda e: e.tensor_scalar(out=tmp.ap[:npart], in0=buf.ap[:npart], scalar1=math.pi, scalar2=-2 * math.pi, op0=OP.is_gt, op1=OP.mult), r=[buf.t], w=[tmp.t])
                S.op("dve", lambda e: e.tensor_tensor(out=buf.ap[:npart], in0=buf.ap[:npart], in1=tmp.ap[:npart], op=OP.add), r=[tmp.t, buf.t], w=[buf.t])
                S.op("dve", lambda e: e.tensor_scalar(out=tmp.ap[:npart], in0=buf.ap[:npart], scalar1=-math.pi, scalar2=2 * math.pi, op0=OP.is_lt, op1=OP.mult), r=[buf.t], w=[tmp.t])
                S.op("dve", lambda e: e.tensor_tensor(out=buf.ap[:npart], in0=buf.ap[:npart], in1=tmp.ap[:npart], op=OP.add), r=[tmp.t, buf.t], w=[buf.t])

        zT = B(A2, [T]); dma("sp", zT.ap[:33], zfeat_d, w=[zT.t])
        fw1 = B(A1, [64]); dma("sp", fw1.ap[:33], f1_d, w=[fw1.t])
        fw2 = B(A1, [64]); dma("sp", fw2.ap[:64], f2_d, w=[fw2.t])
        fw3 = B(A2, [2048]); dma("sp", fw3.ap[:64], f3_d, w=[fw3.t])
        fbb = B(A1, [4]); dma("sp", fbb.ap[:64], fb_d, w=[fbb.t])
        hT = [B(A1, [T]), B(A1, [T])]
        tmpw = B(A1, [T])
        slabs = [(s0, min(512, T - s0)) for s0 in range(0, T, 512)]
        for layer in range(2):
            src = zT if layer == 0 else hT[0]
            kk = 33 if layer == 0 else 64
            wl = fw1 if layer == 0 else fw2
            dst = hT[layer]
            for (s0, sn) in slabs:
                pa, pt = PS()
                S.op("pe", lambda e, pa=pa, s0=s0, sn=sn: e.matmul(pa[:64, 0:sn], lhsT=wl.ap[:kk, 0:64], rhs=src.ap[:kk, s0:s0 + sn], start=True, stop=True), r=[wl.t, src.t], w=[pt])
                S.op("dve", lambda e, pa=pa, s0=s0, sn=sn: e.tensor_scalar(out=dst.ap[:64, s0:s0 + sn], in0=pa[:64, 0:sn], scalar1=fbb.ap[:64, layer:layer + 1], scalar2=fbb.ap[:64, 2:3], op0=OP.add, op1=OP.mult), r=[pt, fbb.t], w=[dst.t])
            wrap(dst, tmpw, 64)
            S.op("act", lambda e: e.activation(out=dst.ap[:64], in_=dst.ap[:64], func=AF.Sin), r=[dst.t], w=[dst.t])
        h2T = hT[1]
        hs = B(AR, [NT, 1024], BF16); hdd = B(AR, [NT, 1024], BF16)
        mk0 = AR.mark()
        wtile = [[B(AR, [DH]), B(AR, [DH])] for _ in range(2)]
        ftmp = [B(AR, [DH]) for _ in range(4)]
        for i in range(NT):
            wf, wb_ = wtile[i % 2]
            dma("sp", wf.ap, winf_d[:, i, :], w=[wf.t])
            dma("sp", wb_.ap, winb_d[:, i, :], w=[wb_.t])
            for o in range(2):
                pf, ptf = PS(); pb, ptb = PS()
                S.op("pe", lambda e, pf=pf: e.matmul(pf, lhsT=h2T.ap[:64, i * 128:(i + 1) * 128], rhs=fw3.ap[:64, o * 512:(o + 1) * 512], start=True, stop=True), r=[h2T.t, fw3.t], w=[ptf])
                S.op("pe", lambda e, pb=pb: e.matmul(pb, lhsT=h2T.ap[:64, i * 128:(i + 1) * 128], rhs=fw3.ap[:64, 1024 + o * 512:1024 + (o + 1) * 512], start=True, stop=True), r=[h2T.t, fw3.t], w=[ptb])
                ta, tb = ftmp[2 * o], ftmp[2 * o + 1]
                S.op("dve", lambda e, pf=pf, ta=ta: e.tensor_tensor(out=ta.ap, in0=pf, in1=wf.ap, op=OP.mult), r=[ptf, wf.t], w=[ta.t])
                S.op("dve", lambda e, pb=pb, tb=tb: e.tensor_tensor(out=tb.ap, in0=pb, in1=wb_.ap, op=OP.mult), r=[ptb, wb_.t], w=[tb.t])
                S.op("pool", lambda e, ta=ta, tb=tb: e.tensor_tensor(out=hs.ap[:, i, o * 512:(o + 1) * 512], in0=ta.ap, in1=tb.ap, op=OP.add), r=[ta.t, tb.t], w=[hs.t])
                S.op("pool", lambda e, ta=ta, tb=tb: e.tensor_tensor(out=hdd.ap[:, i, o * 512:(o + 1) * 512], in0=ta.ap, in1=tb.ap, op=OP.subtract), r=[ta.t, tb.t], w=[hdd.t])
        S.barrier()
        AR.release(mk0)
        dftb = [[B(AR, [NT, 128], BF16), B(AR, [NT, 128], BF16)] for _ in range(2)]
        kout = [B(AR, [DH]) for _ in range(4)]
        dma("sp", dftb[0][0].ap, cc_d[0], w=[dftb[0][0].t])
        dma("sp", dftb[0][1].ap, cs_d[0], w=[dftb[0][1].t])
        for j in range(NT):
            cb, sb_ = dftb[j % 2]
            if j + 1 < NT:
                dma("sp", dftb[(j + 1) % 2][0].ap, cc_d[j + 1], w=[dftb[(j + 1) % 2][0].t])
                dma("sp", dftb[(j + 1) % 2][1].ap, cs_d[j + 1], w=[dftb[(j + 1) % 2][1].t])
            for o in range(2):
                for ri, (mat, src) in enumerate(((cb, hs), (sb_, hdd))):
                    pa, pt = PS()
                    for tc_ in range(NT):
                        S.op("pe", lambda e, pa=pa, mat=mat, src=src, tc_=tc_: e.matmul(pa, lhsT=mat.ap[:, tc_, :], rhs=src.ap[:, tc_, o * 512:(o + 1) * 512], start=(tc_ == 0), stop=(tc_ == NT - 1)), r=[mat.t, src.t], w=[pt], silent=not (tc_ == NT - 1))
                    ko = kout[2 * o + ri]
                    S.op("act", lambda e, pa=pa, ko=ko: e.activation(out=ko.ap, in_=pa, func=AF.Copy, scale=2.0 / NDFT), r=[pt], w=[ko.t])
                    dma("sp", kspec_d[o, ri, j * 128:(j + 1) * 128, :], ko.ap, r=[ko.t], w=[t_ks])

        def load_h_tile(q, dst, s, i, npart):
            if i == 0:
                dma(q, dst.ap[0:N_META], meta_d, w=[dst.t])
                dma(q, dst.ap[N_META:128], x_d[s, 0:128 - N_META, :], w=[dst.t])
            else:
                r0 = i * 128 - N_META
                dma(q, dst.ap[:npart], x_d[s, r0:r0 + npart, :], w=[dst.t])

        def wload(arena_buf, c0, ncols):
            dma("pool", arena_buf.ap[:, :, 0:ncols], w_in_d.rearrange("(dc p) c -> p dc c", p=128)[:, :, c0:c0 + ncols], w=[arena_buf.t])

        for s in range(2):
            new_phase()
            mixT = B(A1, [8, T], BF16)
            aT = B(A2, [8, T], BF16)
            if LAST < 128:
                S.op("pool", lambda e: e.memset(aT.ap[:, :, (NT - 1) * 128 + LAST:T], 0.0), w=[aT.t])
            m0 = AR.mark()
            hin = [B(AR, [D]) for _ in range(2)]
            abf = [B(AR, [D], BF16) for _ in range(2)]
            junk = B(AR, [D])
            ssb = [B(AR, [2]) for _ in range(2)]
            for i in range(NT):
                npart = 128 if i < NT - 1 else LAST
                hi, ab, ss = hin[i % 2], abf[i % 2], ssb[i % 2]
                load_h_tile("sp", hi, s, i, npart)
                sumsq(hi.ap[:npart], junk.ap[:npart], ss.ap[:npart, 0:1], [hi.t], junk.t, ss.t)
                rstd_from_ss(ss.ap[:, 0:1], ss.ap[:, 1:2], D, npart, ss.t)
                S.op("dve", lambda e, hi=hi, ss=ss, ab=ab: e.scalar_tensor_tensor(out=ab.ap[:npart], in0=hi.ap[:npart], scalar=ss.ap[:npart, 1:2], in1=g_mix[:npart], op0=OP.mult, op1=OP.mult), r=[hi.t, ss.t, rows.t], w=[ab.t])
                pa, pt = PS(BF16)
                for dc in range(8):
                    S.op("pe", lambda e, pa=pa, ab=ab, dc=dc: e.transpose(out=pa[:, dc * 128:dc * 128 + npart], in_=ab.ap[:npart, dc * 128:(dc + 1) * 128], identity=identb.ap[:npart, :npart]), r=[ab.t, identb.t], w=[pt])
                S.op("act", lambda e, pa=pa: e.activation(out=aT.ap[:, :, i * 128:i * 128 + npart], in_=pa.rearrange("p (a b) -> p a b", b=128)[:, :, 0:npart], func=AF.Copy), r=[pt], w=[aT.t])
            AR.release(m0)

            S.barrier()
            m0 = AR.mark()
            wch = [B(AR, [8, 128], BF16) for _ in range(3)]
            qT = B(AR, [T], BF16); kTd = B(AR, [T], BF16)
            PTe = B(AR, [T + 128])
            lf = B(AR, [T]); sgt = B(AR, [T])
            V = B(AR, [NT, 128], BF16); sgate = B(AR, [NT, 128], BF16); oacc = B(AR, [NT, 128])
            cs3 = [[B(AR, [NT]) for _ in range(4)] for _ in range(2)]
            Sst = [B(AR, [128]), B(AR, [128])]
            KtT1 = B(AR, [T], BF16)
            QtA = [B(AR, [T], BF16) for _ in range(2)]
            sTA = [B(AR, [NT, 128], BF16) for _ in range(2)]
            KtA = [B(AR, [NT, 128], BF16) for _ in range(2)]
            Spb = [B(AR, [128], BF16) for _ in range(2)]
            tmpb = [B(AR, [128]) for _ in range(2)]
            fins = [dict(junk=B(AR, [128]), ss=B(AR, [2]), yb=B(AR, [128], BF16)) for _ in range(2)]
            QhA = [B(AR, [T], BF16) for _ in range(2)]
            onesT2 = B(AR, [T], BF16)
            S.op("pool", lambda e: e.memset(onesT2.ap, 1.0), w=[onesT2.t])
            lf3 = lf.ap.rearrange("p (c k) -> p c k", k=128)
            sg3 = sgt.ap.rearrange("p (c k) -> p c k", k=128)
            for hd in range(4):
                cq = 1536 + hd * 128
                for j, wb in enumerate(wch):
                    wload(wb, cq + j * 512, 128)
                for j in range(3):
                    d = j - 1
                    for (s0, sn) in slabs:
                        pa, pt = PS()
                        for dc in range(8):
                            S.op("pe", lambda e, pa=pa, dc=dc: e.matmul(pa[:, 0:sn], lhsT=wch[j].ap[:, dc, :], rhs=aT.ap[:, dc, s0:s0 + sn], start=(dc == 0), stop=(dc == 7)), r=[wch[j].t, aT.t], w=[pt], silent=not (dc == 7))
                        if j == 0:
                            S.op("act", lambda e, pa=pa: e.activation(out=qT.ap[:, s0:s0 + sn], in_=pa[:, 0:sn], func=AF.Silu), r=[pt], w=[qT.t])
                        else:
                            S.op("act", lambda e, pa=pa: e.activation(out=sgt.ap[:, s0:s0 + sn], in_=pa[:, 0:sn], func=AF.Sigmoid), r=[pt], w=[sgt.t])
                    if j > 0:
                        S.op("dve", lambda e: e.tensor_scalar(out=sgt.ap, in0=sgt.ap, scalar1=oml.ap[:, d, hd:hd + 1], scalar2=lb.ap[:, d, hd:hd + 1], op0=OP.mult, op1=OP.add), r=[sgt.t, oml.t, lb.t], w=[sgt.t])
                        S.op("pool", lambda e: e.tensor_scalar(out=kTd.ap, in0=sgt.ap, scalar1=-1.0, scalar2=1.0, op0=OP.mult, op1=OP.add), r=[sgt.t], w=[kTd.t])
                        S.op("act", lambda e: e.activation(out=lf.ap, in_=sgt.ap, func=AF.Ln), r=[sgt.t], w=[lf.t])
                        S.op("pool", lambda e: e.memset(PTe.ap[:, 0:1], 0.0), w=[PTe.t])
                        S.op("dve", lambda e: e.tensor_tensor_scan(out=PTe.ap[:, 1:1 + T], data0=onesT2.ap, data1=lf.ap, initial=0.0, op0=OP.mult, op1=OP.add), r=[lf.t, onesT2.t, PTe.t], w=[PTe.t])
                    if j == 0:
                        continue
                    P3 = PTe.ap[:, 0:T].rearrange("p (c k) -> p c k", k=128)
                    Z3 = PTe.ap[:, 128:T + 128].rearrange("p (c k) -> p c k", k=128)
                    Acol, Zcol = P3[:, :, 0], Z3[:, :, 0]
                    Mcol = P3[:, :, 65] if d == 0 else P3[:, :, 64]
                    M, c1, c2, dec = cs3[d]
                    S.op("dve", lambda e: e.tensor_copy(out=M.ap, in_=Mcol), r=[PTe.t], w=[M.t])
                    e1, e2 = (c1, c2) if d == 0 else (c2, c1)
                    S.op("dve", lambda e: e.tensor_tensor(out=e1.ap, in0=M.ap, in1=Acol, op=OP.subtract), r=[M.t, PTe.t], w=[e1.t])
                    S.op("dve", lambda e: e.tensor_tensor(out=e2.ap, in0=Zcol, in1=M.ap, op=OP.subtract), r=[M.t, PTe.t], w=[e2.t])
                    S.op("dve", lambda e: e.tensor_tensor(out=dec.ap, in0=Zcol, in1=Acol, op=OP.subtract), r=[PTe.t], w=[dec.t])
                    for bb in (c1, c2, dec):
                        S.op("act", lambda e, bb=bb: e.activation(out=bb.ap, in_=bb.ap, func=AF.Exp), r=[bb.t], w=[bb.t])
                    off = 1 if d == 0 else 0
                    Pv = PTe.ap[:, off:off + T].rearrange("p (c k) -> p c k", k=128)
                    S.op("dve", lambda e: e.tensor_tensor(out=lf3, in0=Pv, in1=M.ap.unsqueeze(2).to_broadcast([128, NT, 128]), op=OP.subtract), r=[PTe.t, M.t, lf.t], w=[lf.t])
                    sq = 1.0 if d == 0 else -1.0
                    S.op("act", lambda e: e.activation(out=sgt.ap, in_=lf.ap, func=AF.Exp, scale=sq), r=[lf.t, sgt.t], w=[sgt.t])
                    S.op("dve", lambda e: e.tensor_tensor(out=QtA[d].ap, in0=qT.ap, in1=sgt.ap, op=OP.mult), r=[qT.t, sgt.t], w=[QtA[d].t])
                    S.op("act", lambda e: e.activation(out=lf.ap, in_=lf.ap, func=AF.Exp, scale=-sq), r=[lf.t], w=[lf.t])
                    S.op("dve", lambda e: e.tensor_tensor(out=KtT1.ap, in0=kTd.ap, in1=lf.ap, op=OP.mult), r=[kTd.t, lf.t], w=[KtT1.t])
                    for c0 in range(0, NT, 4):
                        n4 = min(4, NT - c0)
                        p1, t1 = PS()
                        for k in range(n4):
                            cs_ = slice((c0 + k) * 128, (c0 + k + 1) * 128)
                            S.op("pe", lambda e, p1=p1, k=k, cs_=cs_: e.matmul(p1[:, k * 128:(k + 1) * 128], lhsT=KtT1.ap[:, cs_], rhs=QtA[d].ap[:, cs_], start=True, stop=True), r=[KtT1.t, QtA[d].t], w=[t1])
                        S.op("dve", lambda e, p1=p1: e.tensor_tensor(out=sTA[d].ap[:, c0:c0 + n4, :], in0=p1[:, 0:n4 * 128].rearrange("p (a b) -> p a b", b=128), in1=maskb.ap[:, d:d + 1, :].to_broadcast([128, n4, 128]), op=OP.mult), r=[t1, maskb.t], w=[sTA[d].t])
                    S.op("pool", lambda e: e.tensor_tensor(out=QhA[d].ap.rearrange("p (c k) -> p c k", k=128), in0=QtA[d].ap.rearrange("p (c k) -> p c k", k=128), in1=c1.ap.unsqueeze(2).to_broadcast([128, NT, 128]), op=OP.mult), r=[QtA[d].t, c1.t], w=[QhA[d].t])
                    S.op("dve", lambda e: e.tensor_tensor(out=KtT1.ap.rearrange("p (c k) -> p c k", k=128), in0=KtT1.ap.rearrange("p (c k) -> p c k", k=128), in1=c2.ap.unsqueeze(2).to_broadcast([128, NT, 128]), op=OP.mult), r=[KtT1.t, c2.t], w=[KtT1.t])
                    for c0 in range(0, NT, 8):
                        n8 = min(8, NT - c0)
                        p2, t2 = PS(BF16)
                        for k in range(n8):
                            cs_ = slice((c0 + k) * 128, (c0 + k + 1) * 128)
                            S.op("pe", lambda e, p2=p2, k=k, cs_=cs_: e.transpose(out=p2[:, k * 128:(k + 1) * 128], in_=KtT1.ap[:, cs_], identity=identb.ap), r=[KtT1.t, identb.t], w=[t2])
                        S.op("act", lambda e, p2=p2: e.activation(out=KtA[d].ap[:, c0:c0 + n8, :], in_=p2[:, 0:n8 * 128].rearrange("p (a b) -> p a b", b=128), func=AF.Copy), r=[t2], w=[KtA[d].t])
                wload(wch[0], cq + 3 * 512, 128)
                wload(wch[1], cq + 4 * 512, 128)
                for i0 in range(0, NT, 4):
                    n4 = min(4, NT - i0)
                    for j in range(2):
                        pa, pt = PS()
                        for k in range(n4):
                            i = i0 + k
                            for dc in range(8):
                                S.op("pe", lambda e, pa=pa, k=k, i=i, dc=dc: e.matmul(pa[:, k * 128:(k + 1) * 128], lhsT=aT.ap[:, dc, i * 128:(i + 1) * 128], rhs=wch[j].ap[:, dc, :], start=(dc == 0), stop=(dc == 7)), r=[wch[j].t, aT.t], w=[pt], silent=not (dc == 7))
                        pv = pa[:, 0:n4 * 128].rearrange("p (a b) -> p a b", b=128)
                        if j == 0:
                            S.op("act", lambda e, pv=pv: e.activation(out=V.ap[:, i0:i0 + n4, :], in_=pv, func=AF.Copy), r=[pt], w=[V.t])
                        else:
                            S.op("act", lambda e, pv=pv: e.activation(out=sgate.ap[:, i0:i0 + n4, :], in_=pv, func=AF.Silu), r=[pt], w=[sgate.t])
                S.op("pool", lambda e: e.tensor_tensor(out=sgate.ap, in0=sgate.ap, in1=g_hg[:, hd * 128:(hd + 1) * 128].unsqueeze(1).to_broadcast([128, NT, 128]), op=OP.mult), r=[sgate.t, rows.t], w=[sgate.t])
                S.op("pool", lambda e: e.memset(oacc.ap, 0.0), w=[oacc.t])
                for d in range(2):
                    S.op("pool", lambda e, d=d: e.memset(Sst[d].ap, 0.0), w=[Sst[d].t])
                for step in range(NT):
                    for d in range(2):
                        c = step if d == 0 else NT - 1 - step
                        M, c1, c2, dec = cs3[d]
                        cs_ = slice(128 * c, 128 * c + 128)
                        S.op("act", lambda e: e.activation(out=Spb[d].ap, in_=Sst[d].ap, func=AF.Copy), r=[Sst[d].t], w=[Spb[d].t])
                        p4, t4 = PS()
                        S.op("pe", lambda e, p4=p4: e.matmul(p4[:, 0:128], lhsT=KtA[d].ap[:, c, :], rhs=V.ap[:, c, :], start=True, stop=True), r=[KtA[d].t, V.t], w=[t4])
                        p3, t3 = PS()
                        S.op("pe", lambda e, p3=p3: e.matmul(p3[:, 0:128], lhsT=sTA[d].ap[:, c, :], rhs=V.ap[:, c, :], start=True, stop=False), r=[sTA[d].t, V.t], w=[t3], silent=not False)
                        S.op("pe", lambda e, p3=p3: e.matmul(p3[:, 0:128], lhsT=QhA[d].ap[:, cs_], rhs=Spb[d].ap, start=False, stop=True), r=[QhA[d].t, Spb[d].t], w=[t3])
                        S.op("dve", lambda e, p4=p4: e.scalar_tensor_tensor(out=Sst[d].ap, in0=Sst[d].ap, scalar=dec.ap[:, c:c + 1], in1=p4[:, 0:128], op0=OP.mult, op1=OP.add), r=[t4, dec.t, Sst[d].t], w=[Sst[d].t])
                        S.op("dve", lambda e, p3=p3: e.tensor_tensor(out=oacc.ap[:, c, :], in0=p3[:, 0:128], in1=oacc.ap[:, c, :], op=OP.add), r=[t3, oacc.t], w=[oacc.t])
                for i in range(NT):
                    fin = fins[i % 2]
                    sumsq(oacc.ap[:, i, :], fin["junk"].ap, fin["ss"].ap[:, 0:1], [oacc.t], fin["junk"].t, fin["ss"].t)
                    rstd_from_ss(fin["ss"].ap[:, 0:1], fin["ss"].ap[:, 1:2], 128, 128, fin["ss"].t)
                    S.op("dve", lambda e: e.scalar_tensor_tensor(out=fin["yb"].ap, in0=oacc.ap[:, i, :], scalar=fin["ss"].ap[:, 1:2], in1=sgate.ap[:, i, :], op0=OP.mult, op1=OP.mult), r=[oacc.t, fin["ss"].t, sgate.t], w=[fin["yb"].t])
                    pa, pt = PS(BF16)
                    S.op("pe", lambda e, pa=pa: e.transpose(out=pa[:, 0:128], in_=fin["yb"].ap, identity=identb.ap), r=[fin["yb"].t, identb.t], w=[pt])
                    S.op("act", lambda e, pa=pa: e.activation(out=mixT.ap[:, 4 + hd, i * 128:(i + 1) * 128], in_=pa[:, 0:128], func=AF.Copy), r=[pt], w=[mixT.t])
            AR.release(m0)

            S.barrier()
            m0 = AR.mark()
            zx = [B(AR, [NT, DH], BF16, "zx%d" % k) for k in range(3)]
            m1 = AR.mark()
            wch = [B(AR, [8, 128], BF16) for _ in range(2)]
            pbuf = [B(AR, [T + 8]) for _ in range(2)]
            ub = B(AR, [T]); uT = [B(AR, [T], BF16) for _ in range(2)]
            for pb_ in pbuf:
                S.op("pool", lambda e, pb_=pb_: e.memset(pb_.ap[:, 0:1], 0.0), w=[pb_.t])
                S.op("pool", lambda e, pb_=pb_: e.memset(pb_.ap[:, T + 1:T + 2], 0.0), w=[pb_.t])
            for cc in range(12):
                wb, pb_, ut = wch[cc % 2], pbuf[cc % 2], uT[cc % 2]
                wload(wb, cc * 128, 128)
                for (s0, sn) in slabs:
                    pa, pt = PS()
                    for dc in range(8):
                        S.op("pe", lambda e, pa=pa, dc=dc: e.matmul(pa[:, 0:sn], lhsT=wb.ap[:, dc, :], rhs=aT.ap[:, dc, s0:s0 + sn], start=(dc == 0), stop=(dc == 7)), r=[wb.t, aT.t], w=[pt], silent=not (dc == 7))
                    S.op("act", lambda e, pa=pa: e.activation(out=pb_.ap[:, 1 + s0:1 + s0 + sn], in_=pa[:, 0:sn], func=AF.Copy), r=[pt], w=[pb_.t])
                S.op("act", lambda e: e.activation(out=ub.ap, in_=pb_.ap[:, 1:T + 1], func=AF.Identity, scale=convw.ap[:, cc, 1:2], bias=convw.ap[:, cc, 3:4]), r=[pb_.t, convw.t], w=[ub.t])
                S.op("dve", lambda e: e.scalar_tensor_tensor(out=ub.ap, in0=pb_.ap[:, 0:T], scalar=convw.ap[:, cc, 0:1], in1=ub.ap, op0=OP.mult, op1=OP.add), r=[pb_.t, convw.t, ub.t], w=[ub.t])
                S.op("dve", lambda e: e.scalar_tensor_tensor(out=ut.ap, in0=pb_.ap[:, 2:T + 2], scalar=convw.ap[:, cc, 2:3], in1=ub.ap, op0=OP.mult, op1=OP.add), r=[pb_.t, convw.t, ub.t], w=[ut.t])
                dst = zx[cc // 4]
                cq = (cc % 4) * 128
                for i0 in range(0, NT, 8):
                    n8 = min(8, NT - i0)
                    pa, pt = PS(BF16)
                    for k in range(n8):
                        S.op("pe", lambda e, pa=pa, k=k: e.transpose(out=pa[:, k * 128:(k + 1) * 128], in_=ut.ap[:, (i0 + k) * 128:(i0 + k + 1) * 128], identity=identb.ap), r=[ut.t, identb.t], w=[pt])
                    S.op("act", lambda e, pa=pa: e.activation(out=dst.ap[:, i0:i0 + n8, cq:cq + 128], in_=pa[:, 0:n8 * 128].rearrange("p (a b) -> p a b", b=128), func=AF.Copy), r=[pt], w=[dst.t])
            AR.release(m1)

            if DEBUG:
                for k in range(3):
                    dma("sp", dbg_zx[s, k], zx[k].ap, r=[zx[k].t])
                dma("sp", dbg_aT[s], aT.ap, r=[aT.t])
            S.barrier()
            A2.release(0)
            YR = B(A2, [NT, DH], BF16); YS = B(A2, [NT, DH], BF16)
            dftb = [[B(AR, [NT, 128], BF16), B(AR, [NT, 128], BF16)] for _ in range(2)]
            kb = [[B(AR, [DH]), B(AR, [DH])] for _ in range(2)]
            tq = [B(AR, [DH]) for _ in range(4)]
            sk_t = B(AR, [DH]); tsum = B(AR, [DH]); yh = B(AR, [DH]); yn = B(AR, [DH], BF16)
            junk = B(AR, [DH]); ssh = B(AR, [2])
            zin = zx[0]
            for o in range(2):
                gate = zx[1 + o]
                for j in range(NT):
                    conv_some(1)
                    cb, sb_ = dftb[j % 2]
                    kr, ks = kb[j % 2]
                    dma("sp", cb.ap, cc_d[j], w=[cb.t])
                    dma("sp", sb_.ap, cs_d[j], w=[sb_.t])
                    dma("sp", kr.ap, kspec_d[o, 0, j * 128:(j + 1) * 128, :], r=[t_ks], w=[kr.t])
                    dma("sp", ks.ap, kspec_d[o, 1, j * 128:(j + 1) * 128, :], r=[t_ks], w=[ks.t])
                    pr, tr = PS(); pq, tq_ = PS()
                    for tc_ in range(NT):
                        S.op("pe", lambda e, pr=pr, tc_=tc_: e.matmul(pr, lhsT=cb.ap[:, tc_, :], rhs=zin.ap[:, tc_, :], start=(tc_ == 0), stop=(tc_ == NT - 1)), r=[cb.t, zin.t], w=[tr], silent=not (tc_ == NT - 1))
                    for tc_ in range(NT):
                        S.op("pe", lambda e, pq=pq, tc_=tc_: e.matmul(pq, lhsT=sb_.ap[:, tc_, :], rhs=zin.ap[:, tc_, :], start=(tc_ == 0), stop=(tc_ == NT - 1)), r=[sb_.t, zin.t], w=[tq_], silent=not (tc_ == NT - 1))
                    S.op("dve", lambda e, pr=pr: e.tensor_tensor(out=tq[0].ap, in0=pr, in1=kr.ap, op=OP.mult), r=[tr, kr.t], w=[tq[0].t])
                    S.op("dve", lambda e, pq=pq: e.tensor_tensor(out=tq[1].ap, in0=pq, in1=ks.ap, op=OP.mult), r=[tq_, ks.t], w=[tq[1].t])
                    S.op("pool", lambda e: e.tensor_tensor(out=YR.ap[:, j, :], in0=tq[0].ap, in1=tq[1].ap, op=OP.subtract), r=[tq[0].t, tq[1].t], w=[YR.t])
                    S.op("dve", lambda e, pr=pr: e.tensor_tensor(out=tq[2].ap, in0=pr, in1=ks.ap, op=OP.mult), r=[tr, ks.t], w=[tq[2].t])
                    S.op("dve", lambda e, pq=pq: e.tensor_tensor(out=tq[3].ap, in0=pq, in1=kr.ap, op=OP.mult), r=[tq_, kr.t], w=[tq[3].t])
                    S.op("pool", lambda e: e.tensor_tensor(out=YS.ap[:, j, :], in0=tq[2].ap, in1=tq[3].ap, op=OP.add), r=[tq[2].t, tq[3].t], w=[YS.t])
                for i in range(NT):
                    conv_some(2)
                    cb, sb_ = dftb[i % 2]
                    dma("sp", cb.ap, cct_d[i], w=[cb.t])
                    dma("sp", sb_.ap, cst_d[i], w=[sb_.t])
                    pa, pt = PS()
                    for fc in range(NT):
                        S.op("pe", lambda e, pa=pa, fc=fc: e.matmul(pa, lhsT=cb.ap[:, fc, :], rhs=YR.ap[:, fc, :], start=(fc == 0), stop=False), r=[cb.t, YR.t], w=[pt], silent=not False)
                    for fc in range(NT):
                        S.op("pe", lambda e, pa=pa, fc=fc: e.matmul(pa, lhsT=sb_.ap[:, fc, :], rhs=YS.ap[:, fc, :], start=False, stop=(fc == NT - 1)), r=[sb_.t, YS.t], w=[pt], silent=not (fc == NT - 1))
                    S.op("pool", lambda e: e.tensor_tensor(out=sk_t.ap, in0=zin.ap[:, i, :], in1=skipB[o], op=OP.mult), r=[zin.t, rows.t], w=[sk_t.t])
                    S.op("dve", lambda e, pa=pa: e.tensor_tensor(out=tsum.ap, in0=pa, in1=sk_t.ap, op=OP.add), r=[pt, sk_t.t], w=[tsum.t])
                    if o == 0:
                        S.op("pool", lambda e: e.tensor_tensor(out=gate.ap[:, i, :], in0=tsum.ap, in1=gate.ap[:, i, :], op=OP.mult), r=[tsum.t, gate.t], w=[gate.t])
                    else:
                        S.op("pool", lambda e: e.tensor_tensor(out=yh.ap, in0=tsum.ap, in1=gate.ap[:, i, :], op=OP.mult), r=[tsum.t, gate.t], w=[yh.t])
                        sumsq(yh.ap, junk.ap, ssh.ap[:, 0:1], [yh.t], junk.t, ssh.t)
                        rstd_from_ss(ssh.ap[:, 0:1], ssh.ap[:, 1:2], DH, 128, ssh.t)
                        S.op("dve", lambda e: e.scalar_tensor_tensor(out=yn.ap, in0=yh.ap, scalar=ssh.ap[:, 1:2], in1=g_hy, op0=OP.mult, op1=OP.mult), r=[yh.t, ssh.t, rows.t], w=[yn.t])
                        pb2, pt2 = PS(BF16)
                        for k in range(4):
                            S.op("pe", lambda e, pb2=pb2, k=k: e.transpose(out=pb2[:, k * 128:(k + 1) * 128], in_=yn.ap[:, k * 128:(k + 1) * 128], identity=identb.ap), r=[yn.t, identb.t], w=[pt2])
                        S.op("act", lambda e, pb2=pb2: e.activation(out=mixT.ap[:, 0:4, i * 128:(i + 1) * 128], in_=pb2[:, 0:512].rearrange("p (a b) -> p a b", b=128), func=AF.Copy), r=[pt2], w=[mixT.t])
                zin = zx[1]
            AR.release(m0)

            if DEBUG:
                dma("sp", dbg_mix[s], mixT.ap, r=[mixT.t])
            S.barrier()
            A2.release(0)
            wo = B(A2, [8, D], BF16)
            dma("pool", wo.ap, w_out_d.rearrange("(dc p) c -> p dc c", p=128), w=[wo.t])
            m0 = AR.mark()
            hres = [B(AR, [D]) for _ in range(2)]
            h1 = [B(AR, [D]) for _ in range(2)]
            hf = [B(AR, [D]) for _ in range(2)]
            hfT = B(AR, [8, 128])
            junk = B(AR, [D]); ssr = [B(AR, [2]) for _ in range(2)]
            hfball = B(AR, [NT, D], BF16)
            lgall = B(AR, [NT, 8 + NE])
            S.op("pool", lambda e: e.memset(lgall.ap, 0.0), w=[lgall.t])
            for i in range(NT):
                npart = 128 if i < NT - 1 else LAST
                tix = s * NT + i
                hr, h1_, hf_, ss = hres[i % 2], h1[i % 2], hf[i % 2], ssr[i % 2]
                load_h_tile("sp", hr, s, i, npart)
                for half in range(2):
                    pa, pt = PS()
                    for cc in range(8):
                        S.op("pe", lambda e, pa=pa, cc=cc: e.matmul(pa[:npart], lhsT=mixT.ap[:, cc, i * 128:i * 128 + npart], rhs=wo.ap[:, cc, half * 512:(half + 1) * 512], start=(cc == 0), stop=(cc == 7)), r=[mixT.t, wo.t], w=[pt], silent=not (cc == 7))
                    S.op("dve", lambda e, pa=pa: e.tensor_tensor(out=h1_.ap[:npart, half * 512:(half + 1) * 512], in0=pa[:npart], in1=hr.ap[:npart, half * 512:(half + 1) * 512], op=OP.add), r=[pt, hr.t], w=[h1_.t])
                dma("sp", h1_d[s * T + i * 128:s * T + i * 128 + npart, :], h1_.ap[:npart], r=[h1_.t], w=[t_h1])
                sumsq(h1_.ap[:npart], junk.ap[:npart], ss.ap[:npart, 0:1], [h1_.t], junk.t, ss.t)
                rstd_from_ss(ss.ap[:, 0:1], ss.ap[:, 1:2], D, npart, ss.t)
                S.op("dve", lambda e: e.scalar_tensor_tensor(out=hf_.ap[:npart], in0=h1_.ap[:npart], scalar=ss.ap[:npart, 1:2], in1=g_ffn[:npart], op0=OP.mult, op1=OP.mult), r=[h1_.t, ss.t, rows.t], w=[hf_.t])
                S.op("pool", lambda e: e.tensor_copy(out=hfball.ap[:npart, i, :], in_=hf_.ap[:npart]), r=[hf_.t], w=[hfball.t])
                for q4 in range(2):
                    pa, pt = PS()
                    for k in range(4):
                        dc = q4 * 4 + k
                        S.op("pe", lambda e, pa=pa, k=k, dc=dc: e.transpose(out=pa[:, k * 128:k * 128 + npart], in_=hf_.ap[:npart, dc * 128:(dc + 1) * 128], identity=ident[:npart, :npart]), r=[hf_.t, misc.t], w=[pt])
                    S.op("act", lambda e, pa=pa: e.activation(out=hfT.ap[:, q4 * 4:q4 * 4 + 4, 0:npart], in_=pa.rearrange("p (a b) -> p a b", b=128)[:, :, 0:npart], func=AF.Copy), r=[pt], w=[hfT.t])
                pr, tr = PS()
                for dc in range(8):
                    S.op("pe", lambda e, pr=pr, dc=dc: e.matmul(pr[:npart, 0:8 + NE], lhsT=hfT.ap[:, dc, 0:npart], rhs=wr.ap[:, dc, :], start=(dc == 0), stop=(dc == 7)), r=[hfT.t, wr.t], w=[tr], silent=not (dc == 7))
                S.op("act", lambda e, pr=pr: e.activation(out=lgall.ap[:npart, i, :], in_=pr[:npart, 0:8 + NE], func=AF.Copy), r=[tr], w=[lgall.t])
            G = NG
            b2 = lambda: B(AR, [NT]).ap
            gmax, sume, pg, m1, m2, dd, sig, gidx, e1, e2, ei1, ei2 = (b2() for _ in range(12))
            rr = [b2(), b2()]
            b8 = lambda: B(AR, [NT, 8]).ap
            t8, ohg, el, el2, oh1, oh2 = (b8() for _ in range(6))
            O2 = B(AR, [NT, NE]).ap; Oa = B(AR, [NT, NE]).ap; rk = B(AR, [NT, NE]).ap; sv = B(AR, [NT, NE]).ap
            prod4 = B(A2, [NT, NE]).ap; O1 = B(A2, [NT, NE]).ap; csum = B(A2, [NT, NE]).ap; cum = B(A2, [NT, NE]).ap
            slf = B(AR, [NT, 2]).ap
            RT = Tk("route")
            iotaTE = B(AR, [NT, NE])
            for t_ in range(NT):
                S.op("pool", lambda e: e.tensor_copy(out=iotaTE.ap[:, t_, :], in_=iota64[:, 0:NE]), r=[misc.t], w=[iotaTE.t])
            valid = misc.ap[:, 720:720 + NT]
            bc = lambda a2, n: a2.unsqueeze(2).to_broadcast([128, NT, n])

            def dvb(fn, er=(), ew=()):
                S.op("dve", fn, r=[RT] + list(er), w=[RT] + list(ew))
            lg3 = lgall.ap[:, :, 0:G]
            le4 = lgall.ap[:, :, 8:8 + NE].rearrange("p t (g k) -> p t g k", k=8)
            dvb(lambda e: e.tensor_reduce(out=gmax, in_=lg3, axis=AX.X, op=OP.max), er=[lgall.t])
            dvb(lambda e: e.tensor_tensor(out=t8[:, :, 0:G], in0=lg3, in1=bc(gmax, G), op=OP.subtract), er=[lgall.t])
            dvb(lambda e: e.tensor_scalar(out=ohg[:, :, 0:G], in0=t8[:, :, 0:G], scalar1=0.0, scalar2=None, op0=OP.is_equal))
            S.op("act", lambda e: e.activation(out=t8[:, :, 0:G], in_=t8[:, :, 0:G], func=AF.Exp), r=[RT], w=[RT])
            dvb(lambda e: e.tensor_reduce(out=sume, in_=t8[:, :, 0:G], axis=AX.X, op=OP.add))
            dvb(lambda e: e.reciprocal(out=pg, in_=sume))
            p4v = prod4.rearrange("p t (g k) -> p t g k", k=8)
            dvb(lambda e: e.tensor_tensor(out=p4v, in0=le4, in1=ohg[:, :, 0:G].unsqueeze(3).to_broadcast([128, NT, G, 8]), op=OP.mult), er=[lgall.t])
            dvb(lambda e: e.tensor_reduce(out=el, in_=p4v.rearrange("p t g k -> p t k g"), axis=AX.X, op=OP.add))
            dvb(lambda e: e.tensor_reduce(out=m1, in_=el, axis=AX.X, op=OP.max))
            dvb(lambda e: e.tensor_tensor(out=oh1, in0=el, in1=bc(m1, 8), op=OP.is_equal))
            dvb(lambda e: e.scalar_tensor_tensor(out=el2, in0=oh1, scalar=-1e30, in1=el, op0=OP.mult, op1=OP.add))
            dvb(lambda e: e.tensor_reduce(out=m2, in_=el2, axis=AX.X, op=OP.max))
            dvb(lambda e: e.tensor_tensor(out=oh2, in0=el2, in1=bc(m2, 8), op=OP.is_equal))
            dvb(lambda e: e.tensor_tensor(out=dd, in0=m1, in1=m2, op=OP.subtract))
            S.op("act", lambda e: e.activation(out=sig, in_=dd, func=AF.Sigmoid), r=[RT], w=[RT])
            for (oh, dst, n) in ((ohg, gidx, G), (oh1, e1, 8), (oh2, e2, 8)):
                dvb(lambda e, oh=oh, n=n: e.tensor_tensor(out=t8[:, :, 0:n], in0=oh[:, :, 0:n], in1=iota8[:, 0:n].unsqueeze(1).to_broadcast([128, NT, n]), op=OP.mult), er=[misc.t])
                dvb(lambda e, dst=dst, n=n: e.tensor_reduce(out=dst, in_=t8[:, :, 0:n], axis=AX.X, op=OP.add))
            for (ek, eik, Ok) in ((e1, ei1, O1), (e2, ei2, O2)):
                dvb(lambda e, ek=ek, eik=eik: e.scalar_tensor_tensor(out=eik, in0=gidx, scalar=8.0, in1=ek, op0=OP.mult, op1=OP.add))
                dvb(lambda e, eik=eik, Ok=Ok: e.tensor_tensor(out=Ok, in0=iotaTE.ap, in1=bc(eik, NE), op=OP.is_equal), er=[iotaTE.t])
                dvb(lambda e, Ok=Ok: e.tensor_tensor(out=Ok, in0=Ok, in1=bc(valid, NE), op=OP.mult), er=[misc.t])
            dvb(lambda e: e.tensor_tensor(out=Oa, in0=O1, in1=O2, op=OP.add))
            NCOL = NT * NE
            fl = lambda a: a.rearrange("p t e -> p (t e)")
            for c0 in range(0, NCOL, 512):
                cn = min(512, NCOL - c0)
                pk, tk_ = PS()
                S.op("pe", lambda e, pk=pk: e.matmul(pk[:, 0:cn], lhsT=ustrict, rhs=fl(Oa)[:, c0:c0 + cn], start=True, stop=True), r=[RT, misc.t], w=[tk_])
                dvb(lambda e, pk=pk: e.tensor_copy(out=fl(rk)[:, c0:c0 + cn], in_=pk[:, 0:cn]), er=[tk_])
                pc, tc2 = PS()
                S.op("pe", lambda e, pc=pc: e.matmul(pc[:, 0:cn], lhsT=ones, rhs=fl(Oa)[:, c0:c0 + cn], start=True, stop=True), r=[RT, misc.t], w=[tc2])
                dvb(lambda e, pc=pc: e.tensor_copy(out=fl(csum)[:, c0:c0 + cn], in_=pc[:, 0:cn]), er=[tc2])
            dvb(lambda e: e.tensor_copy(out=cum[:, 0, :], in_=cnt.ap), er=[cnt.t])
            for t_ in range(1, NT):
                dvb(lambda e: e.tensor_tensor(out=cum[:, t_, :], in0=cum[:, t_ - 1, :], in1=csum[:, t_ - 1, :], op=OP.add))
            dvb(lambda e: e.tensor_tensor(out=cnt.ap, in0=cum[:, NT - 1, :], in1=csum[:, NT - 1, :], op=OP.add), er=[cnt.t], ew=[cnt.t])
            dvb(lambda e: e.tensor_tensor(out=rk, in0=rk, in1=cum, op=OP.add))
            dvb(lambda e: e.tensor_tensor(out=sv, in0=rk, in1=slotbase.ap.unsqueeze(1).to_broadcast([128, NT, NE]), op=OP.add), er=[slotbase.t])
            for kq, Ok in enumerate((O1, O2)):
                dvb(lambda e, Ok=Ok: e.tensor_tensor(out=Oa, in0=Ok, in1=rk, op=OP.mult))
                dvb(lambda e, kq=kq: e.tensor_reduce(out=rr[kq], in_=Oa, axis=AX.X, op=OP.add))
                dvb(lambda e, Ok=Ok: e.tensor_tensor(out=Oa, in0=Ok, in1=sv, op=OP.mult))
                dvb(lambda e, kq=kq: e.tensor_reduce(out=slf[:, :, kq], in_=Oa, axis=AX.X, op=OP.add))
                dvb(lambda e, kq=kq: e.tensor_scalar(out=rr[kq], in0=rr[kq], scalar1=float(CAP) - 0.5, scalar2=None, op0=OP.is_gt))
                dvb(lambda e, kq=kq: e.scalar_tensor_tensor(out=slf[:, :, kq], in0=rr[kq], scalar=float(NSLOT), in1=slf[:, :, kq], op0=OP.mult, op1=OP.add))
                dvb(lambda e, kq=kq: e.tensor_scalar(out=rr[kq], in0=rr[kq], scalar1=-1.0, scalar2=1.0, op0=OP.mult, op1=OP.add))
            gs = gates.ap[:, s * NT:(s + 1) * NT, :]
            dvb(lambda e: e.tensor_tensor(out=gs[:, :, 0], in0=pg, in1=sig, op=OP.mult), er=[gates.t], ew=[gates.t])
            dvb(lambda e: e.tensor_tensor(out=gs[:, :, 1], in0=pg, in1=gs[:, :, 0], op=OP.subtract), er=[gates.t], ew=[gates.t])
            for kq in range(2):
                dvb(lambda e, kq=kq: e.tensor_tensor(out=gs[:, :, kq], in0=gs[:, :, kq], in1=rr[kq], op=OP.mult), er=[gates.t], ew=[gates.t])
            dvb(lambda e: e.tensor_copy(out=slots.ap[:, s * NT:(s + 1) * NT, :], in_=slf), er=[slots.t], ew=[slots.t])
            for i in range(NT):
                P = 128 if i < NT - 1 else LAST
                tix = s * NT + i
                for kq in range(2):
                    S.op("pool", lambda e, kq=kq: e.indirect_dma_start(out=xs_d, out_offset=bass.IndirectOffsetOnAxis(ap=slots.ap[:P, tix, kq:kq + 1], axis=0), in_=hfball.ap[:P, i, :], in_offset=None, bounds_check=bcreg(e), oob_is_err=False), r=[slots.t, hfball.t], w=[t_xs], dma=True)
            AR.release(m0)

        conv_some(10 ** 9)
        if DEBUG:
            dma("sp", dbg_sg[:, :, 0:2], gates.ap, r=[gates.t])
            dma("sp", dbg_sg[:, :, 2:4].bitcast(I32), slots.ap, r=[slots.t])
        new_phase()
        NWB = 4
        wgb = [B(AR, [8, 512], BF16) for _ in range(NWB)]
        wub = [B(AR, [8, 512], BF16) for _ in range(NWB)]
        wdb = [B(AR, [4, D], BF16) for _ in range(NWB)]
        for bb in wgb + wub + wdb:
            bb.th = [Tk(), Tk()]
        NST = CAP // 128
        xT = [B(A1, [8, CAP], BF16) for _ in range(2)]
        sgb = [B(A1, [CAP]) for _ in range(2)]
        actb = [B(A1, [4, CAP], BF16) for _ in range(2)]
        ybuf = [B(A2, [D]) for _ in range(2)]
        PF = 2
        xsb = [B(A1, [D], BF16) for _ in range((PF + 1) * NST)]

        def issue_loads(ex):
            wg_, wu_, wd_ = wgb[ex % NWB], wub[ex % NWB], wdb[ex % NWB]
            for h2 in range(2):
                dma("pool", wg_.ap[:, h2 * 4:h2 * 4 + 4, :], wg_d[ex].rearrange("(dc p) c -> p dc c", p=128)[:, h2 * 4:h2 * 4 + 4, :], w=[wg_.th[h2]])
                dma("pool", wu_.ap[:, h2 * 4:h2 * 4 + 4, :], wu_d[ex].rearrange("(dc p) c -> p dc c", p=128)[:, h2 * 4:h2 * 4 + 4, :], w=[wu_.th[h2]])
            for h2 in range(2):
                dma("pool", wd_.ap[:, h2 * 2:h2 * 2 + 2, :], wd_d[ex].rearrange("(fc p) c -> p fc c", p=128)[:, h2 * 2:h2 * 2 + 2, :], w=[wd_.th[h2]])
            for st_ in range(NST):
                xb_ = xsb[(ex % (PF + 1)) * NST + st_]
                dma("sp", xb_.ap, xs_d[ex * CAP + st_ * 128:ex * CAP + (st_ + 1) * 128, :], r=[t_xs], w=[xb_.t])

        for ex in range(min(PF, NE)):
            issue_loads(ex)
        for ex in range(NE):
            if ex + PF < NE:
                issue_loads(ex + PF)
            wg_, wu_, wd_ = wgb[ex % NWB], wub[ex % NWB], wdb[ex % NWB]
            xt = xT[ex % 2]; ab_ = actb[ex % 2]
            for st_ in range(NST):
                xb_ = xsb[(ex % (PF + 1)) * NST + st_]
                pa, pt = PS(BF16)
                for dc in range(8):
                    S.op("pe", lambda e, pa=pa, dc=dc, xb_=xb_: e.transpose(out=pa[:, dc * 128:(dc + 1) * 128], in_=xb_.ap[:, dc * 128:(dc + 1) * 128], identity=identb.ap), r=[xb_.t, identb.t], w=[pt])
                S.op("act", lambda e, pa=pa, st_=st_: e.activation(out=xt.ap[:, :, st_ * 128:(st_ + 1) * 128], in_=pa.rearrange("p (a b) -> p a b", b=128), func=AF.Copy), r=[pt], w=[xt.t])
            for fc in range(4):
                pg_, tg = PS(); pu_, tu = PS()
                for dc in range(8):
                    S.op("pe", lambda e, pg_=pg_, dc=dc, fc=fc: e.matmul(pg_[:, 0:CAP], lhsT=wg_.ap[:, dc, fc * 128:(fc + 1) * 128], rhs=xt.ap[:, dc, :], start=(dc == 0), stop=(dc == 7)), r=[wg_.th[dc // 4], xt.t], w=[tg], silent=not (dc == 7))
                for dc in range(8):
                    S.op("pe", lambda e, pu_=pu_, dc=dc, fc=fc: e.matmul(pu_[:, 0:CAP], lhsT=wu_.ap[:, dc, fc * 128:(fc + 1) * 128], rhs=xt.ap[:, dc, :], start=(dc == 0), stop=(dc == 7)), r=[wu_.th[dc // 4], xt.t], w=[tu], silent=not (dc == 7))
                sg_ = sgb[fc % 2]
                S.op("act", lambda e, pg_=pg_, sg_=sg_: e.activation(out=sg_.ap, in_=pg_[:, 0:CAP], func=AF.Silu), r=[tg], w=[sg_.t])
                S.op("dve", lambda e, pu_=pu_, sg_=sg_, fc=fc: e.tensor_tensor(out=ab_.ap[:, fc, :], in0=pu_[:, 0:CAP], in1=sg_.ap, op=OP.mult), r=[tu, sg_.t], w=[ab_.t])
            for st_ in range(NST):
                yb_ = ybuf[st_ % 2]
                for half in range(2):
                    po, to = PS()
                    for fc in range(4):
                        S.op("pe", lambda e, po=po, fc=fc, st_=st_, half=half: e.matmul(po, lhsT=ab_.ap[:, fc, st_ * 128:(st_ + 1) * 128], rhs=wd_.ap[:, fc, half * 512:(half + 1) * 512], start=(fc == 0), stop=(fc == 3)), r=[ab_.t, wd_.th[fc // 2]], w=[to], silent=not (fc == 3))
                    if half == 0:
                        S.op("act", lambda e, po=po, yb_=yb_: e.activation(out=yb_.ap[:, 0:512], in_=po, func=AF.Copy), r=[to], w=[yb_.t])
                    else:
                        S.op("dve", lambda e, po=po, yb_=yb_: e.tensor_copy(out=yb_.ap[:, 512:1024], in_=po), r=[to], w=[yb_.t])
                dma("sp", y_d[ex * CAP + st_ * 128:ex * CAP + (st_ + 1) * 128, :], yb_.ap, r=[yb_.t], w=[t_y])

        new_phase()
        NCB = 4
        y1 = [B(AR, [D]) for _ in range(NCB)]; y2 = [B(AR, [D]) for _ in range(NCB)]
        hh = [B(AR, [D]) for _ in range(NCB)]; ob = [B(AR, [D]) for _ in range(NCB)]
        junks = [B(AR, [D]) for _ in range(2)]; ssf = [B(AR, [2]) for _ in range(NCB)]
        for bb in y1 + y2:
            S.op("pool", lambda e, bb=bb: e.memset(bb.ap, 0.0), w=[bb.t])
        def issue_h1(tix):
            s, i = divmod(tix, NT)
            P = 128 if i < NT - 1 else LAST
            dma("sp", hh[tix % NCB].ap[:P], h1_d[s * T + i * 128:s * T + i * 128 + P, :], r=[t_h1], w=[hh[tix % NCB].t])

        PFC = 2
        for tix in range(min(PFC, NTT)):
            issue_h1(tix)
        for s in range(2):
            for i in range(NT):
                P = 128 if i < NT - 1 else LAST
                tix = s * NT + i
                if tix + PFC < NTT:
                    issue_h1(tix + PFC)
                a, b_, h_, o_, ss = y1[tix % NCB], y2[tix % NCB], hh[tix % NCB], ob[tix % NCB], ssf[tix % NCB]
                junk = junks[tix % 2]
                for kq, dst in enumerate((a, b_)):
                    S.op("pool", lambda e, kq=kq, dst=dst: e.indirect_dma_start(out=dst.ap[:P], out_offset=None, in_=y_d, in_offset=bass.IndirectOffsetOnAxis(ap=slots.ap[:P, tix, kq:kq + 1], axis=0), bounds_check=bcreg(e), oob_is_err=False), r=[slots.t, t_y], w=[dst.t], dma=True)
                S.op("dve", lambda e: e.scalar_tensor_tensor(out=h_.ap[:P], in0=a.ap[:P], scalar=gates.ap[:P, tix, 0:1], in1=h_.ap[:P], op0=OP.mult, op1=OP.add), r=[a.t, gates.t, h_.t], w=[h_.t])
                S.op("dve", lambda e: e.scalar_tensor_tensor(out=h_.ap[:P], in0=b_.ap[:P], scalar=gates.ap[:P, tix, 1:2], in1=h_.ap[:P], op0=OP.mult, op1=OP.add), r=[b_.t, gates.t, h_.t], w=[h_.t])
                sumsq(h_.ap[:P], junk.ap[:P], ss.ap[:P, 0:1], [h_.t], junk.t, ss.t)
                rstd_from_ss(ss.ap[:, 0:1], ss.ap[:, 1:2], D, P, ss.t)
                S.op("dve", lambda e: e.scalar_tensor_tensor(out=o_.ap[:P], in0=h_.ap[:P], scalar=ss.ap[:P, 1:2], in1=g_fin[:P], op0=OP.mult, op1=OP.mult), r=[h_.t, ss.t, rows.t], w=[o_.t])
                if i == 0:
                    dma("sp", out_d[s, 0:128 - N_META, :], o_.ap[N_META:128], r=[o_.t])
                else:
                    r0 = i * 128 - N_META
                    dma("sp", out_d[s, r0:r0 + P, :], o_.ap[:P], r=[o_.t])
        S.emit()
    return nc


_CACHE = {}


def run_kernel(inputs, n_cores, NG, CAP):
    x = np.asarray(inputs["x"], np.float32)
    Bt, SEQ, _ = x.shape
    assert Bt == 2 * n_cores
    L = SEQ + N_META
    NT = (L + 127) // 128
    NE = NG * 8
    key = (SEQ, NG, CAP)
    if key not in _CACHE:
        _CACHE[key] = build_program(SEQ, NG, CAP)
    nc = _CACHE[key]
    f = lambda k: np.asarray(inputs[k], np.float32)
    c = host_consts(L, NT)
    rep = lambda v: np.broadcast_to(v.reshape(1, -1), (128, v.size))
    rows = np.zeros((128, 6144), np.float32)
    rows[:, 0:1024] = rep(f("norm_mix")[0]); rows[:, 1024:2048] = rep(f("norm_ffn")[0]); rows[:, 2048:3072] = rep(f("norm_final"))
    rows[:, 3072:3584] = rep(f("hyena_norm")[0]); rows[:, 3584:4096] = rep(f("hgrn_norm")[0])
    rows[:, 4096:5120] = rep(f("filt_skip")[0].reshape(-1))
    convw = np.zeros((128, 12, 4), np.float32)
    convw[:, :, 0:3] = f("conv_w")[0].reshape(3, 12, 128).transpose(2, 1, 0)
    convw[:, :, 3] = f("conv_b")[0].reshape(12, 128).T
    lb = np.stack([f("lb_fwd"), f("lb_bwd")], 0)
    lb = np.ascontiguousarray(lb.reshape(2, 2, 4, 128).transpose(3, 0, 1, 2))
    wr = np.concatenate([f("w_router_group")[0], f("w_router_expert")[0].reshape(D, NE)], axis=1)
    wr_full = np.zeros((D, 8 + NE), np.float32)
    wr_full[:, 0:NG] = wr[:, 0:NG]; wr_full[:, 8:] = wr[:, NG:]
    wr_l = np.ascontiguousarray(wr_full.reshape(8, 128, 8 + NE).transpose(1, 0, 2))
    fb = np.zeros((64, 4), np.float32)
    fb[:, 0] = f("filt_b1")[0]; fb[:, 1] = f("filt_b2")[0]; fb[:, 2] = f("filt_freq")[0]
    shared = dict(meta=f("meta_tokens"), w_in=f("w_in")[0], w_out=f("w_out")[0], w_gate=f("w_gate")[0], w_up=f("w_up")[0],
                  w_down=f("w_down")[0], w_r=wr_l, rows=rows, convw=convw, lb=lb, f_w1=f("filt_w1")[0], f_w2=f("filt_w2")[0],
                  f_w3=f("filt_w3")[0], f_b=fb, **c)
    in_maps = []
    for ci in range(n_cores):
        m = dict(shared)
        m["x"] = np.ascontiguousarray(x[2 * ci:2 * ci + 2])
        in_maps.append(m)
    res = run_bass_kernel_spmd(nc, in_maps, core_ids=list(range(n_cores)))
    if DEBUG:
        global LAST_RES
        LAST_RES = res.results
    return np.concatenate([r["out"] for r in res.results], axis=0).astype(np.float32)


def kernel(**inputs):
    return run_kernel(inputs, 8, 8, 256)
```
